# Optimizing a Trainium2 kernel written in Bass

```python
import math
import jax
import jax.numpy as jnp
from jax import lax
import numpy as np

D_MODEL = 1024
BATCH = 16
SEQ = 4096
DEPTH = 2

CTX_LEN = 256
GRID_W = 64

FNET_HEADS = 4
FNET_HEAD_DIM = 64
D_FNET = FNET_HEADS * FNET_HEAD_DIM
HYENA_HEADS = 6
HYENA_HEAD_DIM = 64
D_HYENA = HYENA_HEADS * HYENA_HEAD_DIM
HYENA_ORDER = 2
HYENA_EMB = 33
HYENA_BANDS = (HYENA_EMB - 1) // 2
HYENA_FILTER_HIDDEN = 64
HYENA_FAST_DECAY_PCT = 0.3
HYENA_SLOW_DECAY_PCT = 1.5
HYENA_DECAY_TARGET = 0.01
S5_GROUP = 16
S5_GROUPS = 24
D_S5 = S5_GROUPS * S5_GROUP
S5_STATE = 64
S5_DT_MIN = 0.001
S5_DT_MAX = 0.1
D_MIX = D_FNET + D_HYENA + D_S5
HYENA_OFF = D_FNET
S5_OFF = D_FNET + (HYENA_ORDER + 1) * D_HYENA
D_IN_PROJ = S5_OFF + D_S5
N_MOD = 6
D_FF = 2816
N_EXPERTS = 8
TOP_K = 2
D_FF_EXPERT = 3584
N_DENSE = (DEPTH + 1) // 2
N_MOE = DEPTH // 2
RMS_EPS = 1e-6

kernel_name = 'hybrid_fnet_hyena_s5_moe_dit'


def rmsnorm(x, g):
    xf = x.astype(jnp.float32)
    y = xf * lax.rsqrt(jnp.mean(xf * xf, axis=-1, keepdims=True) + RMS_EPS)
    return (y * g.astype(jnp.float32)).astype(x.dtype)


def modulate(h, shift, scale):
    return h * (1.0 + scale) + shift


def fourier_mix(u, w_f):
    b, l, _ = u.shape
    uh = u.astype(jnp.float32).reshape(b, l, FNET_HEADS, FNET_HEAD_DIM)
    f = jnp.fft.fft2(uh, axes=(1, 3), norm='ortho').real
    y = jnp.einsum('blhd,hde->blhe', f, w_f.astype(jnp.float32))
    return y.reshape(b, l, D_FNET).astype(u.dtype)


def short_conv_seq(u, w, bias):
    up = jnp.pad(u, ((0, 0), (1, 1), (0, 0)))
    return up[:, :-2] * w[0] + up[:, 1:-1] * w[1] + up[:, 2:] * w[2] + bias


def short_conv_grid(u, w, bias):
    b, l, ch = u.shape
    rows = l // GRID_W
    ug = jnp.pad(u.reshape(b, rows, GRID_W, ch), ((0, 0), (0, 0), (1, 1), (0, 0)))
    y = ug[:, :, :-2] * w[0] + ug[:, :, 1:-1] * w[1] + ug[:, :, 2:] * w[2] + bias
    return y.reshape(b, l, ch)


def hyena_filters(l, fw1, fb1, freq1, fw2, fb2, freq2, fw3):
    f32 = jnp.float32
    t = jnp.linspace(0.0, 1.0, l, dtype=f32)[:, None]
    w = (2.0 * math.pi / l) * jnp.arange(l, dtype=f32)[:, None]
    bands = jnp.linspace(1e-4, HYENA_BANDS - 1, HYENA_BANDS, dtype=f32)[None, :]
    z = jnp.concatenate([t, jnp.cos(bands * w), -jnp.sin(bands * w)], axis=-1)
    h = jnp.sin(freq1.astype(f32) * (z @ fw1.astype(f32) + fb1.astype(f32)))
    h = jnp.sin(freq2.astype(f32) * (h @ fw2.astype(f32) + fb2.astype(f32)))
    h = (h @ fw3.astype(f32)).reshape(l, 2, HYENA_ORDER, D_HYENA)
    max_decay = math.log(HYENA_DECAY_TARGET) / HYENA_FAST_DECAY_PCT
    min_decay = math.log(HYENA_DECAY_TARGET) / HYENA_SLOW_DECAY_PCT
    deltas = jnp.abs(jnp.linspace(min_decay, max_decay, D_HYENA, dtype=f32))
    h = h * jnp.exp(-t[:, :, None, None] * deltas)
    h_fwd, h_bwd = h[:, 0], h[:, 1]
    k = jnp.concatenate([h_fwd, jnp.zeros_like(h_fwd[:1]), h_bwd[:0:-1]], axis=0)
    return k / jnp.sum(jnp.abs(k), axis=0, keepdims=True)


def fft_long_conv(u, k, bias):
    l = u.shape[1]
    uf = jnp.fft.rfft(u, n=2 * l, axis=1)
    kf = jnp.fft.rfft(k, n=2 * l, axis=0)
    y = jnp.fft.irfft(uf * kf[None], n=2 * l, axis=1)[:, :l]
    return y + u * bias


def hyena_mix(p, grid, conv_w, conv_b, k, f_bias):
    f32 = jnp.float32
    conv = short_conv_grid if grid else short_conv_seq
    q = conv(p.astype(f32), conv_w.astype(f32), conv_b.astype(f32))
    parts = jnp.split(q, HYENA_ORDER + 1, axis=-1)
    z = parts[0]
    for o in range(HYENA_ORDER):
        z = parts[o + 1] * fft_long_conv(z, k[:, o], f_bias[o].astype(f32))
    return z.astype(p.dtype)


def _ssm_combine(e1, e2):
    a1, b1 = e1
    a2, b2 = e2
    return a2 * a1, a2 * b1 + b2


def s5_discretize(a_re, a_im, log_dt, b_re, b_im):
    f32 = jnp.float32
    lam = lax.complex(a_re.astype(f32), a_im.astype(f32))
    dt = jnp.exp(log_dt.astype(f32))[:, None]
    a_bar = jnp.exp(lam * dt)
    b_c = lax.complex(b_re.astype(f32), b_im.astype(f32))
    b_bar = ((a_bar - 1.0) / lam)[..., None] * b_c
    return a_bar, b_bar


def s5_scan(bu, a_bar, s0, reverse):
    l = bu.shape[1]
    if s0 is not None:
        edge = l - 1 if reverse else 0
        bu = bu.at[:, edge].add(a_bar * s0)
    a = jnp.broadcast_to(a_bar, (1, l) + a_bar.shape)
    _, h = lax.associative_scan(_ssm_combine, (a, bu), axis=1, reverse=reverse)
    return h


def s5_trajectories(u, a_re, a_im, log_dt, b_re, b_im, s0_fwd, s0_bwd):
    b, l, _ = u.shape
    ug = u.astype(jnp.float32).reshape(b, l, S5_GROUPS, S5_GROUP)
    states = []
    for d, (s0, rev) in enumerate(((s0_fwd, False), (s0_bwd, True))):
        a_bar, b_bar = s5_discretize(a_re[d], a_im[d], log_dt[d], b_re[d], b_im[d])
        bu = lax.complex(jnp.einsum('blgh,gph->blgp', ug, b_bar.real),
                         jnp.einsum('blgh,gph->blgp', ug, b_bar.imag))
        states.append(s5_scan(bu, a_bar, s0, rev))
    return states[0], states[1]


def s5_readout(u, h_fwd, h_bwd, c_re, c_im, d_skip, glu_w, glu_b):
    f32 = jnp.float32
    b, l, _ = u.shape
    y = u.astype(f32) * d_skip.astype(f32)
    for d, h in enumerate((h_fwd, h_bwd)):
        yd = (jnp.einsum('blgp,ghp->blgh', h.real, c_re[d].astype(f32))
              - jnp.einsum('blgp,ghp->blgh', h.imag, c_im[d].astype(f32)))
        y = y + yd.reshape(b, l, D_S5)
    y = jax.nn.gelu(y)
    y = y * jax.nn.sigmoid(y @ glu_w.astype(f32) + glu_b.astype(f32))
    return y.astype(u.dtype)


def swiglu(h, w_gate, w_up, w_down):
    return (jax.nn.silu(h @ w_gate) * (h @ w_up)) @ w_down


def moe_swiglu(h, router_w, router_b, w_gate, w_up, w_down):
    logits = (h @ router_w).astype(jnp.float32) + router_b.astype(jnp.float32)
    top_vals, top_idx = lax.top_k(logits, TOP_K)
    top_gates = jax.nn.softmax(top_vals, axis=-1)
    combine = jnp.sum(top_gates[..., None] * jax.nn.one_hot(top_idx, N_EXPERTS, dtype=jnp.float32), axis=-2)
    out = jnp.zeros_like(h)
    for e in range(N_EXPERTS):
        out = out + combine[..., e:e + 1].astype(h.dtype) * swiglu(h, w_gate[e], w_up[e], w_down[e])
    return out


def setup_inputs(seed: int = 0) -> dict:
    key = jax.random.key(seed)
    keys = iter(jax.random.split(key, 64))
    f32 = jnp.float32

    def nrm(shape, std):
        return std * jax.random.normal(next(keys), shape, f32)

    def gain(shape):
        return 1.0 + 0.01 * jax.random.normal(next(keys), shape, f32)

    s5_n = jnp.arange(S5_STATE, dtype=f32)
    hy_proj = (HYENA_ORDER + 1) * D_HYENA
    return {
        'x': nrm((BATCH, SEQ, D_MODEL), 1.0),
        'c': nrm((BATCH, D_MODEL), 1.0),
        'ctx': nrm((BATCH, CTX_LEN, D_MODEL), 1.0),
        'c_ctx': nrm((D_MODEL,), 1.0),
        'ada_w': nrm((DEPTH, D_MODEL, N_MOD * D_MODEL), 0.5 / math.sqrt(D_MODEL)),
        'ada_b': nrm((DEPTH, N_MOD * D_MODEL), 0.01),
        'norm_mix_g': gain((DEPTH, D_MODEL)),
        'norm_ffn_g': gain((DEPTH, D_MODEL)),
        'w_in': nrm((DEPTH, D_MODEL, D_IN_PROJ), D_MODEL ** -0.5),
        'w_out': nrm((DEPTH, D_MIX, D_MODEL), D_MIX ** -0.5),
        'fnet_w': nrm((DEPTH, FNET_HEADS, FNET_HEAD_DIM, FNET_HEAD_DIM), FNET_HEAD_DIM ** -0.5),
        'hy_conv_w': nrm((DEPTH, 3, hy_proj), 3 ** -0.5),
        'hy_conv_b': nrm((DEPTH, hy_proj), 0.01),
        'hy_fw1': nrm((DEPTH, HYENA_EMB, HYENA_FILTER_HIDDEN), HYENA_EMB ** -0.5),
        'hy_fb1': nrm((DEPTH, HYENA_FILTER_HIDDEN), 0.1),
        'hy_freq1': gain((DEPTH, HYENA_FILTER_HIDDEN)),
        'hy_fw2': nrm((DEPTH, HYENA_FILTER_HIDDEN, HYENA_FILTER_HIDDEN), HYENA_FILTER_HIDDEN ** -0.5),
        'hy_fb2': nrm((DEPTH, HYENA_FILTER_HIDDEN), 0.1),
        'hy_freq2': gain((DEPTH, HYENA_FILTER_HIDDEN)),
        'hy_fw3': nrm((DEPTH, HYENA_FILTER_HIDDEN, 2 * HYENA_ORDER * D_HYENA), HYENA_FILTER_HIDDEN ** -0.5),
        'hy_bias': 1.0 + nrm((DEPTH, HYENA_ORDER, D_HYENA), 0.1),
        's5_a_re': -0.5 + nrm((DEPTH, 2, S5_GROUPS, S5_STATE), 0.01),
        's5_a_im': math.pi * s5_n + nrm((DEPTH, 2, S5_GROUPS, S5_STATE), 0.01),
        's5_log_dt': jax.random.uniform(next(keys), (DEPTH, 2, S5_GROUPS), f32,
                                        math.log(S5_DT_MIN), math.log(S5_DT_MAX)),
        's5_b_re': nrm((DEPTH, 2, S5_GROUPS, S5_STATE, S5_GROUP), (2 * S5_GROUP) ** -0.5),
        's5_b_im': nrm((DEPTH, 2, S5_GROUPS, S5_STATE, S5_GROUP), (2 * S5_GROUP) ** -0.5),
        's5_c_re': nrm((DEPTH, 2, S5_GROUPS, S5_GROUP, S5_STATE), 0.25),
        's5_c_im': nrm((DEPTH, 2, S5_GROUPS, S5_GROUP, S5_STATE), 0.25),
        's5_d': nrm((DEPTH, D_S5), 1.0),
        's5_glu_w': nrm((DEPTH, D_S5, D_S5), D_S5 ** -0.5),
        's5_glu_b': nrm((DEPTH, D_S5), 0.01),
        'ffn_w_gate': nrm((N_DENSE, D_MODEL, D_FF), D_MODEL ** -0.5),
        'ffn_w_up': nrm((N_DENSE, D_MODEL, D_FF), D_MODEL ** -0.5),
        'ffn_w_down': nrm((N_DENSE, D_FF, D_MODEL), D_FF ** -0.5),
        'moe_router_w': nrm((N_MOE, D_MODEL, N_EXPERTS), D_MODEL ** -0.5),
        'moe_router_b': nrm((N_MOE, N_EXPERTS), 0.01),
        'moe_w_gate': nrm((N_MOE, N_EXPERTS, D_MODEL, D_FF_EXPERT), D_MODEL ** -0.5),
        'moe_w_up': nrm((N_MOE, N_EXPERTS, D_MODEL, D_FF_EXPERT), D_MODEL ** -0.5),
        'moe_w_down': nrm((N_MOE, N_EXPERTS, D_FF_EXPERT, D_MODEL), D_FF_EXPERT ** -0.5),
        'final_g': gain((D_MODEL,)),
    }


def reference(x, c, ctx, c_ctx, ada_w, ada_b, norm_mix_g, norm_ffn_g, w_in, w_out, fnet_w,
              hy_conv_w, hy_conv_b, hy_fw1, hy_fb1, hy_freq1, hy_fw2, hy_fb2, hy_freq2, hy_fw3, hy_bias,
              s5_a_re, s5_a_im, s5_log_dt, s5_b_re, s5_b_im, s5_c_re, s5_c_im, s5_d, s5_glu_w, s5_glu_b,
              ffn_w_gate, ffn_w_up, ffn_w_down, moe_router_w, moe_router_b, moe_w_gate, moe_w_up,
              moe_w_down, final_g):

    def mix(l, p, grid, s0_fwd, s0_bwd):
        n_tok = p.shape[1]
        p_f, p_h, p_s = jnp.split(p, [HYENA_OFF, S5_OFF], axis=-1)
        y_f = fourier_mix(p_f, fnet_w[l])
        k = hyena_filters(n_tok, hy_fw1[l], hy_fb1[l], hy_freq1[l], hy_fw2[l], hy_fb2[l], hy_freq2[l], hy_fw3[l])
        y_h = hyena_mix(p_h, grid, hy_conv_w[l], hy_conv_b[l], k, hy_bias[l])
        h_fwd, h_bwd = s5_trajectories(p_s, s5_a_re[l], s5_a_im[l], s5_log_dt[l], s5_b_re[l], s5_b_im[l],
                                       s0_fwd, s0_bwd)
        y_s = s5_readout(p_s, h_fwd, h_bwd, s5_c_re[l], s5_c_im[l], s5_d[l], s5_glu_w[l], s5_glu_b[l])
        y = jnp.concatenate([y_f, y_h, y_s], axis=-1) @ w_out[l]
        return y, h_fwd, h_bwd

    def channel_mix(l, h):
        i = l // 2
        if l % 2 == 0:
            return swiglu(h, ffn_w_gate[i], ffn_w_up[i], ffn_w_down[i])
        return moe_swiglu(h, moe_router_w[i], moe_router_b[i], moe_w_gate[i], moe_w_up[i], moe_w_down[i])

    ctx_s = ctx
    for l in range(DEPTH):
        last = l == DEPTH - 1
        mod_lat = (jax.nn.silu(c) @ ada_w[l] + ada_b[l])[:, None, :]
        mod_ctx = (jax.nn.silu(c_ctx) @ ada_w[l] + ada_b[l])[None, None, :]
        sh_m, sc_m, g_m, sh_f, sc_f, g_f = jnp.split(mod_lat, N_MOD, axis=-1)
        csh_m, csc_m, cg_m, csh_f, csc_f, cg_f = jnp.split(mod_ctx, N_MOD, axis=-1)

        hc = modulate(rmsnorm(ctx_s, norm_mix_g[l]), csh_m, csc_m)
        if last:
            hf_c, hb_c = s5_trajectories(hc @ w_in[l][:, S5_OFF:], s5_a_re[l], s5_a_im[l], s5_log_dt[l],
                                         s5_b_re[l], s5_b_im[l], None, None)
        else:
            yc, hf_c, hb_c = mix(l, hc @ w_in[l], False, None, None)
            ctx_s = ctx_s + cg_m * yc
            hcf = modulate(rmsnorm(ctx_s, norm_ffn_g[l]), csh_f, csc_f)
            ctx_s = ctx_s + cg_f * channel_mix(l, hcf)

        hx = modulate(rmsnorm(x, norm_mix_g[l]), sh_m, sc_m)
        yx, _, _ = mix(l, hx @ w_in[l], True, hf_c[:, -1], hb_c[:, 0])
        x = x + g_m * yx
        hxf = modulate(rmsnorm(x, norm_ffn_g[l]), sh_f, sc_f)
        x = x + g_f * channel_mix(l, hxf)

    return rmsnorm(x, final_g)
```

```python
import math
from contextlib import ExitStack
import numpy as np
import ml_dtypes
import concourse.bass as bass
import concourse.mybir as mybir
from concourse.bass_utils import run_bass_kernel_spmd

F32 = mybir.dt.float32
BF16 = mybir.dt.bfloat16
I32 = mybir.dt.int32
AF = mybir.ActivationFunctionType
ALU = mybir.AluOpType
AX = mybir.AxisListType

import os
SKIPR = int(os.environ.get('SKIPR', 0))
SKIPC = int(os.environ.get('SKIPC', 0))
NCORES = 8
NS = 2
D = 1024
KC = 8
L_LAT = 4096
L_CTX = 256
DEPTH = 2
D_IN = 1792
HY_OFF = 256
S5_OFF = 1408
DFF = 2816
DFFE = 3584
NE = 8
EPS = 1e-6
TWO_PI = 2.0 * math.pi


class Sched:
    ENG = ("pe", "act", "dve", "pool", "sp")

    def __init__(self, nc):
        self.nc = nc
        self.q = {e: [] for e in self.ENG}
        self.ecount = {e: 0 for e in self.ENG}
        self.seen = {e: {} for e in self.ENG}
        self.res = {}
        self.dcount = {}
        self.semnames = ["c_pe", "c_act", "c_dve", "c_pool"]
        self.keysem = {}
        self.free_sems = []

    def _r(self, k):
        r = self.res.get(k)
        if r is None:
            r = dict(w=[], r=[], pw=[], pr=[], partial=False)
            self.res[k] = r
        return r

    def _deps(self, reads, writes, partial):
        deps = []
        for k in reads:
            deps += self._r(k)["w"]
        joins = []
        for k in writes:
            r = self._r(k)
            if partial and r["partial"] and not r["r"] and r["w"]:
                deps += r["pw"] + r["pr"]
                joins.append(k)
            else:
                deps += r["w"] + r["r"]
        return deps, joins

    def _commit(self, tok, reads, writes, partial, joins):
        for k in reads:
            rr = self._r(k)["r"]
            rr.append(tok)
            if len(rr) > 64:
                mx = {}
                for (s, v) in rr:
                    if mx.get(s, 0) < v:
                        mx[s] = v
                rr[:] = list(mx.items())
        for k in writes:
            r = self._r(k)
            if k in joins:
                r["w"].append(tok)
                if len(r["w"]) > 64:
                    mx = {}
                    for (s, v) in r["w"]:
                        if mx.get(s, 0) < v:
                            mx[s] = v
                    r["w"][:] = list(mx.items())
            else:
                r["pw"], r["pr"] = r["w"], r["r"]
                r["w"], r["r"] = [tok], []
                r["partial"] = partial

    def _waits(self, eng, deps, skip_self=False):
        need = {}
        for (s, v) in deps:
            if skip_self and s == "c_" + eng:
                continue
            if self.seen[eng].get(s, 0) >= v:
                continue
            if need.get(s, 0) < v:
                need[s] = v
        for s, v in need.items():
            self.seen[eng][s] = v
        return list(need.items())

    def op(self, eng, fn, reads=(), writes=(), partial=False):
        deps, joins = self._deps(reads, writes, partial)
        waits = self._waits(eng, deps, skip_self=(eng == "pe"))
        self.ecount[eng] += 1
        tok = ("c_" + eng, self.ecount[eng])
        self.q[eng].append((waits, fn, tok, 1))
        self._commit(tok, reads, writes, partial, joins)
        return tok

    def dma(self, eng, out, in_, reads=(), writes=(), partial=True, **kw):
        assert len(writes) == 1
        k = writes[0]
        deps, joins = self._deps(reads, writes, partial)
        waits = self._waits(eng, deps)
        sname = self.keysem.get(k)
        if sname is None:
            if self.free_sems:
                sname = self.free_sems.pop()
            else:
                sname = "d_%d" % len(self.dcount)
                self.dcount[sname] = 0
                self.semnames.append(sname)
            self.keysem[k] = sname
        self.dcount[sname] += 16
        tok = (sname, self.dcount[sname])

        def fn(e, out=out, in_=in_, kw=kw):
            return e.dma_start(out=out, in_=in_, **kw)

        self.q[eng].append((waits, fn, tok, 16))
        self._commit(tok, reads, writes, partial, joins)
        return tok

    def barrier(self):
        toks = []
        for e in ("pe", "act", "dve", "pool"):
            if self.ecount[e]:
                toks.append(("c_" + e, self.ecount[e]))
        for s, v in self.dcount.items():
            if v:
                toks.append((s, v))
        for e in self.ENG:
            waits = self._waits(e, toks)
            if waits:
                self.q[e].append((waits, None, None, 0))
        self.keysem = {}
        self.free_sems = list(self.dcount.keys())

    def emit(self):
        nc = self.nc
        with ExitStack() as st:
            sems = {}
            for i, s in enumerate(self.semnames):
                sems[s] = st.enter_context(nc.semaphore("s%d" % i))
            block = st.enter_context(nc.Block())
            engs = {"pe": block.tensor, "act": block.scalar, "dve": block.vector,
                    "pool": block.gpsimd, "sp": block.sync}
            for ename, deco in engs.items():
                items = self.q[ename]

                def body(e, items=items):
                    for (waits, fn, tok, inc) in items:
                        for (s, v) in waits:
                            e.wait_ge(sems[s], v)
                        if fn is None:
                            continue
                        ins = fn(e)
                        ins.then_inc(sems[tok[0]], inc)
                deco(body)


def _dsize(dt):
    return {F32: 4, BF16: 2, I32: 4}[dt]


class Arena:
    def __init__(self, nc, st, nbytes):
        self.t = st.enter_context(nc.sbuf_tensor("arena", [128, nbytes // 4], F32))
        self.nbytes = nbytes
        self.off = 0
        self.uid = 0

    def mark(self):
        return self.off

    def release(self, m):
        self.off = m

    def alloc(self, free_shape, dt, parts=128):
        n = int(np.prod(free_shape))
        size = (n * _dsize(dt) + 31) // 32 * 32
        assert self.off + size <= self.nbytes, ("arena overflow", self.off, size)
        a = self.t[0:parts, self.off // 4:(self.off + size) // 4]
        if dt != F32:
            a = a.bitcast(dt)
        a = a[:, 0:n]
        if len(free_shape) == 2:
            a = a.rearrange("p (a b) -> p a b", a=free_shape[0])
        elif len(free_shape) == 3:
            a = a.rearrange("p (a b c) -> p a b c", a=free_shape[0], b=free_shape[1])
        self.off += size
        self.uid += 1
        return a


def _consts():
    c = {}
    for L in (L_LAT, L_CTX):
        t = np.arange(L, dtype=np.int64)
        tf = (np.outer(t, t) % L).astype(np.float64) * (2.0 * np.pi / L)
        c["dftc%d" % L] = (np.cos(tf) / np.sqrt(L)).astype(ml_dtypes.bfloat16)
        c["dfts%d" % L] = (-np.sin(tf) / np.sqrt(L)).astype(ml_dtypes.bfloat16)
        tt = np.linspace(0.0, 1.0, L, dtype=np.float32)[:, None]
        w = (np.float32(2.0 * math.pi / L) * np.arange(L, dtype=np.float32))[:, None]
        bands = np.linspace(1e-4, 15, 16, dtype=np.float32)[None, :]
        z = np.concatenate([tt, np.cos(bands * w), -np.sin(bands * w)], axis=-1).astype(np.float32)
        c["zpos%d" % L] = np.ascontiguousarray(z.T)
        max_decay = math.log(0.01) / 0.3
        min_decay = math.log(0.01) / 1.5
        deltas = np.abs(np.linspace(min_decay, max_decay, 384, dtype=np.float32))
        c["dec%d" % L] = np.exp(-tt.T.astype(np.float32) * deltas[:, None]).astype(np.float32)
    k = np.arange(64)
    a = np.outer(k, k) * (2.0 * np.pi / 64)
    c64 = np.cos(a) / 8.0
    s64 = np.sin(a) / 8.0
    z64 = np.zeros((64, 64))
    c["c64bd"] = np.block([[c64, z64], [z64, c64]]).astype(np.float32)
    c["s64bd"] = np.block([[s64, z64], [z64, s64]]).astype(np.float32)
    ii = np.arange(128) // 16
    c["mask_le"] = (ii[:, None] <= ii[None, :]).astype(np.float32)
    c["mask_ge"] = (ii[:, None] >= ii[None, :]).astype(np.float32)
    c["ident"] = np.eye(128, dtype=np.float32)
    pw = list(range(-7, 9)) + [8 * (2 ** k) for k in range(10)]
    c["s5pw"] = np.tile(np.array(pw, dtype=np.float32)[None, :], (128, 1))
    return c


S5PW = list(range(-7, 9)) + [8 * (2 ** k) for k in range(10)]


def PWI(n):
    return S5PW.index(n)


IN_SPECS = [
    ("x", [NS * L_LAT, D]), ("ctx", [NS * L_CTX, D]), ("cvec", [128, KC, 3]),
    ("ada_w", [DEPTH, D, 6 * D]), ("ada_b", [DEPTH, 6 * D]),
    ("norm_mix_g", [DEPTH, D]), ("norm_ffn_g", [DEPTH, D]),
    ("w_in", [DEPTH, D, D_IN]), ("w_out", [DEPTH, D, D]),
    ("fnet_w", [DEPTH, 4, 64, 64]),
    ("hy_conv_w", [DEPTH, 3, 1152]), ("hy_conv_b", [DEPTH, 1152]),
    ("hy_fw1", [DEPTH, 33, 64]), ("hy_fb1", [DEPTH, 64]), ("hy_freq1", [DEPTH, 64]),
    ("hy_fw2", [DEPTH, 64, 64]), ("hy_fb2", [DEPTH, 64]), ("hy_freq2", [DEPTH, 64]),
    ("hy_fw3", [DEPTH, 64, 1536]), ("hy_bias", [DEPTH, 2, 384]),
    ("s5_a_re", [DEPTH, 2, 24, 64]), ("s5_a_im", [DEPTH, 2, 24, 64]), ("s5_log_dt", [DEPTH, 2, 24]),
    ("s5_b_re", [DEPTH, 2, 24, 64, 16]), ("s5_b_im", [DEPTH, 2, 24, 64, 16]),
    ("s5_c_re", [DEPTH, 2, 24, 16, 64]), ("s5_c_im", [DEPTH, 2, 24, 16, 64]),
    ("s5_d", [DEPTH, 384]), ("s5_glu_w", [DEPTH, 384, 384]), ("s5_glu_b", [DEPTH, 384]),
    ("ffn_w_gate", [1, D, DFF]), ("ffn_w_up", [1, D, DFF]), ("ffn_w_down", [1, DFF, D]),
    ("moe_router_w", [1, D, NE]), ("moe_router_b", [1, NE]),
    ("moe_w_gate", [1, NE, D, DFFE]), ("moe_w_up", [1, NE, D, DFFE]), ("moe_w_down", [1, NE, DFFE, D]),
    ("final_g", [D]),
]
CONST_SPECS = [
    ("dftc4096", [4096, 4096], BF16), ("dfts4096", [4096, 4096], BF16),
    ("dftc256", [256, 256], BF16), ("dfts256", [256, 256], BF16),
    ("zpos4096", [33, 4096], F32), ("zpos256", [33, 256], F32),
    ("dec4096", [384, 4096], F32), ("dec256", [384, 256], F32),
    ("c64bd", [128, 128], F32), ("s64bd", [128, 128], F32),
    ("mask_le", [128, 128], F32), ("mask_ge", [128, 128], F32),
    ("ident", [128, 128], F32), ("s5pw", [128, 26], F32),
]


class Builder:
    def __init__(self, debug=(), stop_after=None, ne_cast=NE, ne_comp=NE):
        self.ne_cast = ne_cast
        self.ne_comp = ne_comp
        self.debug = set(debug)
        self.stop_after = stop_after
        nc = bass.Bass("TRN2", target_bir_lowering=False)
        self.nc = nc
        self.S = Sched(nc)
        self.st = ExitStack()
        self.I = {}
        for name, shape in IN_SPECS:
            self.I[name] = nc.dram_tensor(name, shape, F32, kind="ExternalInput")
        for name, shape, dt in CONST_SPECS:
            self.I[name] = nc.dram_tensor(name, shape, dt, kind="ExternalInput")
        self.out = nc.dram_tensor("out", [NS * L_LAT, D], F32, kind="ExternalOutput")
        self.scr = {}
        self.names = {id(self.out): "out"}
        self.arena = Arena(nc, self.st, 188 * 1024)
        self.ps = [self.st.enter_context(nc.psum_tensor("psb%d" % i, [128, 512], F32))[:] for i in range(8)]
        self.psk = ["ps%d" % i for i in range(8)]
        self.uid = 0
        A = self.arena
        self.ident = A.alloc([128], F32)
        self.identb = A.alloc([128], BF16)
        self.ones = A.alloc([128], F32)
        self.epsb = A.alloc([1], F32)
        self.Am = [A.alloc([KC, 3], F32) for _ in range(2)]
        self.Bm = [A.alloc([KC, 3], F32) for _ in range(2)]
        self.s0 = A.alloc([48, NS], F32)
        self.RHSf = A.alloc([2, 256], BF16)
        self.s0n = A.alloc([48, NS], F32)
        S = self.S
        S.dma("sp", self.ident, self.I["ident"].ap(), writes=["ident"])
        S.op("dve", lambda e: e.tensor_copy(out=self.identb, in_=self.ident), reads=["ident"], writes=["identb"])
        S.op("pool", lambda e: e.memset(self.ones, 1.0), writes=["ones"])
        S.op("pool", lambda e: e.memset(self.epsb, EPS), writes=["epsb"])

    def dscr(self, name, shape, dt):
        kind = "ExternalOutput" if name in self.debug else "Internal"
        t = self.nc.dram_tensor(name, shape, dt, kind=kind)
        self.scr[name] = t
        self.names[id(t)] = name
        return t

    def nm(self, t):
        return self.names.get(id(t), "input")

    def dump(self, name, ap, key, shape, dt):
        if name not in self.debug:
            return
        t = self.dscr(name, shape, dt)
        self.S.dma("sp", t.ap(), ap, reads=[key], writes=["dbg_" + name])

    def key(self, base):
        self.uid += 1
        return "%s#%d" % (base, self.uid)

    def range_reduce_sin(self, eng, out, ang, tmp_i, tmp_f, keys_r, key_w, key_i, key_f, shift=0.0):
        S = self.S
        S.op(eng, lambda e: e.tensor_scalar(out=tmp_i, in0=ang, scalar1=shift, scalar2=1.0 / TWO_PI, op0=ALU.add, op1=ALU.mult),
             reads=keys_r, writes=[key_i])
        S.op(eng, lambda e: e.tensor_copy(out=tmp_f, in_=tmp_i), reads=[key_i], writes=[key_f])
        S.op(eng, lambda e: e.scalar_tensor_tensor(out=tmp_f, in0=tmp_f, scalar=-TWO_PI, in1=ang, op0=ALU.mult, op1=ALU.add),
             reads=[key_f] + list(keys_r), writes=[key_f])
        S.op(eng, lambda e: e.tensor_scalar(out=tmp_f, in0=tmp_f, scalar1=shift, scalar2=math.pi, op0=ALU.add, op1=ALU.min),
             reads=[key_f], writes=[key_f])
        S.op(eng, lambda e: e.tensor_scalar(out=tmp_f, in0=tmp_f, scalar1=-math.pi, scalar2=None, op0=ALU.max),
             reads=[key_f], writes=[key_f])
        S.op("act", lambda e: e.activation(out=out, in_=tmp_f, func=AF.Sin), reads=[key_f], writes=[key_w], partial=True)

    def prep_mod(self, l):
        S, A, I = self.S, self.arena, self.I
        m0 = A.mark()
        cv = A.alloc([KC, 3], F32)
        adab = A.alloc([48], F32)
        gmix = A.alloc([KC], F32)
        gffn = A.alloc([KC], F32)
        modT = A.alloc([48, 3], F32)
        wt = [A.alloc([KC, 512], F32) for _ in range(2)]
        u = self.key("pm")
        S.dma("sp", cv, I["cvec"].ap(), writes=[u + "cv"])
        S.op("act", lambda e: e.activation(out=cv, in_=cv, func=AF.Silu), reads=[u + "cv"], writes=[u + "cv"])
        S.dma("sp", adab, I["ada_b"].ap()[l].rearrange("(f p) -> p f", p=128), writes=[u + "adab"], allow_slow_non_contiguous=True)
        S.dma("sp", gmix, I["norm_mix_g"].ap()[l].rearrange("(f p) -> p f", p=128), writes=[u + "gmix"], allow_slow_non_contiguous=True)
        S.dma("sp", gffn, I["norm_ffn_g"].ap()[l].rearrange("(f p) -> p f", p=128), writes=[u + "gffn"], allow_slow_non_contiguous=True)
        for blk in range(12):
            w = wt[blk % 2]
            wk = u + "wt%d" % (blk % 2)
            S.dma("sp", w, I["ada_w"].ap()[l][:, blk * 512:(blk + 1) * 512].rearrange("(kc p) n -> p kc n", p=128), writes=[wk])
            pst = self.ps[blk % 2]
            pk = self.psk[blk % 2]

            def mmf(e, w=w, pst=pst):
                ins = None
                for m in range(4):
                    for kc in range(KC):
                        ins = e.matmul(pst[:, m * 3:(m + 1) * 3], lhsT=w[:, kc, m * 128:(m + 1) * 128], rhs=cv[:, kc, :],
                                       start=(kc == 0), stop=(kc == KC - 1))
                return ins
            S.op("pe", mmf, reads=[wk, u + "cv"], writes=[pk])
            S.op("dve", lambda e, pst=pst, blk=blk: e.tensor_tensor(
                out=modT[:, blk * 4:(blk + 1) * 4, :], in0=pst[:, 0:12].rearrange("p (m b) -> p m b", m=4),
                in1=adab[:, blk * 4:(blk + 1) * 4].unsqueeze(2).to_broadcast([128, 4, 3]), op=ALU.add),
                reads=[pk, u + "adab"], writes=[u + "modT"], partial=True)
        for wi, (gv, gk, so, ho) in enumerate(((gmix, u + "gmix", 8, 0), (gffn, u + "gffn", 32, 24))):
            S.op("dve", lambda e, wi=wi, so=so: e.tensor_scalar(out=self.Am[wi], in0=modT[:, so:so + 8, :], scalar1=1.0, scalar2=None, op0=ALU.add),
                 reads=[u + "modT"], writes=["Am%d" % wi])
            S.op("dve", lambda e, wi=wi, gv=gv: e.tensor_tensor(out=self.Am[wi], in0=self.Am[wi], in1=gv.unsqueeze(2).to_broadcast([128, 8, 3]), op=ALU.mult),
                 reads=["Am%d" % wi, gk], writes=["Am%d" % wi])
            S.op("dve", lambda e, wi=wi, ho=ho: e.tensor_copy(out=self.Bm[wi], in_=modT[:, ho:ho + 8, :]),
                 reads=[u + "modT"], writes=["Bm%d" % wi])
        md = self.scr.get("modD%d" % l) or self.dscr("modD%d" % l, [3, 6 * D], F32)
        for b in range(3):
            S.dma("sp", md.ap()[b].rearrange("(f p) -> p f", p=128), modT[:, :, b], reads=[u + "modT"], writes=["modD%d" % l],
                  allow_slow_non_contiguous=True)
        self.S.barrier()
        A.release(m0)

    def norm_transpose(self, xt, xk, hT, hk, col0, which, b, hT32=None, single32=False):
        S = self.S
        u = self.key("nt")
        ss = self._nt_ss
        junk = self._nt_junk
        S.op("act", lambda e: e.activation(out=junk, in_=xt, func=AF.Square, accum_out=ss[:, 0:1]), reads=[xk], writes=["nt_ss", "nt_junk"])
        S.op("act", lambda e: e.activation(out=ss[:, 1:2], in_=ss[:, 0:1], func=AF.Sqrt, scale=1.0 / D, bias=self.epsb[:, 0:1]),
             reads=["nt_ss", "epsb"], writes=["nt_ss"])
        S.op("dve", lambda e: e.reciprocal(out=ss[:, 2:3], in_=ss[:, 1:2]), reads=["nt_ss"], writes=["nt_rs"])
        S.op("dve", lambda e: e.tensor_scalar(out=junk, in0=xt, scalar1=ss[:, 2:3], scalar2=None, op0=ALU.mult),
             reads=[xk, "nt_rs"], writes=["nt_junk"])
        for half in range(2):
            pst = self.ps[6 + half]
            pk = self.psk[6 + half]

            def tr(e, half=half, pst=pst):
                ins = None
                for q in range(4):
                    kc = half * 4 + q
                    ins = e.transpose(pst[:, q * 128:(q + 1) * 128], junk[:, kc * 128:(kc + 1) * 128], self.ident)
                return ins
            S.op("pe", tr, reads=["nt_junk", "ident"], writes=[pk])
            for q in range(4):
                kc = half * 4 + q
                S.op("act", lambda e, q=q, kc=kc, pst=pst: e.activation(
                    out=hT[:, kc, col0:col0 + 128], in_=pst[:, q * 128:(q + 1) * 128], func=AF.Identity,
                    scale=self.Am[which][:, kc, b:b + 1], bias=self.Bm[which][:, kc, b:b + 1]),
                    reads=[pk, "Am%d" % which, "Bm%d" % which], writes=[hk], partial=True)
                if hT32 is not None:
                    S.op("act", lambda e, q=q, kc=kc, pst=pst: e.activation(
                        out=(hT32[0][:, kc, :] if single32 else hT32[0][:, kc, col0:col0 + 128]), in_=pst[:, q * 128:(q + 1) * 128],
                        func=AF.Identity, scale=self.Am[which][:, kc, b:b + 1], bias=self.Bm[which][:, kc, b:b + 1]),
                        reads=[pk, "Am%d" % which, "Bm%d" % which], writes=[hT32[1]], partial=True)

    def prep_fnet(self, l):
        S, A, I = self.S, self.arena, self.I
        u = self.key("pf")
        m0 = A.mark()
        cbd = A.alloc([128], F32)
        sbd = A.alloc([128], F32)
        wbd = A.alloc([2, 128], F32)
        S.dma("sp", cbd, I["c64bd"].ap(), writes=[u + "cbd"])
        S.dma("sp", sbd, I["s64bd"].ap(), writes=[u + "sbd"])
        S.op("pool", lambda e: e.memset(wbd, 0.0), writes=[u + "wbd"])
        for h in range(4):
            j, hh = h // 2, h % 2
            S.dma("sp", wbd[hh * 64:(hh + 1) * 64, j, hh * 64:(hh + 1) * 64], I["fnet_w"].ap()[l, h], reads=[], writes=[u + "wbd"])
        for j in range(2):
            pst = self.ps[j]
            pk = self.psk[j]

            def mmf(e, j=j, pst=pst):
                e.matmul(pst[:, 0:128], lhsT=cbd, rhs=wbd[:, j, :], start=True, stop=True)
                return e.matmul(pst[:, 128:256], lhsT=sbd, rhs=wbd[:, j, :], start=True, stop=True)
            S.op("pe", mmf, reads=[u + "cbd", u + "sbd", u + "wbd"], writes=[pk])
            S.op("dve", lambda e, j=j, pst=pst: e.tensor_copy(out=self.RHSf[:, j, :], in_=pst[:, 0:256]), reads=[pk], writes=["RHSf"], partial=True)
        self.S.barrier()
        A.release(m0)

    def phaseA(self, l, Xd, L, grid, bfun, tag):
        S, A, I = self.S, self.arena, self.I
        u = self.key("pA")
        NT = NS * L
        GT = min(512, L)
        TG = GT // 128
        R = 64 if grid else L
        NBk = L // 8
        ABd = self.scr.get("AB" + tag) or self.dscr("AB" + tag, [NT, 512], BF16)
        Hq = self.scr.get("Hq" + tag) or self.dscr("Hq" + tag, [3, NT, 384], BF16)
        Xs = self.scr.get("Xs" + tag) or self.dscr("Xs" + tag, [8, 384, NS, NBk], BF16)
        m0 = A.mark()
        self._nt_ss = A.alloc([4], F32)
        self._nt_junk = A.alloc([D], F32)
        win = A.alloc([KC, 256 + 384], BF16)
        wtap = A.alloc([KC, 3, 1152], BF16)
        cwb = A.alloc([3, 1152], F32)
        cbb = A.alloc([1152], F32)
        S.dma("pool", win[:, :, 0:256], I["w_in"].ap()[l][:, 0:256].rearrange("(kc p) n -> p kc n", p=128), writes=[u + "win"])
        S.dma("pool", win[:, :, 256:640], I["w_in"].ap()[l][:, S5_OFF:D_IN].rearrange("(kc p) n -> p kc n", p=128), writes=[u + "win"])
        S.dma("sp", cwb, I["hy_conv_w"].ap()[l].rearrange("t n -> (t n)").partition_broadcast(128), writes=[u + "cwb"])
        S.dma("sp", cbb, I["hy_conv_b"].ap()[l].partition_broadcast(128), writes=[u + "cbb"])
        m1 = A.mark()
        wf32 = A.alloc([1152], F32)
        for kc in range(KC):
            S.dma("sp", wf32, I["w_in"].ap()[l][kc * 128:(kc + 1) * 128, HY_OFF:S5_OFF], writes=[u + "wf32"], partial=False)
            for tap in range(3):
                S.op("dve" if tap != 1 else "pool", lambda e, kc=kc, tap=tap: e.tensor_tensor(out=wtap[:, kc, tap, :], in0=wf32, in1=cwb[:, tap, :], op=ALU.mult),
                     reads=[u + "wf32", u + "cwb"], writes=[u + "wtap"], partial=True)
        self.dump("dbg_wtap", wtap[:, 0, :, :], u + "wtap", [128, 3, 1152], BF16)
        self.dump("dbg_cwb", cwb, u + "cwb", [128, 3, 1152], F32)
        self.dump("dbg_rhsf", self.RHSf, "RHSf", [128, 2, 256], BF16)
        self.S.barrier()
        A.release(m1)
        xts = [A.alloc([D], F32) for _ in range(2)]
        hT = [[A.alloc([KC, GT], BF16) for _ in range(3)] for _ in range(2)]
        hR = [A.alloc([KC, GT], BF16) for _ in range(3)]
        pfT = A.alloc([2, GT], BF16)
        abt = [A.alloc([512], BF16) for _ in range(2)]
        hqt = [A.alloc([384], BF16) for _ in range(2)]
        psd = [A.alloc([8, GT // 8], BF16) for _ in range(2)]
        for bi in range(2):
            for tp in (0, 2):
                S.op("pool", lambda e, bi=bi, tp=tp: e.memset(hT[bi][tp], 0.0), writes=[u + "hT%d_%d" % (bi, tp)])
        ngroups = NT // GT
        cnt = 0
        for g in range(ngroups):
            bi = g % 2
            s = (g * GT) // L
            b = bfun(s)
            hc, hm, hp = hT[bi][1], hT[bi][0], hT[bi][2]
            hck, hmk, hpk = [u + "hT%d_%d" % (bi, t) for t in (1, 0, 2)]
            for ti in range(TG):
                t = g * TG + ti
                xt = xts[t % 2]
                xk = u + "xt%d" % (t % 2)
                S.dma("sp", xt, Xd.ap()[t * 128:(t + 1) * 128, :], reads=[self.nm(Xd)], writes=[xk], partial=False)
                self.norm_transpose(xt, xk, hc, hck, ti * 128, 0, b)
            hcv = hc.rearrange("p k (r c) -> p (k r) c", c=R)
            hmv = hm.rearrange("p k (r c) -> p (k r) c", c=R)
            hpv = hp.rearrange("p k (r c) -> p (k r) c", c=R)
            S.op("pool", lambda e, hmv=hmv, hcv=hcv: e.tensor_copy(out=hmv[:, :, 1:R], in_=hcv[:, :, 0:R - 1]), reads=[hck], writes=[hmk])
            S.op("dve", lambda e, hpv=hpv, hcv=hcv: e.tensor_copy(out=hpv[:, :, 0:R - 1], in_=hcv[:, :, 1:R]), reads=[hck], writes=[hpk])
            for tp, (src, sk) in enumerate(((hm, hmk), (hc, hck), (hp, hpk))):
                S.op("pool" if tp == 1 else "dve", lambda e, tp=tp, src=src: e.tensor_copy(
                    out=hR[tp].rearrange("p k (t c) -> p (k t) c", c=128),
                    in_=src.rearrange("p k (t c) -> p (k t) c", c=128)[:, :, ::-1]), reads=[sk], writes=[u + "hR%d" % tp])
            for j in range(2):
                pst, pk = self.ps[j], self.psk[j]

                def mmf(e, j=j, pst=pst, hc=hc):
                    ins = None
                    for kc in range(KC):
                        ins = e.matmul(pst[:, 0:GT], lhsT=win[:, kc, j * 128:(j + 1) * 128], rhs=hc[:, kc, :], start=(kc == 0), stop=(kc == KC - 1))
                    return ins
                S.op("pe", mmf, reads=[u + "win", hck], writes=[pk])
                S.op("act", lambda e, j=j, pst=pst: e.copy(out=pfT[:, j, :], in_=pst[:, 0:GT]), reads=[pk], writes=[u + "pfT"], partial=True)
            for ti in range(TG):
                t = g * TG + ti
                pst, pk = self.ps[2 + (t % 2)], self.psk[2 + (t % 2)]

                def mmf(e, ti=ti, pst=pst):
                    e.matmul(pst[:, 0:256], lhsT=pfT[:, 0, ti * 128:(ti + 1) * 128], rhs=self.RHSf[:, 0, :], start=True, stop=True)
                    return e.matmul(pst[:, 256:512], lhsT=pfT[:, 1, ti * 128:(ti + 1) * 128], rhs=self.RHSf[:, 1, :], start=True, stop=True)
                S.op("pe", mmf, reads=[u + "pfT", "RHSf"], writes=[pk])
                ab = abt[t % 2]
                abk = u + "ab%d" % (t % 2)
                S.op("dve", lambda e, ab=ab, pst=pst: e.tensor_copy(out=ab, in_=pst), reads=[pk], writes=[abk])
                S.dma("sp", ABd.ap()[t * 128:(t + 1) * 128, :], ab, reads=[abk], writes=["AB" + tag])
            for ti in range(TG):
                t = g * TG + ti
                for part in range(3):
                    cnt += 1
                    pst, pk = self.ps[4 + (cnt % 2)], self.psk[4 + (cnt % 2)]

                    def mmf(e, ti=ti, part=part, pst=pst, hts=(hm, hc, hp)):
                        ins = None
                        n = 0
                        for tap in range(3):
                            for kc in range(KC):
                                lt = (hR[tap] if part == 1 else hts[tap])[:, kc, ti * 128:(ti + 1) * 128]
                                ins = e.matmul(pst[:, 0:384], lhsT=lt, rhs=wtap[:, kc, tap, part * 384:(part + 1) * 384],
                                               start=(n == 0), stop=(n == 23))
                                n += 1
                        return ins
                    S.op("pe", mmf, reads=[hck, hmk, hpk, u + "wtap", u + "hR0", u + "hR1", u + "hR2"], writes=[pk])
                    hq = hqt[cnt % 2]
                    hqk = u + "hq%d" % (cnt % 2)
                    S.op("dve", lambda e, hq=hq, pst=pst, part=part: e.tensor_tensor(out=hq, in0=pst[:, 0:384], in1=cbb[:, part * 384:(part + 1) * 384], op=ALU.add),
                         reads=[pk, u + "cbb"], writes=[hqk])
                    S.dma("sp", Hq.ap()[part, t * 128:(t + 1) * 128, :], hq, reads=[hqk], writes=["Hq" + tag])
            b0 = ((g * GT) % L) // 8
            for m in range(3):
                cnt += 1
                pst, pk = self.ps[cnt % 2], self.psk[cnt % 2]

                def mmf(e, m=m, pst=pst, hc=hc):
                    ins = None
                    for kc in range(KC):
                        ins = e.matmul(pst[:, 0:GT], lhsT=win[:, kc, 256 + m * 128:256 + (m + 1) * 128], rhs=hc[:, kc, :], start=(kc == 0), stop=(kc == KC - 1))
                    return ins
                S.op("pe", mmf, reads=[u + "win", hck], writes=[pk])
                pd = psd[cnt % 2]
                pdk = u + "psd%d" % (cnt % 2)
                S.op("act", lambda e, pd=pd, pst=pst: e.copy(out=pd, in_=pst[:, 0:GT].rearrange("p (b i) -> p i b", i=8)), reads=[pk], writes=[pdk])
                S.dma("sp", Xs.ap()[:, m * 128:(m + 1) * 128, s, b0:b0 + GT // 8].rearrange("i p b -> p i b"), pd, reads=[pdk], writes=["Xs" + tag])
        self.S.barrier()
        A.release(m0)

    def fnet(self, l, L, tag):
        S, A, I = self.S, self.arena, self.I
        u = self.key("fn")
        NT = NS * L
        NTC = L // 128
        FT = min(512, L)
        ABd = self.scr["AB" + tag]
        Ym = self.scr.get("Ym" + tag) or self.dscr("Ym" + tag, [D, NT], BF16)
        m0 = A.mark()
        ABs = A.alloc([NS * NTC, 512], BF16)
        Cf = A.alloc([NTC, FT], BF16)
        Sf = A.alloc([NTC, FT], BF16)
        yf = [A.alloc([FT], BF16) for _ in range(2)]
        S.dma("sp", ABs, ABd.ap().rearrange("(c p) n -> p c n", p=128), reads=["AB" + tag], writes=[u + "ABs"])
        cnt = 0
        for ft in range(L // FT):
            S.dma("sp", Cf, I["dftc%d" % L].ap()[:, ft * FT:(ft + 1) * FT].rearrange("(c p) n -> p c n", p=128), writes=[u + "Cf"], partial=False)
            S.dma("sp", Sf, I["dfts%d" % L].ap()[:, ft * FT:(ft + 1) * FT].rearrange("(c p) n -> p c n", p=128), writes=[u + "Sf"], partial=False)
            for s in range(NS):
                for j in range(2):
                    cnt += 1
                    pst, pk = self.ps[cnt % 2], self.psk[cnt % 2]

                    def mmf(e, s=s, j=j, pst=pst):
                        ins = None
                        for tc in range(NTC):
                            e.matmul(pst[:, 0:FT], lhsT=ABs[:, s * NTC + tc, j * 256:j * 256 + 128], rhs=Cf[:, tc, :], start=(tc == 0), stop=False)
                            ins = e.matmul(pst[:, 0:FT], lhsT=ABs[:, s * NTC + tc, j * 256 + 128:j * 256 + 256], rhs=Sf[:, tc, :], start=False, stop=(tc == NTC - 1))
                        return ins
                    S.op("pe", mmf, reads=[u + "ABs", u + "Cf", u + "Sf"], writes=[pk])
                    y = yf[cnt % 2]
                    yk = u + "yf%d" % (cnt % 2)
                    S.op("act", lambda e, y=y, pst=pst: e.copy(out=y, in_=pst[:, 0:FT]), reads=[pk], writes=[yk])
                    S.dma("sp", Ym.ap()[j * 128:(j + 1) * 128, s * L + ft * FT:s * L + (ft + 1) * FT], y, reads=[yk], writes=["Ym" + tag])
        self.S.barrier()
        A.release(m0)

    def hyena_filters(self, l, L, tag):
        S, A, I = self.S, self.arena, self.I
        u = self.key("hf")
        Gd = self.scr.get("Gd" + tag) or self.dscr("Gd" + tag, [2, 384, 2 * L], BF16)
        CT = min(512, L)
        m0 = A.mark()
        zT = A.alloc([L], F32)
        H1 = A.alloc([L], F32)
        H2n = A.alloc([L], F32)
        H2r = A.alloc([L], F32)
        fw1 = A.alloc([64], F32)
        fw2 = A.alloc([64], F32)
        fw3 = A.alloc([1536], F32)
        vec = A.alloc([4], F32)
        hb = A.alloc([6], F32)
        dec = A.alloc([3, L], F32)
        G = A.alloc([2 * L], F32)
        Gb = A.alloc([2 * L], BF16)
        tf = A.alloc([CT], F32)
        tf2 = A.alloc([CT], F32)
        ti = A.alloc([CT], I32)
        nrm = A.alloc([4], F32)
        S.dma("sp", zT[0:33, :], I["zpos%d" % L].ap(), writes=[u + "zT"])
        S.dma("sp", fw1[0:33, :], I["hy_fw1"].ap()[l], writes=[u + "fw1"])
        S.dma("sp", fw2[0:64, :], I["hy_fw2"].ap()[l], writes=[u + "fw2"])
        S.dma("sp", fw3[0:64, :], I["hy_fw3"].ap()[l], writes=[u + "fw3"])
        for i, nm in enumerate(("hy_fb1", "hy_freq1", "hy_fb2", "hy_freq2")):
            S.dma("sp", vec[0:64, i:i + 1], I[nm].ap()[l].rearrange("(p o) -> p o", o=1), writes=[u + "vec"])
        S.dma("sp", hb, I["hy_bias"].ap()[l].rearrange("o (c p) -> p (o c)", p=128), writes=[u + "hb"], allow_slow_non_contiguous=True)
        S.dma("sp", dec, I["dec%d" % L].ap().rearrange("(c p) n -> p c n", p=128), writes=[u + "dec"])
        for (src, sk, wgt, wk, kdim, dst, dk, vi) in ((zT, u + "zT", fw1, u + "fw1", 33, H1, u + "H1", 0), (H1, u + "H1", fw2, u + "fw2", 64, H2n, u + "H2n", 2)):
            for ct in range(L // CT):
                pst, pk = self.ps[ct % 2], self.psk[ct % 2]
                S.op("pe", lambda e, pst=pst, src=src, wgt=wgt, kdim=kdim, ct=ct: e.matmul(
                    pst[0:64, 0:CT], lhsT=wgt[0:kdim, 0:64], rhs=src[0:kdim, ct * CT:(ct + 1) * CT], start=True, stop=True),
                    reads=[sk, wk], writes=[pk])
                S.op("dve", lambda e, pst=pst, vi=vi: e.tensor_scalar(out=tf[0:64, :], in0=pst[0:64, 0:CT], scalar1=vec[0:64, vi:vi + 1], scalar2=vec[0:64, vi + 1:vi + 2],
                                                            op0=ALU.add, op1=ALU.mult), reads=[pk, u + "vec"], writes=[u + "tf"])
                self.range_reduce_sin("dve", dst[0:64, ct * CT:(ct + 1) * CT], tf[0:64, :], ti[0:64, :], tf2[0:64, :], [u + "tf"], dk, u + "ti", u + "tf2")
        S.op("dve", lambda e: e.tensor_copy(out=H2r[0:64, :], in_=H2n[0:64, ::-1]), reads=[u + "H2n"], writes=[u + "H2r"])
        cnt = 0
        for o in range(2):
            for cc in range(3):
                if o == 0:
                    segs = [(0, L, 0, "r", 0), (L, 2 * L - 1, 1, "n", 1)]
                else:
                    segs = [(0, L - 1, 1, "r", 0), (L - 1, 2 * L - 1, 0, "n", 0)]
                gk = u + "G"
                S.op("pool", lambda e: e.memset(G[:, 2 * L - 1:2 * L], 0.0), writes=[gk])
                for (a0, a1, dr, wh, po) in segs:
                    col = dr * 768 + o * 384 + cc * 128
                    srcH = H2r if wh == "r" else H2n
                    p = a0
                    while p < a1:
                        n = min(CT, a1 - p)
                        sp = p - a0 + po
                        cnt += 1
                        pst, pk = self.ps[cnt % 2], self.psk[cnt % 2]
                        S.op("pe", lambda e, pst=pst, srcH=srcH, col=col, sp=sp, n=n: e.matmul(
                            pst[:, 0:n], lhsT=fw3[0:64, col:col + 128], rhs=srcH[0:64, sp:sp + n], start=True, stop=True),
                            reads=[u + "H2n", u + "H2r", u + "fw3"], writes=[pk])
                        if wh == "r":
                            dsl = dec[:, cc, L - sp - n:L - sp][:, ::-1]
                        else:
                            dsl = dec[:, cc, sp:sp + n]
                        S.op("dve", lambda e, pst=pst, p=p, n=n, dsl=dsl: e.tensor_tensor(out=G[:, p:p + n], in0=pst[:, 0:n], in1=dsl, op=ALU.mult),
                             reads=[pk, u + "dec"], writes=[gk], partial=True)
                        p += n
                S.op("dve", lambda e: e.tensor_reduce(out=nrm[:, 0:1], in_=G, axis=AX.X, op=ALU.add, apply_absolute_value=True), reads=[gk], writes=[u + "nrm"])
                S.op("dve", lambda e: e.reciprocal(out=nrm[:, 1:2], in_=nrm[:, 0:1]), reads=[u + "nrm"], writes=[u + "nrm"])
                S.op("dve", lambda e: e.tensor_scalar(out=G, in0=G, scalar1=nrm[:, 1:2], scalar2=None, op0=ALU.mult), reads=[gk, u + "nrm"], writes=[gk])
                S.op("dve", lambda e, o=o, cc=cc: e.tensor_scalar(out=G[:, L - 1:L], in0=G[:, L - 1:L], scalar1=hb[:, o * 3 + cc:o * 3 + cc + 1], scalar2=None, op0=ALU.add),
                     reads=[gk, u + "hb"], writes=[gk])
                S.op("act", lambda e: e.copy(out=Gb, in_=G), reads=[gk], writes=[u + "Gb"])
                S.dma("sp", Gd.ap()[o, cc * 128:(cc + 1) * 128, :], Gb, reads=[u + "Gb"], writes=["Gd" + tag])
        self.S.barrier()
        A.release(m0)

    def hyena(self, l, L, tag):
        S, A, I = self.S, self.arena, self.I
        u = self.key("hy")
        NB = L // 128
        NC = NS * NB
        W = 128 * (2 * NB - 1)
        NT = NS * L
        Hq = self.scr["Hq" + tag]
        Gd = self.scr["Gd" + tag]
        Ym = self.scr.get("Ym" + tag) or self.dscr("Ym" + tag, [D, NT], BF16)
        HC = 192
        CG = max(1, min(8, 512 // NC))
        m0 = A.mark()
        Z2 = A.alloc([NC, 384], BF16)
        Ub = A.alloc([NC, HC], BF16)
        Xb = A.alloc([NC, HC], BF16)
        Z1 = A.alloc([NC, HC], BF16)
        Rb = [A.alloc([W], BF16) for _ in range(2)]
        yT = [A.alloc([3, 128], BF16) for _ in range(2)]
        rc = 0
        bc = 0
        dl = [0] + [d for d in range(-(NB - 1), NB) if d != 0]
        for half in range(2):
            h0 = half * HC
            for o in range(2):
                if o == 0:
                    S.dma("sp", Ub, Hq.ap()[0, :, h0:h0 + HC].rearrange("(c p) n -> p c n", p=128), reads=["Hq" + tag], writes=[u + "Ub"], partial=False)
                    S.dma("sp", Xb, Hq.ap()[1, :, h0:h0 + HC].rearrange("(c p) n -> p c n", p=128), reads=["Hq" + tag], writes=[u + "Xb"], partial=False)
                    src, srck, dst, dstk = Ub, u + "Ub", Z1, u + "Z1"
                else:
                    S.dma("sp", Xb, Hq.ap()[2, :, h0:h0 + HC].rearrange("(c p) n -> p c n", p=128), reads=["Hq" + tag], writes=[u + "Xb"], partial=False)
                    src, srck, dst, dstk = Z1, u + "Z1", Z2, u + "Z2"
                srcv = src.rearrange("p (s b) n -> p s b n", s=NS)
                for c8 in range(HC // CG):
                    bc += 1
                    pst, pk = self.ps[bc % 4], self.psk[bc % 4]
                    for ci in range(CG):
                        cl = c8 * CG + ci
                        c = h0 + cl
                        rc += 1
                        Rt = Rb[rc % 2]
                        rk = u + "R%d" % (rc % 2)
                        S.dma("sp", Rt, bass.AP(Gd, (o * 384 + c) * 2 * L, [[1, 128], [1, W]]), reads=["Gd" + tag], writes=[rk], partial=False)
                        pv = pst[:, ci * NC:(ci + 1) * NC].rearrange("p (s b) -> p s b", s=NS)

                        def mmf(e, Rt=Rt, pv=pv, srcv=srcv, cl=cl, o=o):
                            ins = None
                            for k, d in enumerate(dl):
                                i0 = max(0, d)
                                i1 = min(NB - 1, NB - 1 + d)
                                nb = i1 - i0 + 1
                                j0 = i0 - d
                                xd = 128 * (NB - 1 - d) if o == 0 else 128 * (d + NB - 1)
                                ins = e.matmul(pv[:, :, i0:i0 + nb], lhsT=Rt[:, xd:xd + 128], rhs=srcv[:, :, j0:j0 + nb, cl],
                                               start=(k == 0), stop=(k == len(dl) - 1))
                            return ins
                        S.op("pe", mmf, reads=[rk, srck], writes=[pk], partial=True)
                    cl0 = c8 * CG
                    oc0 = cl0 if o == 0 else h0 + cl0
                    S.op("dve", lambda e, pst=pst, dst=dst, cl0=cl0, oc0=oc0: e.tensor_tensor(
                        out=dst[:, :, oc0:oc0 + CG], in0=pst[:, 0:CG * NC].rearrange("p (c n) -> p n c", c=CG),
                        in1=Xb[:, :, cl0:cl0 + CG], op=ALU.mult), reads=[pk, u + "Xb"], writes=[dstk], partial=True)
        for col in range(NC):
            pst, pk = self.ps[4 + col % 2], self.psk[4 + col % 2]
            pb = pst.bitcast(BF16)

            def trf(e, col=col, pb=pb):
                ins = None
                for cc in range(3):
                    ins = e.transpose(pb[:, cc * 128:(cc + 1) * 128], Z2[:, col, cc * 128:(cc + 1) * 128], self.identb)
                return ins
            S.op("pe", trf, reads=[u + "Z2", "identb"], writes=[pk])
            y = yT[col % 2]
            yk = u + "yT%d" % (col % 2)
            S.op("act", lambda e, y=y, pb=pb: e.copy(out=y, in_=pb[:, 0:384].rearrange("p (c t) -> p c t", c=3)), reads=[pk], writes=[yk])
            S.dma("sp", Ym.ap()[256:640, col * 128:(col + 1) * 128].rearrange("(c p) t -> p c t", p=128), y, reads=[yk], writes=["Ym" + tag])
        self.S.barrier()
        A.release(m0)

    def s5_prep(self, l):
        S, A, I = self.S, self.arena, self.I
        u = self.key("sp")
        P = {}
        P["pr"] = A.alloc([24, 26], F32)
        P["pi"] = A.alloc([24, 26], F32)
        P["npi"] = A.alloc([24, 26], F32)
        P["WBT"] = A.alloc([48, 128], BF16)
        P["Mpad"] = A.alloc([24, 8, 32], BF16)
        P["Gpad"] = A.alloc([48, 8, 32], BF16)
        P["gluw"] = A.alloc([4, 384], BF16)
        P["glub"] = A.alloc([4], F32)
        P["u"] = u
        m0 = A.mark()
        lr = A.alloc([24], F32); li = A.alloc([24], F32); dt = A.alloc([24], F32)
        Bre = A.alloc([24, 16], F32); Bim = A.alloc([24, 16], F32)
        Cre = A.alloc([24, 16], F32); Cim = A.alloc([24, 16], F32)
        pwc = A.alloc([26], F32)
        EX = A.alloc([24, 26], F32); ANG = A.alloc([24, 26], F32)
        tI = A.alloc([24, 26], I32); tF = A.alloc([24, 26], F32)
        t1 = A.alloc([24], F32); t2 = A.alloc([24], F32); cr = A.alloc([24], F32); ci = A.alloc([24], F32)
        bbr = A.alloc([24, 16], F32); bbi = A.alloc([24, 16], F32)
        T1 = A.alloc([12, 8, 16], F32); T2 = A.alloc([12, 8, 16], F32)
        Wn = A.alloc([48, 128], F32)
        Pp = A.alloc([48, 128], BF16)
        Qq = A.alloc([48, 128], BF16)
        mle = A.alloc([128], F32); mge = A.alloc([128], F32)
        dv = A.alloc([24], F32)
        Mf = A.alloc([128], F32)
        S.dma("sp", lr, I["s5_a_re"].ap()[l].rearrange("d (q m) s -> (m s) (d q)", m=2), writes=[u + "lr"], allow_slow_non_contiguous=True)
        S.dma("sp", li, I["s5_a_im"].ap()[l].rearrange("d (q m) s -> (m s) (d q)", m=2), writes=[u + "li"], allow_slow_non_contiguous=True)
        for m in range(2):
            S.dma("sp", dt[m * 64:(m + 1) * 64, :], I["s5_log_dt"].ap()[l].rearrange("d (q m) -> m (d q)", m=2)[m].partition_broadcast(64),
                  writes=[u + "dt"], allow_slow_non_contiguous=True)
        Csrc = A.alloc([6, 128], F32)
        CT_ = A.alloc([768], F32)
        for (dst, nm, kk) in ((Cre, "s5_c_re", u + "Cre"), (Cim, "s5_c_im", u + "Cim")):
            for hh in range(2):
                S.dma("sp", Csrc[:, :, hh * 64:(hh + 1) * 64], I[nm].ap()[l].rearrange("d g o s -> (d g o) s").rearrange("(k p) s -> p k s", p=128),
                      writes=[u + "Csrc"])
            for k in range(6):
                pst, pk = self.ps[k % 2], self.psk[k % 2]
                S.op("pe", lambda e, k=k, pst=pst: e.transpose(pst[:, 0:128], Csrc[:, k, :], self.ident), reads=[u + "Csrc", "ident"], writes=[pk])
                S.op("act", lambda e, k=k, pst=pst: e.copy(out=CT_[:, k * 128:(k + 1) * 128], in_=pst[:, 0:128]), reads=[pk], writes=[u + "CT"], partial=True)
            CTv = CT_.rearrange("p (d q m o) -> p d q m o", d=2, q=12, m=2)
            for m in range(2):
                S.op("dve", lambda e, m=m, dst=dst, CTv=CTv: e.tensor_copy(out=dst[m * 64:(m + 1) * 64].rearrange("p (d q) o -> p d q o", d=2), in_=CTv[m * 64:(m + 1) * 64, :, :, m, :]),
                     reads=[u + "CT"], writes=[kk], partial=True)
        S.dma("sp", Bre, I["s5_b_re"].ap()[l].rearrange("d (q m) s c -> (m s) (d q) c", m=2), writes=[u + "Bre"])
        S.dma("sp", Bim, I["s5_b_im"].ap()[l].rearrange("d (q m) s c -> (m s) (d q) c", m=2), writes=[u + "Bim"])
        S.dma("sp", pwc, I["s5pw"].ap(), writes=[u + "pwc"])
        S.dma("sp", mle, I["mask_le"].ap(), writes=[u + "mle"])
        S.dma("sp", mge, I["mask_ge"].ap(), writes=[u + "mge"])
        for i in range(8):
            S.dma("sp", dv[i * 16:(i + 1) * 16, :], I["s5_d"].ap()[l].rearrange("(g c) -> c g", c=16), writes=[u + "dv"], allow_slow_non_contiguous=True)
        S.dma("pool", P["gluw"][0:96], I["s5_glu_w"].ap()[l].rearrange("(c p) n -> p c n", p=96), writes=[u + "gluw"])
        S.dma("sp", P["glub"][0:96], I["s5_glu_b"].ap()[l].rearrange("(c p) -> p c", p=96), writes=[u + "glub"], allow_slow_non_contiguous=True)
        V = lambda e: e
        dve = lambda fn, r, w, **k: S.op("dve", fn, reads=r, writes=w, **k)
        dve(lambda e: e.tensor_copy(out=dt, in_=dt), [u + "dt"], [u + "dt"])
        S.op("act", lambda e: e.activation(out=dt, in_=dt, func=AF.Exp), reads=[u + "dt"], writes=[u + "dt"])
        dve(lambda e: e.tensor_tensor(out=t1, in0=lr, in1=dt, op=ALU.mult), [u + "lr", u + "dt"], [u + "t1"])
        dve(lambda e: e.tensor_tensor(out=t2, in0=li, in1=dt, op=ALU.mult), [u + "li", u + "dt"], [u + "t2"])
        bc3 = lambda a: a.unsqueeze(2).to_broadcast([128, 24, 26])
        pw3 = pwc.unsqueeze(1).to_broadcast([128, 24, 26])
        dve(lambda e: e.tensor_tensor(out=EX, in0=bc3(t1), in1=pw3, op=ALU.mult), [u + "t1", u + "pwc"], [u + "EX"])
        dve(lambda e: e.tensor_tensor(out=ANG, in0=bc3(t2), in1=pw3, op=ALU.mult), [u + "t2", u + "pwc"], [u + "ANG"])
        S.op("act", lambda e: e.activation(out=EX, in_=EX, func=AF.Exp), reads=[u + "EX"], writes=[u + "EX"])
        self.range_reduce_sin("dve", P["pi"], ANG, tI, tF, [u + "ANG"], u + "pi", u + "tI", u + "tF")
        self.range_reduce_sin("dve", P["pr"], ANG, tI, tF, [u + "ANG"], u + "pr", u + "tI", u + "tF", shift=math.pi / 2)
        dve(lambda e: e.tensor_tensor(out=P["pi"], in0=P["pi"], in1=EX, op=ALU.mult), [u + "pi", u + "EX"], [u + "pi"])
        dve(lambda e: e.tensor_tensor(out=P["pr"], in0=P["pr"], in1=EX, op=ALU.mult), [u + "pr", u + "EX"], [u + "pr"])
        dve(lambda e: e.tensor_scalar(out=P["npi"], in0=P["pi"], scalar1=-1.0, scalar2=None, op0=ALU.mult), [u + "pi"], [u + "npi"])
        pk_ = [u + "pr", u + "pi", u + "npi"]
        i1 = PWI(1)
        ar = P["pr"][:, :, i1]; ai = P["pi"][:, :, i1]
        dve(lambda e: e.tensor_tensor(out=t1, in0=lr, in1=lr, op=ALU.mult), [u + "lr", u + "EX"], [u + "t1"])
        dve(lambda e: e.tensor_tensor(out=t2, in0=li, in1=li, op=ALU.mult), [u + "li", u + "ANG"], [u + "t2"])
        dve(lambda e: e.tensor_tensor(out=t1, in0=t1, in1=t2, op=ALU.add), [u + "t1", u + "t2"], [u + "t1"])
        dve(lambda e: e.reciprocal(out=t1, in_=t1), [u + "t1"], [u + "t1"])
        dve(lambda e: e.tensor_scalar(out=t2, in0=ar, scalar1=-1.0, scalar2=None, op0=ALU.add), pk_, [u + "t2"])
        dve(lambda e: e.tensor_tensor(out=cr, in0=t2, in1=lr, op=ALU.mult), [u + "t2", u + "lr"], [u + "cr"])
        dve(lambda e: e.tensor_tensor(out=ci, in0=ai, in1=li, op=ALU.mult), pk_ + [u + "li"], [u + "ci"])
        dve(lambda e: e.tensor_tensor(out=cr, in0=cr, in1=ci, op=ALU.add), [u + "cr", u + "ci"], [u + "cr"])
        dve(lambda e: e.tensor_tensor(out=cr, in0=cr, in1=t1, op=ALU.mult), [u + "cr", u + "t1"], [u + "cr"])
        dve(lambda e: e.tensor_tensor(out=ci, in0=ai, in1=lr, op=ALU.mult), pk_ + [u + "lr", u + "cr"], [u + "ci"])
        dve(lambda e: e.tensor_tensor(out=t2, in0=t2, in1=li, op=ALU.mult), [u + "t2", u + "li"], [u + "t2"])
        dve(lambda e: e.tensor_tensor(out=ci, in0=ci, in1=t2, op=ALU.subtract), [u + "ci", u + "t2"], [u + "ci"])
        dve(lambda e: e.tensor_tensor(out=ci, in0=ci, in1=t1, op=ALU.mult), [u + "ci", u + "t1"], [u + "ci"])
        b16 = lambda a: a.unsqueeze(2).to_broadcast([128, 24, 16])
        dve(lambda e: e.tensor_scalar(out=Cim, in0=Cim, scalar1=-1.0, scalar2=None, op0=ALU.mult), [u + "Cim"], [u + "Cim"])

        def cmul(outr, outi, xr_, xi_, yr_, yi_, rk, wkr, wki, ta, tb, shape_sel=None, neg_im=False):
            dve(lambda e: e.tensor_tensor(out=ta, in0=xr_, in1=yr_, op=ALU.mult), rk, [u + "ta"])
            dve(lambda e: e.tensor_tensor(out=tb, in0=xi_, in1=yi_, op=ALU.mult), rk, [u + "tb"])
            dve(lambda e: e.tensor_tensor(out=outr, in0=ta, in1=tb, op=(ALU.add if neg_im else ALU.subtract)), [u + "ta", u + "tb"], [wkr], partial=True)
            dve(lambda e: e.tensor_tensor(out=ta, in0=xr_, in1=yi_, op=ALU.mult), rk + [wkr], [u + "ta"])
            dve(lambda e: e.tensor_tensor(out=tb, in0=xi_, in1=yr_, op=ALU.mult), rk + [wkr], [u + "tb"])
            dve(lambda e: e.tensor_tensor(out=outi, in0=ta, in1=tb, op=(ALU.subtract if neg_im else ALU.add)), [u + "ta", u + "tb"], [wki], partial=True)
        ta24 = T1.rearrange("p a b c -> p (a b c)")[:, 0:384].rearrange("p (a c) -> p a c", a=24)
        tb24 = T2.rearrange("p a b c -> p (a b c)")[:, 0:384].rearrange("p (a c) -> p a c", a=24)
        cmul(bbr, bbi, b16(cr), b16(ci), Bre, Bim, [u + "cr", u + "ci", u + "Bre", u + "Bim"], u + "bbr", u + "bbi", ta24, tb24)
        def pslice(arr, d, n0, step):
            i0 = PWI(n0)
            if step > 0:
                sl = arr[:, d * 12:(d + 1) * 12, i0:i0 + 8]
            else:
                sl = arr[:, d * 12:(d + 1) * 12, i0 - 7:i0 + 1][:, :, ::-1]
            return sl.unsqueeze(3).to_broadcast([128, 12, 8, 16])
        v16 = lambda a, d: a[:, d * 12:(d + 1) * 12, :].unsqueeze(2).to_broadcast([128, 12, 8, 16])
        Wnv = Wn.rearrange("p (d q r) (i c) -> p d q r i c", d=2, q=12, r=2, i=8)
        Ppv = Pp.rearrange("p (d q r) (i c) -> p d q r i c", d=2, q=12, r=2, i=8)
        Qqv = Qq.rearrange("p (d q r) (i c) -> p d q r i c", d=2, q=12, r=2, i=8)
        bk = [u + "bbr", u + "bbi"] + pk_
        ck = [u + "Cre", u + "Cim"] + pk_
        for d in range(2):
            n0, stp = ((7, -1), (0, 1))[d]
            cmul(Wnv[:, d, :, 0], Wnv[:, d, :, 1], pslice(P["pr"], d, n0, stp), pslice(P["pi"], d, n0, stp), v16(bbr, d), v16(bbi, d),
                 bk, u + "Wn", u + "Wn", T1, T2)
            n0, stp = ((0, -1), (0, 1))[d]
            cmul(Ppv[:, d, :, 0], Ppv[:, d, :, 1], pslice(P["pr"], d, n0, stp), pslice(P["pi"], d, n0, stp), v16(bbr, d), v16(bbi, d),
                 bk, u + "Pp", u + "Pp", T1, T2)
            n0, stp = ((0, 1), (0, -1))[d]
            cmul(Qqv[:, d, :, 0], Qqv[:, d, :, 1], pslice(P["pr"], d, n0, stp), pslice(P["pi"], d, n0, stp), v16(Cre, d), v16(Cim, d),
                 ck, u + "Qq", u + "Qq", T1, T2, neg_im=True)
        S.op("pool", lambda e: e.memset(P["Gpad"], 0.0), writes=[u + "Gpad"])
        Gv = P["Gpad"].rearrange("p (d q r) j n -> p d q r j n", d=2, q=12, r=2)
        for d in range(2):
            n0, stp = ((1, 1), (8, -1))[d]
            for m in range(2):
                hs = slice(m * 64, (m + 1) * 64)
                h = lambda a: a[hs]
                cmul(Gv[hs, d, :, 0, :, m * 16:(m + 1) * 16], Gv[hs, d, :, 1, :, m * 16:(m + 1) * 16],
                     h(pslice(P["pr"], d, n0, stp)), h(pslice(P["pi"], d, n0, stp)), h(v16(Cre, d)), h(v16(Cim, d)),
                     ck + [u + "Gpad"], u + "Gpad", u + "Gpad", T1[hs], T2[hs], neg_im=True)
        for k in range(48):
            pst, pk = self.ps[k % 2], self.psk[k % 2]
            S.op("pe", lambda e, k=k, pst=pst: e.transpose(pst[:, 0:128], Wn[:, k, :], self.ident), reads=[u + "Wn", "ident"], writes=[pk])
            S.op("act", lambda e, k=k, pst=pst: e.copy(out=P["WBT"][:, k, :], in_=pst[:, 0:128]), reads=[pk], writes=[u + "WBT"], partial=True)
        S.op("pool", lambda e: e.memset(P["Mpad"], 0.0), writes=[u + "Mpad"])
        for g in range(24):
            q, m = g // 2, g % 2
            hs = slice(m * 64, (m + 1) * 64)
            psf, pkf = self.ps[2 + (g % 2) * 2], self.psk[2 + (g % 2) * 2]
            psb, pkb = self.ps[3 + (g % 2) * 2], self.psk[3 + (g % 2) * 2]
            for d, pst, pk in ((0, psf, pkf), (1, psb, pkb)):
                kre = (d * 12 + q) * 2
                def mmf(e, pst=pst, kre=kre, hs=hs):
                    e.matmul(pst[:, 0:128], lhsT=Pp[hs, kre, :], rhs=Qq[hs, kre, :], start=True, stop=False)
                    return e.matmul(pst[:, 0:128], lhsT=Pp[hs, kre + 1, :], rhs=Qq[hs, kre + 1, :], start=False, stop=True)
                S.op("pe", mmf, reads=[u + "Pp", u + "Qq"], writes=[pk])
            dve(lambda e, psf=psf: e.tensor_tensor(out=Mf, in0=psf[:, 0:128], in1=mle, op=ALU.mult), [pkf, u + "mle"], [u + "Mf"])
            dve(lambda e, psb=psb: e.tensor_tensor(out=T1.rearrange("p a b c -> p (a b c)")[:, 0:128], in0=psb[:, 0:128], in1=mge, op=ALU.mult), [pkb, u + "mge"], [u + "ta"])
            dve(lambda e: e.tensor_tensor(out=Mf, in0=Mf, in1=T1.rearrange("p a b c -> p (a b c)")[:, 0:128], op=ALU.add), [u + "Mf", u + "ta"], [u + "Mf"])
            dve(lambda e, g=g: e.scalar_tensor_tensor(out=Mf, in0=self.ident, scalar=dv[:, g:g + 1], in1=Mf, op0=ALU.mult, op1=ALU.add), [u + "Mf", u + "dv", "ident"], [u + "Mf"])
            dve(lambda e, g=g, m=m: e.tensor_copy(out=P["Mpad"][:, g, :, m * 16:(m + 1) * 16], in_=Mf.rearrange("p (j o) -> p j o", j=8)), [u + "Mf"], [u + "Mpad"], partial=True)
        self.S.barrier()
        A.release(m0)
        return P

    def s5_main(self, l, L, tag, P, use_s0, readout):
        S, A, I = self.S, self.arena, self.I
        u = self.key("s5")
        pu = P["u"]
        NBk = L // 8
        E = NBk + 1
        NT = NS * L
        Xs = self.scr["Xs" + tag]
        Ym = self.scr.get("Ym" + tag) or self.dscr("Ym" + tag, [D, NT], BF16)
        nlev = 0
        while (1 << nlev) < E:
            nlev += 1
        if nlev % 2:
            nlev += 1
        m0 = A.mark()
        eA = A.alloc([4, NS, E], F32)
        eB = A.alloc([4, NS, E], F32)
        ebf = A.alloc([4, NS, E], BF16)
        Xg = [A.alloc([NS * NBk], BF16) for _ in range(2)]
        if readout:
            YS = A.alloc([NS * L], F32)
            PC = min(2048, NS * L)
            tq = A.alloc([PC], F32)
            ygp = [A.alloc([PC], BF16) for _ in range(2)]
            Ygd = self.scr.get("Yg" + tag) or self.dscr("Yg" + tag, [384, NT], BF16)
        pk_ = [pu + "pr", pu + "pi", pu + "npi"]
        bc = 0
        for q in range(12):
            for m in range(2):
                g = 2 * q + m
                for i in range(8):
                    S.dma("sp", Xg[m][i * 16:(i + 1) * 16, :], Xs.ap()[i, g * 16:(g + 1) * 16].rearrange("c s b -> c (s b)"),
                          reads=["Xs" + tag], writes=[u + "Xg%d" % m])
            for d in range(2):
                col = 0 if d == 0 else NBk
                for ri in range(2):
                    k = d * 2 + ri
                    if use_s0:
                        S.op("act", lambda e, k=k, col=col, d=d, ri=ri, q=q: e.copy(out=eA[:, k, :, col], in_=self.s0[:, (d * 12 + q) * 2 + ri, :]),
                             reads=["s0"], writes=[u + "eA%d" % d], partial=True)
                    else:
                        S.op("pool", lambda e, k=k, col=col: e.memset(eA[:, k, :, col], 0.0), writes=[u + "eA%d" % d], partial=True)
            for d in range(2):
                off = 1 if d == 0 else 0
                for ri in range(2):
                    for sq in range(NS):
                        bc += 1
                        pst, pk = self.ps[bc % 2], self.psk[bc % 2]
                        kk = (d * 12 + q) * 2 + ri

                        def mmf(e, pst=pst, kk=kk, sq=sq):
                            e.matmul(pst[0:64, 0:NBk], lhsT=P["WBT"][:, kk, 0:64], rhs=Xg[0][:, sq * NBk:(sq + 1) * NBk], start=True, stop=True)
                            return e.matmul(pst[64:128, 0:NBk], lhsT=P["WBT"][:, kk, 64:128], rhs=Xg[1][:, sq * NBk:(sq + 1) * NBk], start=True, stop=True)
                        S.op("pe", mmf, reads=[pu + "WBT", u + "Xg0", u + "Xg1"], writes=[pk])
                        S.op("act", lambda e, pst=pst, d=d, ri=ri, sq=sq, off=off: e.copy(out=eA[:, d * 2 + ri, sq, off:off + NBk], in_=pst[:, 0:NBk]),
                             reads=[pk], writes=[u + "eA%d" % d], partial=True)
            for d, eng in ((0, "dve"), (1, "dve")):
                cur, nxt = eA, eB
                ck_, nk_ = u + "eA%d" % d, u + "eB%d" % d
                for lev in range(nlev):
                    dist = 1 << lev
                    ix = PWI(8 * dist)
                    ar = P["pr"][:, d * 12 + q, ix:ix + 1]
                    ai = P["pi"][:, d * 12 + q, ix:ix + 1]
                    nai = P["npi"][:, d * 12 + q, ix:ix + 1]
                    cr_, ci_ = cur[:, d * 2 + 0], cur[:, d * 2 + 1]
                    nr_, ni_ = nxt[:, d * 2 + 0], nxt[:, d * 2 + 1]
                    if dist < E:
                        n = E - dist
                        if d == 0:
                            srcs, dsts = slice(0, n), slice(dist, E)
                            keep = slice(0, dist)
                        else:
                            srcs, dsts = slice(dist, E), slice(0, n)
                            keep = slice(n, E)
                        S.op(eng, lambda e, cr_=cr_, nr_=nr_, ar=ar, srcs=srcs, dsts=dsts: e.scalar_tensor_tensor(
                            out=nr_[:, :, dsts], in0=cr_[:, :, srcs], scalar=ar, in1=cr_[:, :, dsts], op0=ALU.mult, op1=ALU.add),
                            reads=[ck_] + pk_, writes=[nk_], partial=True)
                        S.op(eng, lambda e, ci_=ci_, nr_=nr_, nai=nai, srcs=srcs, dsts=dsts: e.scalar_tensor_tensor(
                            out=nr_[:, :, dsts], in0=ci_[:, :, srcs], scalar=nai, in1=nr_[:, :, dsts], op0=ALU.mult, op1=ALU.add),
                            reads=[ck_, nk_] + pk_, writes=[nk_], partial=True)
                        S.op(eng, lambda e, ci_=ci_, ni_=ni_, ar=ar, srcs=srcs, dsts=dsts: e.scalar_tensor_tensor(
                            out=ni_[:, :, dsts], in0=ci_[:, :, srcs], scalar=ar, in1=ci_[:, :, dsts], op0=ALU.mult, op1=ALU.add),
                            reads=[ck_] + pk_, writes=[nk_], partial=True)
                        S.op(eng, lambda e, cr_=cr_, ni_=ni_, ai=ai, srcs=srcs, dsts=dsts: e.scalar_tensor_tensor(
                            out=ni_[:, :, dsts], in0=cr_[:, :, srcs], scalar=ai, in1=ni_[:, :, dsts], op0=ALU.mult, op1=ALU.add),
                            reads=[ck_, nk_] + pk_, writes=[nk_], partial=True)
                    else:
                        keep = slice(0, E)
                    S.op("act", lambda e, cur=cur, nxt=nxt, d=d, keep=keep: e.copy(out=nxt[:, d * 2:d * 2 + 2, :, keep], in_=cur[:, d * 2:d * 2 + 2, :, keep]),
                         reads=[ck_], writes=[nk_], partial=True)
                    cur, nxt = nxt, cur
                    ck_, nk_ = nk_, ck_
            if not readout or True:
                for d in range(2):
                    col = NBk if d == 0 else 0
                    for ri in range(2):
                        S.op("act", lambda e, d=d, ri=ri, col=col, q=q: e.copy(out=self.s0n[:, (d * 12 + q) * 2 + ri, :], in_=eA[:, d * 2 + ri, :, col]),
                             reads=[u + "eA%d" % d], writes=["s0n"], partial=True)
            if not readout:
                continue
            S.op("act", lambda e: e.copy(out=ebf, in_=eA), reads=[u + "eA0", u + "eA1"], writes=[u + "ebf"])
            qq = q % 3
            gc = q // 3
            YSv = YS.rearrange("p (s b j) -> p s b j", s=NS, j=8)
            for j in range(8):
                for sq in range(NS):
                    bc += 1
                    pst, pk = self.ps[2 + bc % 4], self.psk[2 + bc % 4]

                    def mmf(e, pst=pst, j=j, sq=sq, q=q, qq=qq):
                        o = pst[qq * 32:(qq + 1) * 32, 0:NBk]
                        e.matmul(o, lhsT=P["Mpad"][:, 2 * q, j, :], rhs=Xg[0][:, sq * NBk:(sq + 1) * NBk], start=True, stop=False)
                        e.matmul(o, lhsT=P["Mpad"][:, 2 * q + 1, j, :], rhs=Xg[1][:, sq * NBk:(sq + 1) * NBk], start=False, stop=False)
                        ins = None
                        for d in range(2):
                            c0 = 0 if d == 0 else 1
                            for ri in range(2):
                                ins = e.matmul(o, lhsT=P["Gpad"][:, (d * 12 + q) * 2 + ri, j, :], rhs=ebf[:, d * 2 + ri, sq, c0:c0 + NBk],
                                               start=False, stop=(d == 1 and ri == 1))
                        return ins
                    S.op("pe", mmf, reads=[pu + "Mpad", pu + "Gpad", u + "Xg0", u + "Xg1", u + "ebf"], writes=[pk])
                    S.op("dve", lambda e, pst=pst, j=j, sq=sq, qq=qq: e.tensor_copy(out=YSv[qq * 32:(qq + 1) * 32, sq, :, j], in_=pst[qq * 32:(qq + 1) * 32, 0:NBk]),
                         reads=[pk], writes=[u + "YS"], partial=True)
            if qq == 2:
                for pc in range(NS * L // PC):
                    cs = slice(pc * PC, (pc + 1) * PC)
                    yp, ypk = ygp[pc % 2], u + "ygp%d" % (pc % 2)
                    S.op("act", lambda e, cs=cs: e.activation(out=tq[0:96], in_=YS[0:96, cs], func=AF.Square), reads=[u + "YS"], writes=[u + "tq"])
                    S.op("dve", lambda e: e.tensor_scalar(out=tq[0:96], in0=tq[0:96], scalar1=0.044715, scalar2=1.0, op0=ALU.mult, op1=ALU.add), reads=[u + "tq"], writes=[u + "tq"])
                    S.op("dve", lambda e, cs=cs: e.tensor_tensor(out=tq[0:96], in0=tq[0:96], in1=YS[0:96, cs], op=ALU.mult), reads=[u + "tq", u + "YS"], writes=[u + "tq"])
                    S.op("act", lambda e: e.activation(out=tq[0:96], in_=tq[0:96], func=AF.Sigmoid, scale=1.5957691216057308), reads=[u + "tq"], writes=[u + "tq"])
                    S.op("dve", lambda e, cs=cs, yp=yp: e.tensor_tensor(out=yp[0:96], in0=tq[0:96], in1=YS[0:96, cs], op=ALU.mult), reads=[u + "tq", u + "YS"], writes=[ypk])
                    S.dma("sp", Ygd.ap()[gc * 96:(gc + 1) * 96, cs], yp[0:96], reads=[ypk], writes=["Yg" + tag])
        if readout:
            TT = min(512, NT)
            yo = [A.alloc([TT], BF16) for _ in range(2)]
            sg = [A.alloc([TT], F32) for _ in range(2)]
            ygt = [A.alloc([4, TT], BF16) for _ in range(2)]
            for tt in range(NT // TT):
                yt, ytk = ygt[tt % 2], u + "ygt%d" % (tt % 2)
                S.dma("sp", yt[0:96], Ygd.ap()[:, tt * TT:(tt + 1) * TT].rearrange("(c p) t -> p c t", p=96), reads=["Yg" + tag], writes=[ytk], partial=False)
                for mo in range(4):
                    bc += 1
                    pst, pk = self.ps[bc % 2], self.psk[bc % 2]

                    def mmf(e, pst=pst, yt=yt, mo=mo):
                        ins = None
                        for cc in range(4):
                            ins = e.matmul(pst[0:96, 0:TT], lhsT=P["gluw"][0:96, cc, mo * 96:(mo + 1) * 96], rhs=yt[0:96, cc, :], start=(cc == 0), stop=(cc == 3))
                        return ins
                    S.op("pe", mmf, reads=[pu + "gluw", ytk], writes=[pk])
                    sgt, sk = sg[bc % 2], u + "sg%d" % (bc % 2)
                    S.op("act", lambda e, pst=pst, sgt=sgt, mo=mo: e.activation(out=sgt[0:96], in_=pst[0:96, 0:TT], func=AF.Sigmoid, bias=P["glub"][0:96, mo:mo + 1], scale=1.0),
                         reads=[pk, pu + "glub"], writes=[sk])
                    yot, yk = yo[bc % 2], u + "yo%d" % (bc % 2)
                    S.op("dve", lambda e, yot=yot, sgt=sgt, mo=mo, yt=yt: e.tensor_tensor(out=yot[0:96], in0=sgt[0:96], in1=yt[0:96, mo, :], op=ALU.mult),
                         reads=[sk, ytk], writes=[yk])
                    S.dma("sp", Ym.ap()[640 + mo * 96:640 + (mo + 1) * 96, tt * TT:(tt + 1) * TT], yot[0:96], reads=[yk], writes=["Ym" + tag])
        self.S.barrier()
        A.release(m0)

    def cast_weights(self, name, src_ap_2d, rows, cols):
        t = self.dscr(name, [rows, cols], BF16)
        step = 512
        for r0 in range(0, rows, step):
            r1 = min(rows, r0 + step)
            self.S.dma("pool", t.ap()[r0:r1, :], src_ap_2d[r0:r1, :], writes=[name])
        return t

    def phaseC(self, l, Xsrc, Xdst, L, tag, bl, moe, final):
        S, A, I = self.S, self.arena, self.I
        u = self.key("pC")
        NT = NS * L
        GT = min(512, L)
        TG = GT // 128
        Ym = self.scr["Ym" + tag]
        md = self.scr["modD%d" % l]
        if moe:
            experts = []
            for e in range(self.ne_cast):
                experts.append((self.cast_weights("wg%d_%d" % (l, e), I["moe_w_gate"].ap()[0, e], D, DFFE),
                                self.cast_weights("wu%d_%d" % (l, e), I["moe_w_up"].ap()[0, e], D, DFFE),
                                self.cast_weights("wd%d_%d" % (l, e), I["moe_w_down"].ap()[0, e], DFFE, D), e))
            experts = experts[:self.ne_comp]
            dff, FBW = DFFE, 512
        else:
            if ("wgd%d" % l) not in self.scr:
                self.cast_weights("wgd%d" % l, I["ffn_w_gate"].ap()[l // 2], D, DFF)
                self.cast_weights("wud%d" % l, I["ffn_w_up"].ap()[l // 2], D, DFF)
                self.cast_weights("wdd%d" % l, I["ffn_w_down"].ap()[l // 2], DFF, D)
            experts = [(self.scr["wgd%d" % l], self.scr["wud%d" % l], self.scr["wdd%d" % l], None)]
            dff, FBW = DFF, 256
        NCH = dff // 128
        FB = FBW // 128
        NBLK = dff // FBW
        DB = 7 if moe else 11
        m0 = A.mark()
        self._nt_ss = A.alloc([4], F32)
        self._nt_junk = A.alloc([D], F32)
        wout = A.alloc([KC, D], BF16)
        grow = A.alloc([len(bl), 2, D], F32)
        S.dma("pool", wout, I["w_out"].ap()[l].rearrange("(kc p) n -> p kc n", p=128), writes=[u + "wout"])
        for bi, b in enumerate(bl):
            S.dma("sp", grow[:, bi, 0, :], md.ap()[b, 2 * D:3 * D].partition_broadcast(128), reads=["modD%d" % l], writes=[u + "grow"])
            S.dma("sp", grow[:, bi, 1, :], md.ap()[b, 5 * D:6 * D].partition_broadcast(128), reads=["modD%d" % l], writes=[u + "grow"])
        if final:
            fg = A.alloc([D], F32)
            S.dma("sp", fg, I["final_g"].ap().partition_broadcast(128), writes=[u + "fg"])
        if moe:
            rw = A.alloc([KC, NE], F32)
            rb = A.alloc([NE], F32)
            if SKIPR < 2 or SKIPR == 3:
                S.dma("sp", rw, I["moe_router_w"].ap()[0].rearrange("(kc p) n -> p kc n", p=128), writes=[u + "rw"])
                S.dma("sp", rb, I["moe_router_b"].ap()[0].partition_broadcast(128), writes=[u + "rb"])
            h32 = A.alloc([KC, 128], F32)
            comb = A.alloc([TG, NE], F32)
            zb = A.alloc([1], F32)
            tmpc = [A.alloc([512], F32) for _ in range(2)]
            S.op("pool", lambda e: e.memset(zb, 0.0), writes=[u + "zb"])
            rt = A.alloc([6, NE], F32)
        x1t = A.alloc([TG, D], F32)
        acc = A.alloc([TG, D], F32)
        ymg = A.alloc([KC, GT], BF16)
        hfT = A.alloc([KC, GT], BF16)
        actT = A.alloc([NCH, GT], BF16)
        wgb = [A.alloc([KC, FBW], BF16) for _ in range(2)]
        wub = [A.alloc([KC, FBW], BF16) for _ in range(2)]
        wdb = [A.alloc([DB, 512], BF16) for _ in range(2)]
        sgt = [A.alloc([GT], F32) for _ in range(2)]
        xin = [A.alloc([D], F32) for _ in range(2)]
        wc = 0
        dc = 0
        pc = 0
        ec = 0
        for g in range(NT // GT):
            s_ = (g * GT) // L
            bi = s_ if len(bl) > 1 else 0
            b = bl[bi]
            S.dma("sp", ymg, Ym.ap()[:, g * GT:(g + 1) * GT].rearrange("(kc p) t -> p kc t", p=128), reads=["Ym" + tag], writes=[u + "ymg"], partial=False)
            for ti in range(TG):
                t = g * TG + ti
                xt, xk = xin[t % 2], u + "xin%d" % (t % 2)
                S.dma("sp", xt, Xsrc.ap()[t * 128:(t + 1) * 128, :], reads=[self.nm(Xsrc)], writes=[xk], partial=False)
                for half in range(2):
                    pst, pk = self.ps[half], self.psk[half]

                    def mmf(e, pst=pst, ti=ti, half=half):
                        ins = None
                        for kc in range(KC):
                            ins = e.matmul(pst, lhsT=ymg[:, kc, ti * 128:(ti + 1) * 128], rhs=wout[:, kc, half * 512:(half + 1) * 512], start=(kc == 0), stop=(kc == KC - 1))
                        return ins
                    S.op("pe", mmf, reads=[u + "ymg", u + "wout"], writes=[pk])
                    hs = slice(half * 512, (half + 1) * 512)
                    S.op("dve", lambda e, pst=pst, hs=hs, ti=ti, bi=bi: e.tensor_tensor(out=x1t[:, ti, hs], in0=pst, in1=grow[:, bi, 0, hs], op=ALU.mult),
                         reads=[pk, u + "grow"], writes=[u + "x1t%d" % ti], partial=True)
                S.op("dve", lambda e, ti=ti, xt=xt: e.tensor_tensor(out=x1t[:, ti, :], in0=x1t[:, ti, :], in1=xt, op=ALU.add),
                     reads=[u + "x1t%d" % ti, xk], writes=[u + "x1t%d" % ti])
                self.norm_transpose(x1t[:, ti, :], u + "x1t%d" % ti, hfT, u + "hfT", ti * 128, 1, b, hT32=((h32, u + "h32") if (moe and SKIPR < 2) else None), single32=True)
                if moe and SKIPR:
                    pass
                if moe and SKIPR:
                    S.op("pool", lambda e, ti=ti: e.memset(comb[:, ti, :], 0.5), reads=[u + "h32"], writes=[u + "comb"])
                elif moe:
                    pst, pk = self.ps[2], self.psk[2]

                    def mmr(e, pst=pst):
                        ins = None
                        for kc in range(KC):
                            ins = e.matmul(pst[:, 0:NE], lhsT=h32[:, kc, :], rhs=rw[:, kc, :], start=(kc == 0), stop=(kc == KC - 1))
                        return ins
                    S.op("pe", mmr, reads=[u + "h32", u + "rw"], writes=[pk])
                    lg, m1, k1, l2, m2, k2 = (rt[:, i, :] for i in range(6))
                    rk_ = u + "rt"
                    dv_ = lambda fn, r=(), w=(rk_,): S.op("dve", fn, reads=[rk_] + list(r), writes=list(w))
                    dv_(lambda e, pst=pst: e.tensor_tensor(out=lg, in0=pst[:, 0:NE], in1=rb, op=ALU.add), [pk, u + "rb"])
                    dv_(lambda e: e.tensor_reduce(out=m1[:, 0:1], in_=lg, axis=AX.X, op=ALU.max))
                    dv_(lambda e: e.tensor_scalar(out=k1, in0=lg, scalar1=m1[:, 0:1], scalar2=None, op0=ALU.is_equal))
                    dv_(lambda e: e.scalar_tensor_tensor(out=l2, in0=k1, scalar=-1e30, in1=lg, op0=ALU.mult, op1=ALU.add))
                    dv_(lambda e: e.tensor_reduce(out=m2[:, 0:1], in_=l2, axis=AX.X, op=ALU.max))
                    dv_(lambda e: e.tensor_scalar(out=k2, in0=l2, scalar1=m2[:, 0:1], scalar2=None, op0=ALU.is_equal))
                    dv_(lambda e: e.tensor_tensor(out=m2[:, 1:2], in0=m2[:, 0:1], in1=m1[:, 0:1], op=ALU.subtract))
                    S.op("act", lambda e: e.activation(out=m2[:, 2:3], in_=m2[:, 1:2], func=AF.Exp), reads=[rk_], writes=[rk_])
                    dv_(lambda e: e.tensor_scalar(out=m2[:, 3:4], in0=m2[:, 2:3], scalar1=1.0, scalar2=None, op0=ALU.add))
                    dv_(lambda e: e.reciprocal(out=m2[:, 3:4], in_=m2[:, 3:4]))
                    dv_(lambda e: e.tensor_tensor(out=m2[:, 4:5], in0=m2[:, 2:3], in1=m2[:, 3:4], op=ALU.mult))
                    dv_(lambda e: e.tensor_scalar(out=k1, in0=k1, scalar1=m2[:, 3:4], scalar2=None, op0=ALU.mult))
                    dv_(lambda e, ti=ti: e.scalar_tensor_tensor(out=comb[:, ti, :], in0=k2, scalar=m2[:, 4:5], in1=k1, op0=ALU.mult, op1=ALU.add), w=(rk_, u + "comb"))
            for xi, (wg, wu, wd, eidx) in enumerate(experts):
                for fb in range(NBLK):
                    wc += 1
                    wgt, wut = wgb[wc % 2], wub[wc % 2]
                    wgk, wuk = u + "wg%d" % (wc % 2), u + "wu%d" % (wc % 2)
                    S.dma("sp", wgt, wg.ap()[:, fb * FBW:(fb + 1) * FBW].rearrange("(kc p) n -> p kc n", p=128), reads=[self.nm(wg)], writes=[wgk], partial=False)
                    S.dma("sp", wut, wu.ap()[:, fb * FBW:(fb + 1) * FBW].rearrange("(kc p) n -> p kc n", p=128), reads=[self.nm(wu)], writes=[wuk], partial=False)
                    for fc in range(FB):
                        pc += 1
                        pg, pgk = self.ps[(pc % 2) * 2], self.psk[(pc % 2) * 2]
                        pu_, puk = self.ps[(pc % 2) * 2 + 1], self.psk[(pc % 2) * 2 + 1]
                        for (pst, pk, wt, wk) in ((pg, pgk, wgt, wgk), (pu_, puk, wut, wuk)):
                            def mmf(e, pst=pst, wt=wt, fc=fc):
                                ins = None
                                for kc in range(KC):
                                    ins = e.matmul(pst[:, 0:GT], lhsT=wt[:, kc, fc * 128:(fc + 1) * 128], rhs=hfT[:, kc, :], start=(kc == 0), stop=(kc == KC - 1))
                                return ins
                            S.op("pe", mmf, reads=[wk, u + "hfT"], writes=[pk])
                        sg_, sgk = sgt[pc % 2], u + "sgt%d" % (pc % 2)
                        S.op("act", lambda e, sg_=sg_, pg=pg: e.activation(out=sg_, in_=pg[:, 0:GT], func=AF.Silu), reads=[pgk], writes=[sgk])
                        S.op("dve", lambda e, sg_=sg_, pu_=pu_, ch=fb * FB + fc: e.tensor_tensor(out=actT[:, ch, :], in0=sg_, in1=pu_[:, 0:GT], op=ALU.mult),
                             reads=[sgk, puk], writes=[u + "actT"], partial=True)
                for half in range(2):
                    hs = slice(half * 512, (half + 1) * 512)
                    for db in range(NCH // DB):
                        dc += 1
                        wdt, wdk = wdb[dc % 2], u + "wd%d" % (dc % 2)
                        S.dma("sp", wdt, wd.ap()[db * DB * 128:(db + 1) * DB * 128, hs].rearrange("(c p) n -> p c n", p=128), reads=[self.nm(wd)], writes=[wdk], partial=False)

                        def mmd(e, wdt=wdt, db=db):
                            ins = None
                            for cc in range(DB):
                                ch = db * DB + cc
                                for ti in range(TG):
                                    ins = e.matmul(self.ps[4 + ti], lhsT=actT[:, ch, ti * 128:(ti + 1) * 128], rhs=wdt[:, cc, :], start=(ch == 0), stop=(ch == NCH - 1))
                            return ins
                        S.op("pe", mmd, reads=[wdk, u + "actT"], writes=[self.psk[4 + ti] for ti in range(TG)], partial=(db > 0))
                    for ti in range(TG):
                        if eidx is None or SKIPC:
                            S.op("dve", lambda e, ti=ti, hs=hs: e.tensor_copy(out=acc[:, ti, hs], in_=self.ps[4 + ti]), reads=[self.psk[4 + ti]], writes=[u + "acc%d" % ti], partial=True)
                        elif xi == 0:
                            S.op("act", lambda e, ti=ti, hs=hs, eidx=eidx: e.activation(out=acc[:, ti, hs], in_=self.ps[4 + ti], func=AF.Identity,
                                                                                      scale=comb[:, ti, eidx:eidx + 1], bias=zb[:, 0:1]),
                                 reads=[self.psk[4 + ti], u + "comb", u + "zb"], writes=[u + "acc%d" % ti], partial=True)
                        else:
                            ec += 1
                            tmp, tk = tmpc[ec % 2], u + "tmpc%d" % (ec % 2)
                            S.op("act", lambda e, ti=ti, tmp=tmp, eidx=eidx: e.activation(out=tmp, in_=self.ps[4 + ti], func=AF.Identity,
                                                                                        scale=comb[:, ti, eidx:eidx + 1], bias=zb[:, 0:1]),
                                 reads=[self.psk[4 + ti], u + "comb", u + "zb"], writes=[tk])
                            S.op("dve", lambda e, ti=ti, hs=hs, tmp=tmp: e.tensor_tensor(out=acc[:, ti, hs], in0=acc[:, ti, hs], in1=tmp, op=ALU.add),
                                 reads=[tk, u + "acc%d" % ti], writes=[u + "acc%d" % ti])
            for ti in range(TG):
                t = g * TG + ti
                ak = u + "acc%d" % ti
                S.op("dve", lambda e, ti=ti, bi=bi: e.tensor_tensor(out=acc[:, ti, :], in0=acc[:, ti, :], in1=grow[:, bi, 1, :], op=ALU.mult), reads=[ak, u + "grow"], writes=[ak])
                S.op("dve", lambda e, ti=ti: e.tensor_tensor(out=acc[:, ti, :], in0=acc[:, ti, :], in1=x1t[:, ti, :], op=ALU.add), reads=[ak, u + "x1t%d" % ti], writes=[ak])
                if final:
                    ss = self._nt_ss
                    S.op("act", lambda e, ti=ti: e.activation(out=self._nt_junk, in_=acc[:, ti, :], func=AF.Square, accum_out=ss[:, 0:1]), reads=[ak], writes=["nt_ss", "nt_junk"])
                    S.op("act", lambda e: e.activation(out=ss[:, 1:2], in_=ss[:, 0:1], func=AF.Sqrt, scale=1.0 / D, bias=self.epsb[:, 0:1]), reads=["nt_ss", "epsb"], writes=["nt_ss"])
                    S.op("dve", lambda e: e.reciprocal(out=ss[:, 2:3], in_=ss[:, 1:2]), reads=["nt_ss"], writes=["nt_rs"])
                    S.op("dve", lambda e, ti=ti: e.scalar_tensor_tensor(out=acc[:, ti, :], in0=acc[:, ti, :], scalar=ss[:, 2:3], in1=fg, op0=ALU.mult, op1=ALU.mult),
                         reads=[ak, "nt_rs", u + "fg"], writes=[ak])
                S.dma("sp", Xdst.ap()[t * 128:(t + 1) * 128, :], acc[:, ti, :], reads=[ak], writes=[self.nm(Xdst)])
        self.S.barrier()
        A.release(m0)

    def program(self):
        S = self.S
        stop = self.stop_after
        xres = self.dscr("xres", [NS * L_LAT, D], F32)
        cres = self.dscr("cres", [NS * L_CTX, D], F32)
        for l in range(DEPTH):
            last = (l == DEPTH - 1)
            self.prep_mod(l)
            self.prep_fnet(l)
            S.barrier()
            csrc = self.I["ctx"] if l == 0 else cres
            self.phaseA(l, csrc, L_CTX, False, lambda s: 2, "c")
            if not last:
                self.fnet(l, L_CTX, "c")
                self.hyena_filters(l, L_CTX, "c")
                self.hyena(l, L_CTX, "c")
            ms = self.arena.mark()
            P = self.s5_prep(l)
            self.s5_main(l, L_CTX, "c", P, False, not last)
            S.op("act", lambda e: e.copy(out=self.s0, in_=self.s0n), reads=["s0n"], writes=["s0"])
            S.barrier()
            self.arena.release(ms)
            if not last:
                self.phaseC(l, csrc, cres, L_CTX, "c", [2], False, False)
            if stop == "C%d" % l:
                break
            xsrc = self.I["x"] if l == 0 else xres
            xdst = self.out if last else xres
            self.phaseA(l, xsrc, L_LAT, True, lambda s: s, "x")
            if stop == "xA%d" % l:
                break
            self.fnet(l, L_LAT, "x")
            if stop == "xF%d" % l:
                break
            self.hyena_filters(l, L_LAT, "x")
            if stop == "xG%d" % l:
                break
            self.hyena(l, L_LAT, "x")
            if stop == "xH%d" % l:
                break
            ms = self.arena.mark()
            P = self.s5_prep(l)
            self.s5_main(l, L_LAT, "x", P, True, True)
            S.barrier()
            self.arena.release(ms)
            if stop == "M%d" % l:
                break
            self.phaseC(l, xsrc, xdst, L_LAT, "x", [0, 1], last, last)
            if stop == "L%d" % l:
                break
        self.finish()

    def finish(self):
        S = self.S
        keys = [k for k in S.res.keys() if k in self.debug or k == "out"]
        toks = []
        for k in list(S.res.keys()):
            r = S.res[k]
            toks += r["w"] + r["r"]
        waits = S._waits("sp", toks)
        S.q["sp"].append((waits, None, None, 0))
        S.emit()
        self.st.close()


_CACHE = {}


def make_in_maps(inputs, cores=range(NCORES)):
    cst = _CACHE.get("consts")
    if cst is None:
        cst = _consts()
        _CACHE["consts"] = cst
    maps = []
    f32 = lambda a: np.ascontiguousarray(np.asarray(a, dtype=np.float32))
    shared = {}
    for name, shape in IN_SPECS:
        if name in ("x", "ctx", "cvec"):
            continue
        shared[name] = f32(inputs[name]).reshape(shape)
    x = np.asarray(inputs["x"])
    ctx = np.asarray(inputs["ctx"])
    c = np.asarray(inputs["c"])
    c_ctx = np.asarray(inputs["c_ctx"])
    for ci in cores:
        m = dict(shared)
        m.update(cst)
        m["x"] = f32(x[ci * NS:(ci + 1) * NS]).reshape(NS * L_LAT, D)
        m["ctx"] = f32(ctx[ci * NS:(ci + 1) * NS]).reshape(NS * L_CTX, D)
        cc = np.stack([c[ci * NS], c[ci * NS + 1], c_ctx], axis=0)
        m["cvec"] = f32(cc.T.reshape(KC, 128, 3).transpose(1, 0, 2))
        maps.append(m)
    return maps


def kernel(**inputs):
    B = _CACHE.get("B")
    if B is None:
        B = Builder()
        B.program()
        _CACHE["B"] = B
    maps = make_in_maps(inputs)
    res = run_bass_kernel_spmd(B.nc, maps, core_ids=list(range(NCORES)))
    out = np.concatenate([r["out"].reshape(NS, L_LAT, D) for r in res.results], axis=0)
    return out.astype(np.float32)
```

```python
import math
from contextlib import ExitStack
import numpy as np
import ml_dtypes
import concourse.bass as bass
import concourse.mybir as mybir
from concourse.bass_utils import run_bass_kernel_spmd

F32 = mybir.dt.float32
BF16 = mybir.dt.bfloat16
I32 = mybir.dt.int32
AF = mybir.ActivationFunctionType
ALU = mybir.AluOpType
AX = mybir.AxisListType

import os
SKIPR = int(os.environ.get('SKIPR', 0))
SKIPC = int(os.environ.get('SKIPC', 0))
SCOPES = int(os.environ.get('SCOPES', 0))
NCORES = 8
NS = 2
D = 1024
KC = 8
L_LAT = 4096
L_CTX = 256
DEPTH = 2
D_IN = 1792
HY_OFF = 256
S5_OFF = 1408
DFF = 2816
DFFE = 3584
NE = 8
EPS = 1e-6
TWO_PI = 2.0 * math.pi


class Sched:
    ENG = ("pe", "act", "dve", "pool", "sp")

    def __init__(self, nc):
        self.nc = nc
        self.q = {e: [] for e in self.ENG}
        self.ecount = {e: 0 for e in self.ENG}
        self.seen = {e: {} for e in self.ENG}
        self.res = {}
        self.dcount = {}
        self.semnames = ["c_pe", "c_act", "c_dve", "c_pool"]
        self.keysem = {}
        self.free_sems = []
        self.phase = "init"

    def _r(self, k):
        r = self.res.get(k)
        if r is None:
            r = dict(w=[], r=[], pw=[], pr=[], partial=False)
            self.res[k] = r
        return r

    def _deps(self, reads, writes, partial):
        deps = []
        for k in reads:
            deps += self._r(k)["w"]
        joins = []
        for k in writes:
            r = self._r(k)
            if partial and r["partial"] and not r["r"] and r["w"]:
                deps += r["pw"] + r["pr"]
                joins.append(k)
            else:
                deps += r["w"] + r["r"]
        return deps, joins

    def _commit(self, tok, reads, writes, partial, joins):
        for k in reads:
            rr = self._r(k)["r"]
            rr.append(tok)
            if len(rr) > 64:
                mx = {}
                for (s, v) in rr:
                    if mx.get(s, 0) < v:
                        mx[s] = v
                rr[:] = list(mx.items())
        for k in writes:
            r = self._r(k)
            if k in joins:
                r["w"].append(tok)
                if len(r["w"]) > 64:
                    mx = {}
                    for (s, v) in r["w"]:
                        if mx.get(s, 0) < v:
                            mx[s] = v
                    r["w"][:] = list(mx.items())
            else:
                r["pw"], r["pr"] = r["w"], r["r"]
                r["w"], r["r"] = [tok], []
                r["partial"] = partial

    def _waits(self, eng, deps, skip_self=False):
        need = {}
        for (s, v) in deps:
            if skip_self and s == "c_" + eng:
                continue
            if self.seen[eng].get(s, 0) >= v:
                continue
            if need.get(s, 0) < v:
                need[s] = v
        for s, v in need.items():
            self.seen[eng][s] = v
        return list(need.items())

    def op(self, eng, fn, reads=(), writes=(), partial=False):
        deps, joins = self._deps(reads, writes, partial)
        waits = self._waits(eng, deps, skip_self=(eng == "pe"))
        self.ecount[eng] += 1
        tok = ("c_" + eng, self.ecount[eng])
        self.q[eng].append((waits, fn, tok, 1, self.phase))
        self._commit(tok, reads, writes, partial, joins)
        return tok

    def dma(self, eng, out, in_, reads=(), writes=(), partial=True, **kw):
        assert len(writes) == 1
        k = writes[0]
        deps, joins = self._deps(reads, writes, partial)
        waits = self._waits(eng, deps)
        sname = self.keysem.get(k)
        if sname is None:
            if self.free_sems:
                sname = self.free_sems.pop()
            else:
                sname = "d_%d" % len(self.dcount)
                self.dcount[sname] = 0
                self.semnames.append(sname)
            self.keysem[k] = sname
        self.dcount[sname] += 16
        tok = (sname, self.dcount[sname])

        def fn(e, out=out, in_=in_, kw=kw):
            return e.dma_start(out=out, in_=in_, **kw)

        self.q[eng].append((waits, fn, tok, 16, self.phase))
        self._commit(tok, reads, writes, partial, joins)
        return tok

    def barrier(self):
        toks = []
        for e in ("pe", "act", "dve", "pool"):
            if self.ecount[e]:
                toks.append(("c_" + e, self.ecount[e]))
        for s, v in self.dcount.items():
            if v:
                toks.append((s, v))
        for e in self.ENG:
            waits = self._waits(e, toks)
            if waits:
                self.q[e].append((waits, None, None, 0, self.phase))
        self.keysem = {}
        self.free_sems = list(self.dcount.keys())

    def emit(self):
        nc = self.nc
        with ExitStack() as st:
            sems = {}
            for i, s in enumerate(self.semnames):
                sems[s] = st.enter_context(nc.semaphore("s%d" % i))
            block = st.enter_context(nc.Block())
            engs = {"pe": block.tensor, "act": block.scalar, "dve": block.vector,
                    "pool": block.gpsimd, "sp": block.sync}
            for ename, deco in engs.items():
                items = self.q[ename]

                def body(e, items=items):
                    cur = None
                    cm = None
                    for (waits, fn, tok, inc, ph) in items:
                        if SCOPES and ph != cur:
                            if cm is not None:
                                cm.__exit__(None, None, None)
                            cm = nc.named_scope(ph)
                            cm.__enter__()
                            cur = ph
                        for (s, v) in waits:
                            e.wait_ge(sems[s], v)
                        if fn is None:
                            continue
                        ins = fn(e)
                        ins.then_inc(sems[tok[0]], inc)
                    if cm is not None:
                        cm.__exit__(None, None, None)
                deco(body)


def _dsize(dt):
    return {F32: 4, BF16: 2, I32: 4}[dt]


class Arena:
    def __init__(self, nc, st, nbytes):
        self.t = st.enter_context(nc.sbuf_tensor("arena", [128, nbytes // 4], F32))
        self.nbytes = nbytes
        self.off = 0
        self.uid = 0

    def mark(self):
        return self.off

    def release(self, m):
        self.off = m

    def alloc(self, free_shape, dt, parts=128):
        n = int(np.prod(free_shape))
        size = (n * _dsize(dt) + 31) // 32 * 32
        assert self.off + size <= self.nbytes, ("arena overflow", self.off, size)
        a = self.t[0:parts, self.off // 4:(self.off + size) // 4]
        if dt != F32:
            a = a.bitcast(dt)
        a = a[:, 0:n]
        if len(free_shape) == 2:
            a = a.rearrange("p (a b) -> p a b", a=free_shape[0])
        elif len(free_shape) == 3:
            a = a.rearrange("p (a b c) -> p a b c", a=free_shape[0], b=free_shape[1])
        self.off += size
        self.uid += 1
        return a


def _consts():
    c = {}
    for L in (L_LAT, L_CTX):
        t = np.arange(L, dtype=np.int64)
        tf = (np.outer(t, t) % L).astype(np.float64) * (2.0 * np.pi / L)
        c["dftc%d" % L] = (np.cos(tf) / np.sqrt(L)).astype(ml_dtypes.bfloat16)
        c["dfts%d" % L] = (-np.sin(tf) / np.sqrt(L)).astype(ml_dtypes.bfloat16)
        tt = np.linspace(0.0, 1.0, L, dtype=np.float32)[:, None]
        w = (np.float32(2.0 * math.pi / L) * np.arange(L, dtype=np.float32))[:, None]
        bands = np.linspace(1e-4, 15, 16, dtype=np.float32)[None, :]
        z = np.concatenate([tt, np.cos(bands * w), -np.sin(bands * w)], axis=-1).astype(np.float32)
        c["zpos%d" % L] = np.ascontiguousarray(z.T)
        max_decay = math.log(0.01) / 0.3
        min_decay = math.log(0.01) / 1.5
        deltas = np.abs(np.linspace(min_decay, max_decay, 384, dtype=np.float32))
        c["dec%d" % L] = np.exp(-tt.T.astype(np.float32) * deltas[:, None]).astype(np.float32)
    k = np.arange(64)
    a = np.outer(k, k) * (2.0 * np.pi / 64)
    c64 = np.cos(a) / 8.0
    s64 = np.sin(a) / 8.0
    z64 = np.zeros((64, 64))
    c["c64bd"] = np.block([[c64, z64], [z64, c64]]).astype(np.float32)
    c["s64bd"] = np.block([[s64, z64], [z64, s64]]).astype(np.float32)
    ii = np.arange(128) // 16
    c["mask_le"] = (ii[:, None] <= ii[None, :]).astype(np.float32)
    c["mask_ge"] = (ii[:, None] >= ii[None, :]).astype(np.float32)
    c["ident"] = np.eye(128, dtype=np.float32)
    pw = list(range(-7, 9)) + [8 * (2 ** k) for k in range(10)]
    c["s5pw"] = np.tile(np.array(pw, dtype=np.float32)[None, :], (128, 1))
    return c


S5PW = list(range(-7, 9)) + [8 * (2 ** k) for k in range(10)]


def PWI(n):
    return S5PW.index(n)


IN_SPECS = [
    ("x", [NS * L_LAT, D]), ("ctx", [NS * L_CTX, D]), ("cvec", [128, KC, 3]),
    ("ada_w", [DEPTH, D, 6 * D]), ("ada_b", [DEPTH, 6 * D]),
    ("norm_mix_g", [DEPTH, D]), ("norm_ffn_g", [DEPTH, D]),
    ("w_in", [DEPTH, D, D_IN]), ("w_out", [DEPTH, D, D]),
    ("fnet_w", [DEPTH, 4, 64, 64]),
    ("hy_conv_w", [DEPTH, 3, 1152]), ("hy_conv_b", [DEPTH, 1152]),
    ("hy_fw1", [DEPTH, 33, 64]), ("hy_fb1", [DEPTH, 64]), ("hy_freq1", [DEPTH, 64]),
    ("hy_fw2", [DEPTH, 64, 64]), ("hy_fb2", [DEPTH, 64]), ("hy_freq2", [DEPTH, 64]),
    ("hy_fw3", [DEPTH, 64, 1536]), ("hy_bias", [DEPTH, 2, 384]),
    ("s5_a_re", [DEPTH, 2, 24, 64]), ("s5_a_im", [DEPTH, 2, 24, 64]), ("s5_log_dt", [DEPTH, 2, 24]),
    ("s5_b_re", [DEPTH, 2, 24, 64, 16]), ("s5_b_im", [DEPTH, 2, 24, 64, 16]),
    ("s5_c_re", [DEPTH, 2, 24, 16, 64]), ("s5_c_im", [DEPTH, 2, 24, 16, 64]),
    ("s5_d", [DEPTH, 384]), ("s5_glu_w", [DEPTH, 384, 384]), ("s5_glu_b", [DEPTH, 384]),
    ("ffn_w_gate", [1, D, DFF]), ("ffn_w_up", [1, D, DFF]), ("ffn_w_down", [1, DFF, D]),
    ("moe_router_w", [1, D, NE]), ("moe_router_b", [1, NE]),
    ("moe_w_gate", [1, NE, D, DFFE]), ("moe_w_up", [1, NE, D, DFFE]), ("moe_w_down", [1, NE, DFFE, D]),
    ("final_g", [D]),
]
CONST_SPECS = [
    ("dftc4096", [4096, 4096], BF16), ("dfts4096", [4096, 4096], BF16),
    ("dftc256", [256, 256], BF16), ("dfts256", [256, 256], BF16),
    ("zpos4096", [33, 4096], F32), ("zpos256", [33, 256], F32),
    ("dec4096", [384, 4096], F32), ("dec256", [384, 256], F32),
    ("c64bd", [128, 128], F32), ("s64bd", [128, 128], F32),
    ("mask_le", [128, 128], F32), ("mask_ge", [128, 128], F32),
    ("ident", [128, 128], F32), ("s5pw", [128, 26], F32),
]


class Builder:
    def __init__(self, debug=(), stop_after=None, ne_cast=NE, ne_comp=NE):
        self.ne_cast = ne_cast
        self.ne_comp = ne_comp
        self.debug = set(debug)
        self.stop_after = stop_after
        nc = bass.Bass("TRN2", target_bir_lowering=False)
        self.nc = nc
        self.S = Sched(nc)
        self.st = ExitStack()
        self.I = {}
        for name, shape in IN_SPECS:
            self.I[name] = nc.dram_tensor(name, shape, F32, kind="ExternalInput")
        for name, shape, dt in CONST_SPECS:
            self.I[name] = nc.dram_tensor(name, shape, dt, kind="ExternalInput")
        self.out = nc.dram_tensor("out", [NS * L_LAT, D], F32, kind="ExternalOutput")
        self.scr = {}
        self.names = {id(self.out): "out"}
        self.arena = Arena(nc, self.st, 192 * 1024)
        self.ps = [self.st.enter_context(nc.psum_tensor("psb%d" % i, [128, 512], F32))[:] for i in range(8)]
        self.psk = ["ps%d" % i for i in range(8)]
        self.uid = 0
        self._moe_experts = None
        A = self.arena
        self.ident = A.alloc([128], F32)
        self.identb = A.alloc([128], BF16)
        self.ones = A.alloc([128], F32)
        self.epsb = A.alloc([1], F32)
        self.Am = [A.alloc([KC, 3], F32) for _ in range(2)]
        self.Bm = [A.alloc([KC, 3], F32) for _ in range(2)]
        self.s0 = A.alloc([48, NS], F32)
        self.RHSf = A.alloc([2, 256], BF16)
        self.s0n = A.alloc([48, NS], F32)
        S = self.S
        S.dma("sp", self.ident, self.I["ident"].ap(), writes=["ident"])
        S.op("dve", lambda e: e.tensor_copy(out=self.identb, in_=self.ident), reads=["ident"], writes=["identb"])
        S.op("pool", lambda e: e.memset(self.ones, 1.0), writes=["ones"])
        S.op("pool", lambda e: e.memset(self.epsb, EPS), writes=["epsb"])

    def dscr(self, name, shape, dt):
        kind = "ExternalOutput" if name in self.debug else "Internal"
        t = self.nc.dram_tensor(name, shape, dt, kind=kind)
        self.scr[name] = t
        self.names[id(t)] = name
        return t

    def nm(self, t):
        return self.names.get(id(t), "input")

    def dump(self, name, ap, key, shape, dt):
        if name not in self.debug:
            return
        t = self.dscr(name, shape, dt)
        self.S.dma("sp", t.ap(), ap, reads=[key], writes=["dbg_" + name])

    def key(self, base):
        self.uid += 1
        return "%s#%d" % (base, self.uid)

    def range_reduce_sin(self, eng, out, ang, tmp_i, tmp_f, keys_r, key_w, key_i, key_f, shift=0.0):
        S = self.S
        S.op(eng, lambda e: e.tensor_scalar(out=tmp_i, in0=ang, scalar1=shift, scalar2=1.0 / TWO_PI, op0=ALU.add, op1=ALU.mult),
             reads=keys_r, writes=[key_i])
        S.op(eng, lambda e: e.tensor_copy(out=tmp_f, in_=tmp_i), reads=[key_i], writes=[key_f])
        S.op(eng, lambda e: e.scalar_tensor_tensor(out=tmp_f, in0=tmp_f, scalar=-TWO_PI, in1=ang, op0=ALU.mult, op1=ALU.add),
             reads=[key_f] + list(keys_r), writes=[key_f])
        S.op(eng, lambda e: e.tensor_scalar(out=tmp_f, in0=tmp_f, scalar1=shift, scalar2=math.pi, op0=ALU.add, op1=ALU.min),
             reads=[key_f], writes=[key_f])
        S.op(eng, lambda e: e.tensor_scalar(out=tmp_f, in0=tmp_f, scalar1=-math.pi, scalar2=None, op0=ALU.max),
             reads=[key_f], writes=[key_f])
        S.op("act", lambda e: e.activation(out=out, in_=tmp_f, func=AF.Sin), reads=[key_f], writes=[key_w], partial=True)

    def prep_mod(self, l):
        S, A, I = self.S, self.arena, self.I
        m0 = A.mark()
        cv = A.alloc([KC, 3], F32)
        adab = A.alloc([48], F32)
        gmix = A.alloc([KC], F32)
        gffn = A.alloc([KC], F32)
        modT = A.alloc([48, 3], F32)
        wt = [A.alloc([KC, 512], F32) for _ in range(2)]
        u = self.key("pm")
        S.dma("sp", cv, I["cvec"].ap(), writes=[u + "cv"])
        S.op("act", lambda e: e.activation(out=cv, in_=cv, func=AF.Silu), reads=[u + "cv"], writes=[u + "cv"])
        S.dma("sp", adab, I["ada_b"].ap()[l].rearrange("(f p) -> p f", p=128), writes=[u + "adab"], allow_slow_non_contiguous=True)
        S.dma("sp", gmix, I["norm_mix_g"].ap()[l].rearrange("(f p) -> p f", p=128), writes=[u + "gmix"], allow_slow_non_contiguous=True)
        S.dma("sp", gffn, I["norm_ffn_g"].ap()[l].rearrange("(f p) -> p f", p=128), writes=[u + "gffn"], allow_slow_non_contiguous=True)
        for blk in range(12):
            w = wt[blk % 2]
            wk = u + "wt%d" % (blk % 2)
            S.dma("sp", w, I["ada_w"].ap()[l][:, blk * 512:(blk + 1) * 512].rearrange("(kc p) n -> p kc n", p=128), writes=[wk])
            pst = self.ps[blk % 2]
            pk = self.psk[blk % 2]

            def mmf(e, w=w, pst=pst):
                ins = None
                for m in range(4):
                    for kc in range(KC):
                        ins = e.matmul(pst[:, m * 3:(m + 1) * 3], lhsT=w[:, kc, m * 128:(m + 1) * 128], rhs=cv[:, kc, :],
                                       start=(kc == 0), stop=(kc == KC - 1))
                return ins
            S.op("pe", mmf, reads=[wk, u + "cv"], writes=[pk])
            S.op("dve", lambda e, pst=pst, blk=blk: e.tensor_tensor(
                out=modT[:, blk * 4:(blk + 1) * 4, :], in0=pst[:, 0:12].rearrange("p (m b) -> p m b", m=4),
                in1=adab[:, blk * 4:(blk + 1) * 4].unsqueeze(2).to_broadcast([128, 4, 3]), op=ALU.add),
                reads=[pk, u + "adab"], writes=[u + "modT"], partial=True)
        for wi, (gv, gk, so, ho) in enumerate(((gmix, u + "gmix", 8, 0), (gffn, u + "gffn", 32, 24))):
            S.op("dve", lambda e, wi=wi, so=so: e.tensor_scalar(out=self.Am[wi], in0=modT[:, so:so + 8, :], scalar1=1.0, scalar2=None, op0=ALU.add),
                 reads=[u + "modT"], writes=["Am%d" % wi])
            S.op("dve", lambda e, wi=wi, gv=gv: e.tensor_tensor(out=self.Am[wi], in0=self.Am[wi], in1=gv.unsqueeze(2).to_broadcast([128, 8, 3]), op=ALU.mult),
                 reads=["Am%d" % wi, gk], writes=["Am%d" % wi])
            S.op("dve", lambda e, wi=wi, ho=ho: e.tensor_copy(out=self.Bm[wi], in_=modT[:, ho:ho + 8, :]),
                 reads=[u + "modT"], writes=["Bm%d" % wi])
        md = self.scr.get("modD%d" % l) or self.dscr("modD%d" % l, [3, 6 * D], F32)
        for b in range(3):
            S.dma("sp", md.ap()[b].rearrange("(f p) -> p f", p=128), modT[:, :, b], reads=[u + "modT"], writes=["modD%d" % l],
                  allow_slow_non_contiguous=True)
        self.S.barrier()
        A.release(m0)

    def norm_transpose(self, xt, xk, hT, hk, col0, which, b, hT32=None, single32=False):
        S = self.S
        u = self.key("nt")
        ss = self._nt_ss
        junk = self._nt_junk
        S.op("act", lambda e: e.activation(out=junk, in_=xt, func=AF.Square, accum_out=ss[:, 0:1]), reads=[xk], writes=["nt_ss", "nt_junk"])
        S.op("act", lambda e: e.activation(out=ss[:, 1:2], in_=ss[:, 0:1], func=AF.Sqrt, scale=1.0 / D, bias=self.epsb[:, 0:1]),
             reads=["nt_ss", "epsb"], writes=["nt_ss"])
        S.op("dve", lambda e: e.reciprocal(out=ss[:, 2:3], in_=ss[:, 1:2]), reads=["nt_ss"], writes=["nt_rs"])
        S.op("dve", lambda e: e.tensor_scalar(out=junk, in0=xt, scalar1=ss[:, 2:3], scalar2=None, op0=ALU.mult),
             reads=[xk, "nt_rs"], writes=["nt_junk"])
        for half in range(2):
            pst = self.ps[6 + half]
            pk = self.psk[6 + half]

            def tr(e, half=half, pst=pst):
                ins = None
                for q in range(4):
                    kc = half * 4 + q
                    ins = e.transpose(pst[:, q * 128:(q + 1) * 128], junk[:, kc * 128:(kc + 1) * 128], self.ident)
                return ins
            S.op("pe", tr, reads=["nt_junk", "ident"], writes=[pk])
            for q in range(4):
                kc = half * 4 + q
                S.op("act", lambda e, q=q, kc=kc, pst=pst: e.activation(
                    out=hT[:, kc, col0:col0 + 128], in_=pst[:, q * 128:(q + 1) * 128], func=AF.Identity,
                    scale=self.Am[which][:, kc, b:b + 1], bias=self.Bm[which][:, kc, b:b + 1]),
                    reads=[pk, "Am%d" % which, "Bm%d" % which], writes=[hk], partial=True)
                if hT32 is not None:
                    S.op("act", lambda e, q=q, kc=kc, pst=pst: e.activation(
                        out=(hT32[0][:, kc, :] if single32 else hT32[0][:, kc, col0:col0 + 128]), in_=pst[:, q * 128:(q + 1) * 128],
                        func=AF.Identity, scale=self.Am[which][:, kc, b:b + 1], bias=self.Bm[which][:, kc, b:b + 1]),
                        reads=[pk, "Am%d" % which, "Bm%d" % which], writes=[hT32[1]], partial=True)

    def prep_fnet(self, l):
        S, A, I = self.S, self.arena, self.I
        u = self.key("pf")
        m0 = A.mark()
        cbd = A.alloc([128], F32)
        sbd = A.alloc([128], F32)
        wbd = A.alloc([2, 128], F32)
        S.dma("sp", cbd, I["c64bd"].ap(), writes=[u + "cbd"])
        S.dma("sp", sbd, I["s64bd"].ap(), writes=[u + "sbd"])
        S.op("pool", lambda e: e.memset(wbd, 0.0), writes=[u + "wbd"])
        for h in range(4):
            j, hh = h // 2, h % 2
            S.dma("sp", wbd[hh * 64:(hh + 1) * 64, j, hh * 64:(hh + 1) * 64], I["fnet_w"].ap()[l, h], reads=[], writes=[u + "wbd"])
        for j in range(2):
            pst = self.ps[j]
            pk = self.psk[j]

            def mmf(e, j=j, pst=pst):
                e.matmul(pst[:, 0:128], lhsT=cbd, rhs=wbd[:, j, :], start=True, stop=True)
                return e.matmul(pst[:, 128:256], lhsT=sbd, rhs=wbd[:, j, :], start=True, stop=True)
            S.op("pe", mmf, reads=[u + "cbd", u + "sbd", u + "wbd"], writes=[pk])
            S.op("dve", lambda e, j=j, pst=pst: e.tensor_copy(out=self.RHSf[:, j, :], in_=pst[:, 0:256]), reads=[pk], writes=["RHSf"], partial=True)
        self.S.barrier()
        A.release(m0)

    def phaseA(self, l, Xd, L, grid, bfun, tag):
        S, A, I = self.S, self.arena, self.I
        u = self.key("pA")
        NT = NS * L
        GT = min(512, L)
        TG = GT // 128
        R = 64 if grid else L
        NBk = L // 8
        ABd = self.scr.get("AB" + tag) or self.dscr("AB" + tag, [NT, 512], BF16)
        Hq = self.scr.get("Hq" + tag) or self.dscr("Hq" + tag, [3, NT, 384], BF16)
        Xs = self.scr.get("Xs" + tag) or self.dscr("Xs" + tag, [8, 384, NS, NBk], BF16)
        m0 = A.mark()
        self._nt_ss = A.alloc([4], F32)
        self._nt_junk = A.alloc([D], F32)
        win = A.alloc([KC, 256 + 384], BF16)
        wtap = A.alloc([KC, 3, 1152], BF16)
        cwb = A.alloc([3, 1152], F32)
        cbb = A.alloc([1152], F32)
        S.dma("pool", win[:, :, 0:256], I["w_in"].ap()[l][:, 0:256].rearrange("(kc p) n -> p kc n", p=128), writes=[u + "win"])
        S.dma("pool", win[:, :, 256:640], I["w_in"].ap()[l][:, S5_OFF:D_IN].rearrange("(kc p) n -> p kc n", p=128), writes=[u + "win"])
        S.dma("sp", cwb, I["hy_conv_w"].ap()[l].rearrange("t n -> (t n)").partition_broadcast(128), writes=[u + "cwb"])
        S.dma("sp", cbb, I["hy_conv_b"].ap()[l].partition_broadcast(128), writes=[u + "cbb"])
        m1 = A.mark()
        wf32 = A.alloc([1152], F32)
        for kc in range(KC):
            S.dma("sp", wf32, I["w_in"].ap()[l][kc * 128:(kc + 1) * 128, HY_OFF:S5_OFF], writes=[u + "wf32"], partial=False)
            for tap in range(3):
                S.op("dve" if tap != 1 else "pool", lambda e, kc=kc, tap=tap: e.tensor_tensor(out=wtap[:, kc, tap, :], in0=wf32, in1=cwb[:, tap, :], op=ALU.mult),
                     reads=[u + "wf32", u + "cwb"], writes=[u + "wtap"], partial=True)
        self.dump("dbg_wtap", wtap[:, 0, :, :], u + "wtap", [128, 3, 1152], BF16)
        self.dump("dbg_cwb", cwb, u + "cwb", [128, 3, 1152], F32)
        self.dump("dbg_rhsf", self.RHSf, "RHSf", [128, 2, 256], BF16)
        self.S.barrier()
        A.release(m1)
        xts = [A.alloc([D], F32) for _ in range(2)]
        hT = [[A.alloc([KC, GT], BF16) for _ in range(3)] for _ in range(2)]
        hR = [A.alloc([KC, GT], BF16) for _ in range(3)]
        pfT = A.alloc([2, GT], BF16)
        abt = [A.alloc([512], BF16) for _ in range(2)]
        hqt = [A.alloc([384], BF16) for _ in range(2)]
        psd = [A.alloc([8, GT // 8], BF16) for _ in range(2)]
        for bi in range(2):
            for tp in (0, 2):
                S.op("pool", lambda e, bi=bi, tp=tp: e.memset(hT[bi][tp], 0.0), writes=[u + "hT%d_%d" % (bi, tp)])
        ngroups = NT // GT
        cnt = 0
        for g in range(ngroups):
            bi = g % 2
            s = (g * GT) // L
            b = bfun(s)
            hc, hm, hp = hT[bi][1], hT[bi][0], hT[bi][2]
            hck, hmk, hpk = [u + "hT%d_%d" % (bi, t) for t in (1, 0, 2)]
            for ti in range(TG):
                t = g * TG + ti
                xt = xts[t % 2]
                xk = u + "xt%d" % (t % 2)
                S.dma("sp", xt, Xd.ap()[t * 128:(t + 1) * 128, :], reads=[self.nm(Xd)], writes=[xk], partial=False)
                self.norm_transpose(xt, xk, hc, hck, ti * 128, 0, b)
            hcv = hc.rearrange("p k (r c) -> p (k r) c", c=R)
            hmv = hm.rearrange("p k (r c) -> p (k r) c", c=R)
            hpv = hp.rearrange("p k (r c) -> p (k r) c", c=R)
            S.op("pool", lambda e, hmv=hmv, hcv=hcv: e.tensor_copy(out=hmv[:, :, 1:R], in_=hcv[:, :, 0:R - 1]), reads=[hck], writes=[hmk])
            S.op("dve", lambda e, hpv=hpv, hcv=hcv: e.tensor_copy(out=hpv[:, :, 0:R - 1], in_=hcv[:, :, 1:R]), reads=[hck], writes=[hpk])
            for tp, (src, sk) in enumerate(((hm, hmk), (hc, hck), (hp, hpk))):
                S.op("pool" if tp == 1 else "dve", lambda e, tp=tp, src=src: e.tensor_copy(
                    out=hR[tp].rearrange("p k (t c) -> p (k t) c", c=128),
                    in_=src.rearrange("p k (t c) -> p (k t) c", c=128)[:, :, ::-1]), reads=[sk], writes=[u + "hR%d" % tp])
            for j in range(2):
                pst, pk = self.ps[j], self.psk[j]

                def mmf(e, j=j, pst=pst, hc=hc):
                    ins = None
                    for kc in range(KC):
                        ins = e.matmul(pst[:, 0:GT], lhsT=win[:, kc, j * 128:(j + 1) * 128], rhs=hc[:, kc, :], start=(kc == 0), stop=(kc == KC - 1))
                    return ins
                S.op("pe", mmf, reads=[u + "win", hck], writes=[pk])
                S.op("act", lambda e, j=j, pst=pst: e.copy(out=pfT[:, j, :], in_=pst[:, 0:GT]), reads=[pk], writes=[u + "pfT"], partial=True)
            for ti in range(TG):
                t = g * TG + ti
                pst, pk = self.ps[2 + (t % 2)], self.psk[2 + (t % 2)]

                def mmf(e, ti=ti, pst=pst):
                    e.matmul(pst[:, 0:256], lhsT=pfT[:, 0, ti * 128:(ti + 1) * 128], rhs=self.RHSf[:, 0, :], start=True, stop=True)
                    return e.matmul(pst[:, 256:512], lhsT=pfT[:, 1, ti * 128:(ti + 1) * 128], rhs=self.RHSf[:, 1, :], start=True, stop=True)
                S.op("pe", mmf, reads=[u + "pfT", "RHSf"], writes=[pk])
                ab = abt[t % 2]
                abk = u + "ab%d" % (t % 2)
                S.op("dve", lambda e, ab=ab, pst=pst: e.tensor_copy(out=ab, in_=pst), reads=[pk], writes=[abk])
                S.dma("sp", ABd.ap()[t * 128:(t + 1) * 128, :], ab, reads=[abk], writes=["AB" + tag])
            for ti in range(TG):
                t = g * TG + ti
                for part in range(3):
                    cnt += 1
                    pst, pk = self.ps[4 + (cnt % 2)], self.psk[4 + (cnt % 2)]

                    def mmf(e, ti=ti, part=part, pst=pst, hts=(hm, hc, hp)):
                        ins = None
                        n = 0
                        for tap in range(3):
                            for kc in range(KC):
                                lt = (hR[tap] if part == 1 else hts[tap])[:, kc, ti * 128:(ti + 1) * 128]
                                ins = e.matmul(pst[:, 0:384], lhsT=lt, rhs=wtap[:, kc, tap, part * 384:(part + 1) * 384],
                                               start=(n == 0), stop=(n == 23))
                                n += 1
                        return ins
                    S.op("pe", mmf, reads=[hck, hmk, hpk, u + "wtap", u + "hR0", u + "hR1", u + "hR2"], writes=[pk])
                    hq = hqt[cnt % 2]
                    hqk = u + "hq%d" % (cnt % 2)
                    S.op("dve", lambda e, hq=hq, pst=pst, part=part: e.tensor_tensor(out=hq, in0=pst[:, 0:384], in1=cbb[:, part * 384:(part + 1) * 384], op=ALU.add),
                         reads=[pk, u + "cbb"], writes=[hqk])
                    S.dma("sp", Hq.ap()[part, t * 128:(t + 1) * 128, :], hq, reads=[hqk], writes=["Hq" + tag])
            b0 = ((g * GT) % L) // 8
            for m in range(3):
                cnt += 1
                pst, pk = self.ps[cnt % 2], self.psk[cnt % 2]

                def mmf(e, m=m, pst=pst, hc=hc):
                    ins = None
                    for kc in range(KC):
                        ins = e.matmul(pst[:, 0:GT], lhsT=win[:, kc, 256 + m * 128:256 + (m + 1) * 128], rhs=hc[:, kc, :], start=(kc == 0), stop=(kc == KC - 1))
                    return ins
                S.op("pe", mmf, reads=[u + "win", hck], writes=[pk])
                pd = psd[cnt % 2]
                pdk = u + "psd%d" % (cnt % 2)
                S.op("act", lambda e, pd=pd, pst=pst: e.copy(out=pd, in_=pst[:, 0:GT].rearrange("p (b i) -> p i b", i=8)), reads=[pk], writes=[pdk])
                S.dma("sp", Xs.ap()[:, m * 128:(m + 1) * 128, s, b0:b0 + GT // 8].rearrange("i p b -> p i b"), pd, reads=[pdk], writes=["Xs" + tag])
        self.S.barrier()
        A.release(m0)

    def fnet(self, l, L, tag):
        S, A, I = self.S, self.arena, self.I
        u = self.key("fn")
        NT = NS * L
        NTC = L // 128
        FT = min(512, L)
        ABd = self.scr["AB" + tag]
        Ym = self.scr.get("Ym" + tag) or self.dscr("Ym" + tag, [D, NT], BF16)
        m0 = A.mark()
        ABs = A.alloc([NS * NTC, 512], BF16)
        Cf = A.alloc([NTC, FT], BF16)
        Sf = A.alloc([NTC, FT], BF16)
        yf = [A.alloc([FT], BF16) for _ in range(2)]
        S.dma("sp", ABs, ABd.ap().rearrange("(c p) n -> p c n", p=128), reads=["AB" + tag], writes=[u + "ABs"])
        cnt = 0
        for ft in range(L // FT):
            S.dma("sp", Cf, I["dftc%d" % L].ap()[:, ft * FT:(ft + 1) * FT].rearrange("(c p) n -> p c n", p=128), writes=[u + "Cf"], partial=False)
            S.dma("sp", Sf, I["dfts%d" % L].ap()[:, ft * FT:(ft + 1) * FT].rearrange("(c p) n -> p c n", p=128), writes=[u + "Sf"], partial=False)
            for s in range(NS):
                for j in range(2):
                    cnt += 1
                    pst, pk = self.ps[cnt % 2], self.psk[cnt % 2]

                    def mmf(e, s=s, j=j, pst=pst):
                        ins = None
                        for tc in range(NTC):
                            e.matmul(pst[:, 0:FT], lhsT=ABs[:, s * NTC + tc, j * 256:j * 256 + 128], rhs=Cf[:, tc, :], start=(tc == 0), stop=False)
                            ins = e.matmul(pst[:, 0:FT], lhsT=ABs[:, s * NTC + tc, j * 256 + 128:j * 256 + 256], rhs=Sf[:, tc, :], start=False, stop=(tc == NTC - 1))
                        return ins
                    S.op("pe", mmf, reads=[u + "ABs", u + "Cf", u + "Sf"], writes=[pk])
                    y = yf[cnt % 2]
                    yk = u + "yf%d" % (cnt % 2)
                    S.op("act", lambda e, y=y, pst=pst: e.copy(out=y, in_=pst[:, 0:FT]), reads=[pk], writes=[yk])
                    S.dma("sp", Ym.ap()[j * 128:(j + 1) * 128, s * L + ft * FT:s * L + (ft + 1) * FT], y, reads=[yk], writes=["Ym" + tag])
        self.S.barrier()
        A.release(m0)

    def hyena_filters(self, l, L, tag):
        S, A, I = self.S, self.arena, self.I
        u = self.key("hf")
        Gd = self.scr.get("Gd" + tag) or self.dscr("Gd" + tag, [2, 384, 2 * L], BF16)
        CT = min(512, L)
        m0 = A.mark()
        zT = A.alloc([L], F32)
        H1 = A.alloc([L], F32)
        H2n = A.alloc([L], F32)
        H2r = A.alloc([L], F32)
        fw1 = A.alloc([64], F32)
        fw2 = A.alloc([64], F32)
        fw3 = A.alloc([1536], F32)
        vec = A.alloc([4], F32)
        hb = A.alloc([6], F32)
        dec = A.alloc([3, L], F32)
        G = A.alloc([2 * L], F32)
        Gb = A.alloc([2 * L], BF16)
        tf = A.alloc([CT], F32)
        tf2 = A.alloc([CT], F32)
        ti = A.alloc([CT], I32)
        nrm = A.alloc([4], F32)
        S.dma("sp", zT[0:33, :], I["zpos%d" % L].ap(), writes=[u + "zT"])
        S.dma("sp", fw1[0:33, :], I["hy_fw1"].ap()[l], writes=[u + "fw1"])
        S.dma("sp", fw2[0:64, :], I["hy_fw2"].ap()[l], writes=[u + "fw2"])
        S.dma("sp", fw3[0:64, :], I["hy_fw3"].ap()[l], writes=[u + "fw3"])
        for i, nm in enumerate(("hy_fb1", "hy_freq1", "hy_fb2", "hy_freq2")):
            S.dma("sp", vec[0:64, i:i + 1], I[nm].ap()[l].rearrange("(p o) -> p o", o=1), writes=[u + "vec"])
        S.dma("sp", hb, I["hy_bias"].ap()[l].rearrange("o (c p) -> p (o c)", p=128), writes=[u + "hb"], allow_slow_non_contiguous=True)
        S.dma("sp", dec, I["dec%d" % L].ap().rearrange("(c p) n -> p c n", p=128), writes=[u + "dec"])
        for (src, sk, wgt, wk, kdim, dst, dk, vi) in ((zT, u + "zT", fw1, u + "fw1", 33, H1, u + "H1", 0), (H1, u + "H1", fw2, u + "fw2", 64, H2n, u + "H2n", 2)):
            for ct in range(L // CT):
                pst, pk = self.ps[ct % 2], self.psk[ct % 2]
                S.op("pe", lambda e, pst=pst, src=src, wgt=wgt, kdim=kdim, ct=ct: e.matmul(
                    pst[0:64, 0:CT], lhsT=wgt[0:kdim, 0:64], rhs=src[0:kdim, ct * CT:(ct + 1) * CT], start=True, stop=True),
                    reads=[sk, wk], writes=[pk])
                S.op("dve", lambda e, pst=pst, vi=vi: e.tensor_scalar(out=tf[0:64, :], in0=pst[0:64, 0:CT], scalar1=vec[0:64, vi:vi + 1], scalar2=vec[0:64, vi + 1:vi + 2],
                                                            op0=ALU.add, op1=ALU.mult), reads=[pk, u + "vec"], writes=[u + "tf"])
                self.range_reduce_sin("dve", dst[0:64, ct * CT:(ct + 1) * CT], tf[0:64, :], ti[0:64, :], tf2[0:64, :], [u + "tf"], dk, u + "ti", u + "tf2")
        S.op("dve", lambda e: e.tensor_copy(out=H2r[0:64, :], in_=H2n[0:64, ::-1]), reads=[u + "H2n"], writes=[u + "H2r"])
        cnt = 0
        for o in range(2):
            for cc in range(3):
                if o == 0:
                    segs = [(0, L, 0, "r", 0), (L, 2 * L - 1, 1, "n", 1)]
                else:
                    segs = [(0, L - 1, 1, "r", 0), (L - 1, 2 * L - 1, 0, "n", 0)]
                gk = u + "G"
                S.op("pool", lambda e: e.memset(G[:, 2 * L - 1:2 * L], 0.0), writes=[gk])
                for (a0, a1, dr, wh, po) in segs:
                    col = dr * 768 + o * 384 + cc * 128
                    srcH = H2r if wh == "r" else H2n
                    p = a0
                    while p < a1:
                        n = min(CT, a1 - p)
                        sp = p - a0 + po
                        cnt += 1
                        pst, pk = self.ps[cnt % 2], self.psk[cnt % 2]
                        S.op("pe", lambda e, pst=pst, srcH=srcH, col=col, sp=sp, n=n: e.matmul(
                            pst[:, 0:n], lhsT=fw3[0:64, col:col + 128], rhs=srcH[0:64, sp:sp + n], start=True, stop=True),
                            reads=[u + "H2n", u + "H2r", u + "fw3"], writes=[pk])
                        if wh == "r":
                            dsl = dec[:, cc, L - sp - n:L - sp][:, ::-1]
                        else:
                            dsl = dec[:, cc, sp:sp + n]
                        S.op("dve", lambda e, pst=pst, p=p, n=n, dsl=dsl: e.tensor_tensor(out=G[:, p:p + n], in0=pst[:, 0:n], in1=dsl, op=ALU.mult),
                             reads=[pk, u + "dec"], writes=[gk], partial=True)
                        p += n
                S.op("dve", lambda e: e.tensor_reduce(out=nrm[:, 0:1], in_=G, axis=AX.X, op=ALU.add, apply_absolute_value=True), reads=[gk], writes=[u + "nrm"])
                S.op("dve", lambda e: e.reciprocal(out=nrm[:, 1:2], in_=nrm[:, 0:1]), reads=[u + "nrm"], writes=[u + "nrm"])
                S.op("dve", lambda e: e.tensor_scalar(out=G, in0=G, scalar1=nrm[:, 1:2], scalar2=None, op0=ALU.mult), reads=[gk, u + "nrm"], writes=[gk])
                S.op("dve", lambda e, o=o, cc=cc: e.tensor_scalar(out=G[:, L - 1:L], in0=G[:, L - 1:L], scalar1=hb[:, o * 3 + cc:o * 3 + cc + 1], scalar2=None, op0=ALU.add),
                     reads=[gk, u + "hb"], writes=[gk])
                S.op("act", lambda e: e.copy(out=Gb, in_=G), reads=[gk], writes=[u + "Gb"])
                S.dma("sp", Gd.ap()[o, cc * 128:(cc + 1) * 128, :], Gb, reads=[u + "Gb"], writes=["Gd" + tag])
        self.S.barrier()
        A.release(m0)

    def hyena(self, l, L, tag):
        S, A, I = self.S, self.arena, self.I
        u = self.key("hy")
        NB = L // 128
        NC = NS * NB
        W = 128 * (2 * NB - 1)
        NT = NS * L
        Hq = self.scr["Hq" + tag]
        Gd = self.scr["Gd" + tag]
        Ym = self.scr.get("Ym" + tag) or self.dscr("Ym" + tag, [D, NT], BF16)
        HC = 192
        CG = max(1, min(8, 512 // NC))
        m0 = A.mark()
        Z2 = A.alloc([NC, 384], BF16)
        Ub = A.alloc([NC, HC], BF16)
        Xb = A.alloc([NC, HC], BF16)
        Z1 = A.alloc([NC, HC], BF16)
        Rb = [A.alloc([W], BF16) for _ in range(2)]
        yT = [A.alloc([3, 128], BF16) for _ in range(2)]
        rc = 0
        bc = 0
        dl = [0] + [d for d in range(-(NB - 1), NB) if d != 0]
        for half in range(2):
            h0 = half * HC
            for o in range(2):
                if o == 0:
                    S.dma("sp", Ub, Hq.ap()[0, :, h0:h0 + HC].rearrange("(c p) n -> p c n", p=128), reads=["Hq" + tag], writes=[u + "Ub"], partial=False)
                    S.dma("sp", Xb, Hq.ap()[1, :, h0:h0 + HC].rearrange("(c p) n -> p c n", p=128), reads=["Hq" + tag], writes=[u + "Xb"], partial=False)
                    src, srck, dst, dstk = Ub, u + "Ub", Z1, u + "Z1"
                else:
                    S.dma("sp", Xb, Hq.ap()[2, :, h0:h0 + HC].rearrange("(c p) n -> p c n", p=128), reads=["Hq" + tag], writes=[u + "Xb"], partial=False)
                    src, srck, dst, dstk = Z1, u + "Z1", Z2, u + "Z2"
                srcv = src.rearrange("p (s b) n -> p s b n", s=NS)
                for c8 in range(HC // CG):
                    bc += 1
                    pst, pk = self.ps[bc % 4], self.psk[bc % 4]
                    for ci in range(CG):
                        cl = c8 * CG + ci
                        c = h0 + cl
                        rc += 1
                        Rt = Rb[rc % 2]
                        rk = u + "R%d" % (rc % 2)
                        S.dma("sp", Rt, bass.AP(Gd, (o * 384 + c) * 2 * L, [[1, 128], [1, W]]), reads=["Gd" + tag], writes=[rk], partial=False)
                        pv = pst[:, ci * NC:(ci + 1) * NC].rearrange("p (s b) -> p s b", s=NS)

                        def mmf(e, Rt=Rt, pv=pv, srcv=srcv, cl=cl, o=o):
                            ins = None
                            for k, d in enumerate(dl):
                                i0 = max(0, d)
                                i1 = min(NB - 1, NB - 1 + d)
                                nb = i1 - i0 + 1
                                j0 = i0 - d
                                xd = 128 * (NB - 1 - d) if o == 0 else 128 * (d + NB - 1)
                                ins = e.matmul(pv[:, :, i0:i0 + nb], lhsT=Rt[:, xd:xd + 128], rhs=srcv[:, :, j0:j0 + nb, cl],
                                               start=(k == 0), stop=(k == len(dl) - 1))
                            return ins
                        S.op("pe", mmf, reads=[rk, srck], writes=[pk], partial=True)
                    cl0 = c8 * CG
                    oc0 = cl0 if o == 0 else h0 + cl0
                    S.op("dve", lambda e, pst=pst, dst=dst, cl0=cl0, oc0=oc0: e.tensor_tensor(
                        out=dst[:, :, oc0:oc0 + CG], in0=pst[:, 0:CG * NC].rearrange("p (c n) -> p n c", c=CG),
                        in1=Xb[:, :, cl0:cl0 + CG], op=ALU.mult), reads=[pk, u + "Xb"], writes=[dstk], partial=True)
        for col in range(NC):
            pst, pk = self.ps[4 + col % 2], self.psk[4 + col % 2]
            pb = pst.bitcast(BF16)

            def trf(e, col=col, pb=pb):
                ins = None
                for cc in range(3):
                    ins = e.transpose(pb[:, cc * 128:(cc + 1) * 128], Z2[:, col, cc * 128:(cc + 1) * 128], self.identb)
                return ins
            S.op("pe", trf, reads=[u + "Z2", "identb"], writes=[pk])
            y = yT[col % 2]
            yk = u + "yT%d" % (col % 2)
            S.op("act", lambda e, y=y, pb=pb: e.copy(out=y, in_=pb[:, 0:384].rearrange("p (c t) -> p c t", c=3)), reads=[pk], writes=[yk])
            S.dma("sp", Ym.ap()[256:640, col * 128:(col + 1) * 128].rearrange("(c p) t -> p c t", p=128), y, reads=[yk], writes=["Ym" + tag])
        self.S.barrier()
        A.release(m0)

    def s5_prep(self, l):
        S, A, I = self.S, self.arena, self.I
        u = self.key("sp")
        P = {}
        P["pr"] = A.alloc([24, 26], F32)
        P["pi"] = A.alloc([24, 26], F32)
        P["npi"] = A.alloc([24, 26], F32)
        P["WBT"] = A.alloc([48, 128], BF16)
        P["Mpad"] = A.alloc([24, 8, 32], BF16)
        P["Gpad"] = A.alloc([48, 8, 32], BF16)
        P["gluw"] = A.alloc([4, 384], BF16)
        P["glub"] = A.alloc([4], F32)
        P["u"] = u
        m0 = A.mark()
        lr = A.alloc([24], F32); li = A.alloc([24], F32); dt = A.alloc([24], F32)
        Bre = A.alloc([24, 16], F32); Bim = A.alloc([24, 16], F32)
        Cre = A.alloc([24, 16], F32); Cim = A.alloc([24, 16], F32)
        pwc = A.alloc([26], F32)
        EX = A.alloc([24, 26], F32); ANG = A.alloc([24, 26], F32)
        tI = A.alloc([24, 26], I32); tF = A.alloc([24, 26], F32)
        t1 = A.alloc([24], F32); t2 = A.alloc([24], F32); cr = A.alloc([24], F32); ci = A.alloc([24], F32)
        bbr = A.alloc([24, 16], F32); bbi = A.alloc([24, 16], F32)
        T1 = A.alloc([12, 8, 16], F32); T2 = A.alloc([12, 8, 16], F32)
        Wn = A.alloc([48, 128], F32)
        Pp = A.alloc([48, 128], BF16)
        Qq = A.alloc([48, 128], BF16)
        mle = A.alloc([128], F32); mge = A.alloc([128], F32)
        dv = A.alloc([24], F32)
        Mf = A.alloc([128], F32)
        S.dma("sp", lr, I["s5_a_re"].ap()[l].rearrange("d (q m) s -> (m s) (d q)", m=2), writes=[u + "lr"], allow_slow_non_contiguous=True)
        S.dma("sp", li, I["s5_a_im"].ap()[l].rearrange("d (q m) s -> (m s) (d q)", m=2), writes=[u + "li"], allow_slow_non_contiguous=True)
        for m in range(2):
            S.dma("sp", dt[m * 64:(m + 1) * 64, :], I["s5_log_dt"].ap()[l].rearrange("d (q m) -> m (d q)", m=2)[m].partition_broadcast(64),
                  writes=[u + "dt"], allow_slow_non_contiguous=True)
        Csrc = A.alloc([6, 128], F32)
        CT_ = A.alloc([768], F32)
        for (dst, nm, kk) in ((Cre, "s5_c_re", u + "Cre"), (Cim, "s5_c_im", u + "Cim")):
            for hh in range(2):
                S.dma("sp", Csrc[:, :, hh * 64:(hh + 1) * 64], I[nm].ap()[l].rearrange("d g o s -> (d g o) s").rearrange("(k p) s -> p k s", p=128),
                      writes=[u + "Csrc"])
            for k in range(6):
                pst, pk = self.ps[k % 2], self.psk[k % 2]
                S.op("pe", lambda e, k=k, pst=pst: e.transpose(pst[:, 0:128], Csrc[:, k, :], self.ident), reads=[u + "Csrc", "ident"], writes=[pk])
                S.op("act", lambda e, k=k, pst=pst: e.copy(out=CT_[:, k * 128:(k + 1) * 128], in_=pst[:, 0:128]), reads=[pk], writes=[u + "CT"], partial=True)
            CTv = CT_.rearrange("p (d q m o) -> p d q m o", d=2, q=12, m=2)
            for m in range(2):
                S.op("dve", lambda e, m=m, dst=dst, CTv=CTv: e.tensor_copy(out=dst[m * 64:(m + 1) * 64].rearrange("p (d q) o -> p d q o", d=2), in_=CTv[m * 64:(m + 1) * 64, :, :, m, :]),
                     reads=[u + "CT"], writes=[kk], partial=True)
        S.dma("sp", Bre, I["s5_b_re"].ap()[l].rearrange("d (q m) s c -> (m s) (d q) c", m=2), writes=[u + "Bre"])
        S.dma("sp", Bim, I["s5_b_im"].ap()[l].rearrange("d (q m) s c -> (m s) (d q) c", m=2), writes=[u + "Bim"])
        S.dma("sp", pwc, I["s5pw"].ap(), writes=[u + "pwc"])
        S.dma("sp", mle, I["mask_le"].ap(), writes=[u + "mle"])
        S.dma("sp", mge, I["mask_ge"].ap(), writes=[u + "mge"])
        for i in range(8):
            S.dma("sp", dv[i * 16:(i + 1) * 16, :], I["s5_d"].ap()[l].rearrange("(g c) -> c g", c=16), writes=[u + "dv"], allow_slow_non_contiguous=True)
        S.dma("pool", P["gluw"][0:96], I["s5_glu_w"].ap()[l].rearrange("(c p) n -> p c n", p=96), writes=[u + "gluw"])
        S.dma("sp", P["glub"][0:96], I["s5_glu_b"].ap()[l].rearrange("(c p) -> p c", p=96), writes=[u + "glub"], allow_slow_non_contiguous=True)
        V = lambda e: e
        dve = lambda fn, r, w, **k: S.op("dve", fn, reads=r, writes=w, **k)
        dve(lambda e: e.tensor_copy(out=dt, in_=dt), [u + "dt"], [u + "dt"])
        S.op("act", lambda e: e.activation(out=dt, in_=dt, func=AF.Exp), reads=[u + "dt"], writes=[u + "dt"])
        dve(lambda e: e.tensor_tensor(out=t1, in0=lr, in1=dt, op=ALU.mult), [u + "lr", u + "dt"], [u + "t1"])
        dve(lambda e: e.tensor_tensor(out=t2, in0=li, in1=dt, op=ALU.mult), [u + "li", u + "dt"], [u + "t2"])
        bc3 = lambda a: a.unsqueeze(2).to_broadcast([128, 24, 26])
        pw3 = pwc.unsqueeze(1).to_broadcast([128, 24, 26])
        dve(lambda e: e.tensor_tensor(out=EX, in0=bc3(t1), in1=pw3, op=ALU.mult), [u + "t1", u + "pwc"], [u + "EX"])
        dve(lambda e: e.tensor_tensor(out=ANG, in0=bc3(t2), in1=pw3, op=ALU.mult), [u + "t2", u + "pwc"], [u + "ANG"])
        S.op("act", lambda e: e.activation(out=EX, in_=EX, func=AF.Exp), reads=[u + "EX"], writes=[u + "EX"])
        self.range_reduce_sin("dve", P["pi"], ANG, tI, tF, [u + "ANG"], u + "pi", u + "tI", u + "tF")
        self.range_reduce_sin("dve", P["pr"], ANG, tI, tF, [u + "ANG"], u + "pr", u + "tI", u + "tF", shift=math.pi / 2)
        dve(lambda e: e.tensor_tensor(out=P["pi"], in0=P["pi"], in1=EX, op=ALU.mult), [u + "pi", u + "EX"], [u + "pi"])
        dve(lambda e: e.tensor_tensor(out=P["pr"], in0=P["pr"], in1=EX, op=ALU.mult), [u + "pr", u + "EX"], [u + "pr"])
        dve(lambda e: e.tensor_scalar(out=P["npi"], in0=P["pi"], scalar1=-1.0, scalar2=None, op0=ALU.mult), [u + "pi"], [u + "npi"])
        pk_ = [u + "pr", u + "pi", u + "npi"]
        i1 = PWI(1)
        ar = P["pr"][:, :, i1]; ai = P["pi"][:, :, i1]
        dve(lambda e: e.tensor_tensor(out=t1, in0=lr, in1=lr, op=ALU.mult), [u + "lr", u + "EX"], [u + "t1"])
        dve(lambda e: e.tensor_tensor(out=t2, in0=li, in1=li, op=ALU.mult), [u + "li", u + "ANG"], [u + "t2"])
        dve(lambda e: e.tensor_tensor(out=t1, in0=t1, in1=t2, op=ALU.add), [u + "t1", u + "t2"], [u + "t1"])
        dve(lambda e: e.reciprocal(out=t1, in_=t1), [u + "t1"], [u + "t1"])
        dve(lambda e: e.tensor_scalar(out=t2, in0=ar, scalar1=-1.0, scalar2=None, op0=ALU.add), pk_, [u + "t2"])
        dve(lambda e: e.tensor_tensor(out=cr, in0=t2, in1=lr, op=ALU.mult), [u + "t2", u + "lr"], [u + "cr"])
        dve(lambda e: e.tensor_tensor(out=ci, in0=ai, in1=li, op=ALU.mult), pk_ + [u + "li"], [u + "ci"])
        dve(lambda e: e.tensor_tensor(out=cr, in0=cr, in1=ci, op=ALU.add), [u + "cr", u + "ci"], [u + "cr"])
        dve(lambda e: e.tensor_tensor(out=cr, in0=cr, in1=t1, op=ALU.mult), [u + "cr", u + "t1"], [u + "cr"])
        dve(lambda e: e.tensor_tensor(out=ci, in0=ai, in1=lr, op=ALU.mult), pk_ + [u + "lr", u + "cr"], [u + "ci"])
        dve(lambda e: e.tensor_tensor(out=t2, in0=t2, in1=li, op=ALU.mult), [u + "t2", u + "li"], [u + "t2"])
        dve(lambda e: e.tensor_tensor(out=ci, in0=ci, in1=t2, op=ALU.subtract), [u + "ci", u + "t2"], [u + "ci"])
        dve(lambda e: e.tensor_tensor(out=ci, in0=ci, in1=t1, op=ALU.mult), [u + "ci", u + "t1"], [u + "ci"])
        b16 = lambda a: a.unsqueeze(2).to_broadcast([128, 24, 16])
        dve(lambda e: e.tensor_scalar(out=Cim, in0=Cim, scalar1=-1.0, scalar2=None, op0=ALU.mult), [u + "Cim"], [u + "Cim"])

        def cmul(outr, outi, xr_, xi_, yr_, yi_, rk, wkr, wki, ta, tb, shape_sel=None, neg_im=False):
            dve(lambda e: e.tensor_tensor(out=ta, in0=xr_, in1=yr_, op=ALU.mult), rk, [u + "ta"])
            dve(lambda e: e.tensor_tensor(out=tb, in0=xi_, in1=yi_, op=ALU.mult), rk, [u + "tb"])
            dve(lambda e: e.tensor_tensor(out=outr, in0=ta, in1=tb, op=(ALU.add if neg_im else ALU.subtract)), [u + "ta", u + "tb"], [wkr], partial=True)
            dve(lambda e: e.tensor_tensor(out=ta, in0=xr_, in1=yi_, op=ALU.mult), rk + [wkr], [u + "ta"])
            dve(lambda e: e.tensor_tensor(out=tb, in0=xi_, in1=yr_, op=ALU.mult), rk + [wkr], [u + "tb"])
            dve(lambda e: e.tensor_tensor(out=outi, in0=ta, in1=tb, op=(ALU.subtract if neg_im else ALU.add)), [u + "ta", u + "tb"], [wki], partial=True)
        ta24 = T1.rearrange("p a b c -> p (a b c)")[:, 0:384].rearrange("p (a c) -> p a c", a=24)
        tb24 = T2.rearrange("p a b c -> p (a b c)")[:, 0:384].rearrange("p (a c) -> p a c", a=24)
        cmul(bbr, bbi, b16(cr), b16(ci), Bre, Bim, [u + "cr", u + "ci", u + "Bre", u + "Bim"], u + "bbr", u + "bbi", ta24, tb24)
        def pslice(arr, d, n0, step):
            i0 = PWI(n0)
            if step > 0:
                sl = arr[:, d * 12:(d + 1) * 12, i0:i0 + 8]
            else:
                sl = arr[:, d * 12:(d + 1) * 12, i0 - 7:i0 + 1][:, :, ::-1]
            return sl.unsqueeze(3).to_broadcast([128, 12, 8, 16])
        v16 = lambda a, d: a[:, d * 12:(d + 1) * 12, :].unsqueeze(2).to_broadcast([128, 12, 8, 16])
        Wnv = Wn.rearrange("p (d q r) (i c) -> p d q r i c", d=2, q=12, r=2, i=8)
        Ppv = Pp.rearrange("p (d q r) (i c) -> p d q r i c", d=2, q=12, r=2, i=8)
        Qqv = Qq.rearrange("p (d q r) (i c) -> p d q r i c", d=2, q=12, r=2, i=8)
        bk = [u + "bbr", u + "bbi"] + pk_
        ck = [u + "Cre", u + "Cim"] + pk_
        for d in range(2):
            n0, stp = ((7, -1), (0, 1))[d]
            cmul(Wnv[:, d, :, 0], Wnv[:, d, :, 1], pslice(P["pr"], d, n0, stp), pslice(P["pi"], d, n0, stp), v16(bbr, d), v16(bbi, d),
                 bk, u + "Wn", u + "Wn", T1, T2)
            n0, stp = ((0, -1), (0, 1))[d]
            cmul(Ppv[:, d, :, 0], Ppv[:, d, :, 1], pslice(P["pr"], d, n0, stp), pslice(P["pi"], d, n0, stp), v16(bbr, d), v16(bbi, d),
                 bk, u + "Pp", u + "Pp", T1, T2)
            n0, stp = ((0, 1), (0, -1))[d]
            cmul(Qqv[:, d, :, 0], Qqv[:, d, :, 1], pslice(P["pr"], d, n0, stp), pslice(P["pi"], d, n0, stp), v16(Cre, d), v16(Cim, d),
                 ck, u + "Qq", u + "Qq", T1, T2, neg_im=True)
        S.op("pool", lambda e: e.memset(P["Gpad"], 0.0), writes=[u + "Gpad"])
        Gv = P["Gpad"].rearrange("p (d q r) j n -> p d q r j n", d=2, q=12, r=2)
        for d in range(2):
            n0, stp = ((1, 1), (8, -1))[d]
            for m in range(2):
                hs = slice(m * 64, (m + 1) * 64)
                h = lambda a: a[hs]
                cmul(Gv[hs, d, :, 0, :, m * 16:(m + 1) * 16], Gv[hs, d, :, 1, :, m * 16:(m + 1) * 16],
                     h(pslice(P["pr"], d, n0, stp)), h(pslice(P["pi"], d, n0, stp)), h(v16(Cre, d)), h(v16(Cim, d)),
                     ck + [u + "Gpad"], u + "Gpad", u + "Gpad", T1[hs], T2[hs], neg_im=True)
        for k in range(48):
            pst, pk = self.ps[k % 2], self.psk[k % 2]
            S.op("pe", lambda e, k=k, pst=pst: e.transpose(pst[:, 0:128], Wn[:, k, :], self.ident), reads=[u + "Wn", "ident"], writes=[pk])
            S.op("act", lambda e, k=k, pst=pst: e.copy(out=P["WBT"][:, k, :], in_=pst[:, 0:128]), reads=[pk], writes=[u + "WBT"], partial=True)
        S.op("pool", lambda e: e.memset(P["Mpad"], 0.0), writes=[u + "Mpad"])
        for g in range(24):
            q, m = g // 2, g % 2
            hs = slice(m * 64, (m + 1) * 64)
            psf, pkf = self.ps[2 + (g % 2) * 2], self.psk[2 + (g % 2) * 2]
            psb, pkb = self.ps[3 + (g % 2) * 2], self.psk[3 + (g % 2) * 2]
            for d, pst, pk in ((0, psf, pkf), (1, psb, pkb)):
                kre = (d * 12 + q) * 2
                def mmf(e, pst=pst, kre=kre, hs=hs):
                    e.matmul(pst[:, 0:128], lhsT=Pp[hs, kre, :], rhs=Qq[hs, kre, :], start=True, stop=False)
                    return e.matmul(pst[:, 0:128], lhsT=Pp[hs, kre + 1, :], rhs=Qq[hs, kre + 1, :], start=False, stop=True)
                S.op("pe", mmf, reads=[u + "Pp", u + "Qq"], writes=[pk])
            dve(lambda e, psf=psf: e.tensor_tensor(out=Mf, in0=psf[:, 0:128], in1=mle, op=ALU.mult), [pkf, u + "mle"], [u + "Mf"])
            dve(lambda e, psb=psb: e.tensor_tensor(out=T1.rearrange("p a b c -> p (a b c)")[:, 0:128], in0=psb[:, 0:128], in1=mge, op=ALU.mult), [pkb, u + "mge"], [u + "ta"])
            dve(lambda e: e.tensor_tensor(out=Mf, in0=Mf, in1=T1.rearrange("p a b c -> p (a b c)")[:, 0:128], op=ALU.add), [u + "Mf", u + "ta"], [u + "Mf"])
            dve(lambda e, g=g: e.scalar_tensor_tensor(out=Mf, in0=self.ident, scalar=dv[:, g:g + 1], in1=Mf, op0=ALU.mult, op1=ALU.add), [u + "Mf", u + "dv", "ident"], [u + "Mf"])
            dve(lambda e, g=g, m=m: e.tensor_copy(out=P["Mpad"][:, g, :, m * 16:(m + 1) * 16], in_=Mf.rearrange("p (j o) -> p j o", j=8)), [u + "Mf"], [u + "Mpad"], partial=True)
        self.S.barrier()
        A.release(m0)
        return P

    def s5_main(self, l, L, tag, P, use_s0, readout):
        S, A, I = self.S, self.arena, self.I
        u = self.key("s5")
        pu = P["u"]
        NBk = L // 8
        E = NBk + 1
        NT = NS * L
        Xs = self.scr["Xs" + tag]
        Ym = self.scr.get("Ym" + tag) or self.dscr("Ym" + tag, [D, NT], BF16)
        nlev = 0
        while (1 << nlev) < E:
            nlev += 1
        if nlev % 2:
            nlev += 1
        m0 = A.mark()
        eA = A.alloc([4, NS, E], F32)
        eB = A.alloc([4, NS, E], F32)
        ebf = A.alloc([4, NS, E], BF16)
        Xg = [A.alloc([NS * NBk], BF16) for _ in range(2)]
        if readout:
            YS = A.alloc([NS * L], F32)
            PC = min(2048, NS * L)
            tq = A.alloc([PC], F32)
            ygp = [A.alloc([PC], BF16) for _ in range(2)]
            Ygd = self.scr.get("Yg" + tag) or self.dscr("Yg" + tag, [384, NT], BF16)
        pk_ = [pu + "pr", pu + "pi", pu + "npi"]
        bc = 0
        for q in range(12):
            for m in range(2):
                g = 2 * q + m
                for i in range(8):
                    S.dma("sp", Xg[m][i * 16:(i + 1) * 16, :], Xs.ap()[i, g * 16:(g + 1) * 16].rearrange("c s b -> c (s b)"),
                          reads=["Xs" + tag], writes=[u + "Xg%d" % m])
            for d in range(2):
                col = 0 if d == 0 else NBk
                for ri in range(2):
                    k = d * 2 + ri
                    if use_s0:
                        S.op("act", lambda e, k=k, col=col, d=d, ri=ri, q=q: e.copy(out=eA[:, k, :, col], in_=self.s0[:, (d * 12 + q) * 2 + ri, :]),
                             reads=["s0"], writes=[u + "eA%d" % d], partial=True)
                    else:
                        S.op("pool", lambda e, k=k, col=col: e.memset(eA[:, k, :, col], 0.0), writes=[u + "eA%d" % d], partial=True)
            for d in range(2):
                off = 1 if d == 0 else 0
                for ri in range(2):
                    for sq in range(NS):
                        bc += 1
                        pst, pk = self.ps[bc % 2], self.psk[bc % 2]
                        kk = (d * 12 + q) * 2 + ri

                        def mmf(e, pst=pst, kk=kk, sq=sq):
                            e.matmul(pst[0:64, 0:NBk], lhsT=P["WBT"][:, kk, 0:64], rhs=Xg[0][:, sq * NBk:(sq + 1) * NBk], start=True, stop=True)
                            return e.matmul(pst[64:128, 0:NBk], lhsT=P["WBT"][:, kk, 64:128], rhs=Xg[1][:, sq * NBk:(sq + 1) * NBk], start=True, stop=True)
                        S.op("pe", mmf, reads=[pu + "WBT", u + "Xg0", u + "Xg1"], writes=[pk])
                        S.op("act", lambda e, pst=pst, d=d, ri=ri, sq=sq, off=off: e.copy(out=eA[:, d * 2 + ri, sq, off:off + NBk], in_=pst[:, 0:NBk]),
                             reads=[pk], writes=[u + "eA%d" % d], partial=True)
            for d, eng in ((0, "dve"), (1, "dve")):
                cur, nxt = eA, eB
                ck_, nk_ = u + "eA%d" % d, u + "eB%d" % d
                for lev in range(nlev):
                    dist = 1 << lev
                    ix = PWI(8 * dist)
                    ar = P["pr"][:, d * 12 + q, ix:ix + 1]
                    ai = P["pi"][:, d * 12 + q, ix:ix + 1]
                    nai = P["npi"][:, d * 12 + q, ix:ix + 1]
                    cr_, ci_ = cur[:, d * 2 + 0], cur[:, d * 2 + 1]
                    nr_, ni_ = nxt[:, d * 2 + 0], nxt[:, d * 2 + 1]
                    if dist < E:
                        n = E - dist
                        if d == 0:
                            srcs, dsts = slice(0, n), slice(dist, E)
                            keep = slice(0, dist)
                        else:
                            srcs, dsts = slice(dist, E), slice(0, n)
                            keep = slice(n, E)
                        S.op(eng, lambda e, cr_=cr_, nr_=nr_, ar=ar, srcs=srcs, dsts=dsts: e.scalar_tensor_tensor(
                            out=nr_[:, :, dsts], in0=cr_[:, :, srcs], scalar=ar, in1=cr_[:, :, dsts], op0=ALU.mult, op1=ALU.add),
                            reads=[ck_] + pk_, writes=[nk_], partial=True)
                        S.op(eng, lambda e, ci_=ci_, nr_=nr_, nai=nai, srcs=srcs, dsts=dsts: e.scalar_tensor_tensor(
                            out=nr_[:, :, dsts], in0=ci_[:, :, srcs], scalar=nai, in1=nr_[:, :, dsts], op0=ALU.mult, op1=ALU.add),
                            reads=[ck_, nk_] + pk_, writes=[nk_], partial=True)
                        S.op(eng, lambda e, ci_=ci_, ni_=ni_, ar=ar, srcs=srcs, dsts=dsts: e.scalar_tensor_tensor(
                            out=ni_[:, :, dsts], in0=ci_[:, :, srcs], scalar=ar, in1=ci_[:, :, dsts], op0=ALU.mult, op1=ALU.add),
                            reads=[ck_] + pk_, writes=[nk_], partial=True)
                        S.op(eng, lambda e, cr_=cr_, ni_=ni_, ai=ai, srcs=srcs, dsts=dsts: e.scalar_tensor_tensor(
                            out=ni_[:, :, dsts], in0=cr_[:, :, srcs], scalar=ai, in1=ni_[:, :, dsts], op0=ALU.mult, op1=ALU.add),
                            reads=[ck_, nk_] + pk_, writes=[nk_], partial=True)
                    else:
                        keep = slice(0, E)
                    S.op("act", lambda e, cur=cur, nxt=nxt, d=d, keep=keep: e.copy(out=nxt[:, d * 2:d * 2 + 2, :, keep], in_=cur[:, d * 2:d * 2 + 2, :, keep]),
                         reads=[ck_], writes=[nk_], partial=True)
                    cur, nxt = nxt, cur
                    ck_, nk_ = nk_, ck_
            if not readout or True:
                for d in range(2):
                    col = NBk if d == 0 else 0
                    for ri in range(2):
                        S.op("act", lambda e, d=d, ri=ri, col=col, q=q: e.copy(out=self.s0n[:, (d * 12 + q) * 2 + ri, :], in_=eA[:, d * 2 + ri, :, col]),
                             reads=[u + "eA%d" % d], writes=["s0n"], partial=True)
            if not readout:
                continue
            S.op("act", lambda e: e.copy(out=ebf, in_=eA), reads=[u + "eA0", u + "eA1"], writes=[u + "ebf"])
            qq = q % 3
            gc = q // 3
            YSv = YS.rearrange("p (s b j) -> p s b j", s=NS, j=8)
            for j in range(8):
                for sq in range(NS):
                    bc += 1
                    pst, pk = self.ps[2 + bc % 4], self.psk[2 + bc % 4]

                    def mmf(e, pst=pst, j=j, sq=sq, q=q, qq=qq):
                        o = pst[qq * 32:(qq + 1) * 32, 0:NBk]
                        e.matmul(o, lhsT=P["Mpad"][:, 2 * q, j, :], rhs=Xg[0][:, sq * NBk:(sq + 1) * NBk], start=True, stop=False)
                        e.matmul(o, lhsT=P["Mpad"][:, 2 * q + 1, j, :], rhs=Xg[1][:, sq * NBk:(sq + 1) * NBk], start=False, stop=False)
                        ins = None
                        for d in range(2):
                            c0 = 0 if d == 0 else 1
                            for ri in range(2):
                                ins = e.matmul(o, lhsT=P["Gpad"][:, (d * 12 + q) * 2 + ri, j, :], rhs=ebf[:, d * 2 + ri, sq, c0:c0 + NBk],
                                               start=False, stop=(d == 1 and ri == 1))
                        return ins
                    S.op("pe", mmf, reads=[pu + "Mpad", pu + "Gpad", u + "Xg0", u + "Xg1", u + "ebf"], writes=[pk])
                    S.op("dve", lambda e, pst=pst, j=j, sq=sq, qq=qq: e.tensor_copy(out=YSv[qq * 32:(qq + 1) * 32, sq, :, j], in_=pst[qq * 32:(qq + 1) * 32, 0:NBk]),
                         reads=[pk], writes=[u + "YS"], partial=True)
            if qq == 2:
                for pc in range(NS * L // PC):
                    cs = slice(pc * PC, (pc + 1) * PC)
                    yp, ypk = ygp[pc % 2], u + "ygp%d" % (pc % 2)
                    S.op("act", lambda e, cs=cs: e.activation(out=tq[0:96], in_=YS[0:96, cs], func=AF.Square), reads=[u + "YS"], writes=[u + "tq"])
                    S.op("dve", lambda e: e.tensor_scalar(out=tq[0:96], in0=tq[0:96], scalar1=0.044715, scalar2=1.0, op0=ALU.mult, op1=ALU.add), reads=[u + "tq"], writes=[u + "tq"])
                    S.op("dve", lambda e, cs=cs: e.tensor_tensor(out=tq[0:96], in0=tq[0:96], in1=YS[0:96, cs], op=ALU.mult), reads=[u + "tq", u + "YS"], writes=[u + "tq"])
                    S.op("act", lambda e: e.activation(out=tq[0:96], in_=tq[0:96], func=AF.Sigmoid, scale=1.5957691216057308), reads=[u + "tq"], writes=[u + "tq"])
                    S.op("dve", lambda e, cs=cs, yp=yp: e.tensor_tensor(out=yp[0:96], in0=tq[0:96], in1=YS[0:96, cs], op=ALU.mult), reads=[u + "tq", u + "YS"], writes=[ypk])
                    S.dma("sp", Ygd.ap()[gc * 96:(gc + 1) * 96, cs], yp[0:96], reads=[ypk], writes=["Yg" + tag])
        if readout:
            TT = min(512, NT)
            yo = [A.alloc([TT], BF16) for _ in range(2)]
            sg = [A.alloc([TT], F32) for _ in range(2)]
            ygt = [A.alloc([4, TT], BF16) for _ in range(2)]
            for tt in range(NT // TT):
                yt, ytk = ygt[tt % 2], u + "ygt%d" % (tt % 2)
                S.dma("sp", yt[0:96], Ygd.ap()[:, tt * TT:(tt + 1) * TT].rearrange("(c p) t -> p c t", p=96), reads=["Yg" + tag], writes=[ytk], partial=False)
                for mo in range(4):
                    bc += 1
                    pst, pk = self.ps[bc % 2], self.psk[bc % 2]

                    def mmf(e, pst=pst, yt=yt, mo=mo):
                        ins = None
                        for cc in range(4):
                            ins = e.matmul(pst[0:96, 0:TT], lhsT=P["gluw"][0:96, cc, mo * 96:(mo + 1) * 96], rhs=yt[0:96, cc, :], start=(cc == 0), stop=(cc == 3))
                        return ins
                    S.op("pe", mmf, reads=[pu + "gluw", ytk], writes=[pk])
                    sgt, sk = sg[bc % 2], u + "sg%d" % (bc % 2)
                    S.op("act", lambda e, pst=pst, sgt=sgt, mo=mo: e.activation(out=sgt[0:96], in_=pst[0:96, 0:TT], func=AF.Sigmoid, bias=P["glub"][0:96, mo:mo + 1], scale=1.0),
                         reads=[pk, pu + "glub"], writes=[sk])
                    yot, yk = yo[bc % 2], u + "yo%d" % (bc % 2)
                    S.op("dve", lambda e, yot=yot, sgt=sgt, mo=mo, yt=yt: e.tensor_tensor(out=yot[0:96], in0=sgt[0:96], in1=yt[0:96, mo, :], op=ALU.mult),
                         reads=[sk, ytk], writes=[yk])
                    S.dma("sp", Ym.ap()[640 + mo * 96:640 + (mo + 1) * 96, tt * TT:(tt + 1) * TT], yot[0:96], reads=[yk], writes=["Ym" + tag])
        self.S.barrier()
        A.release(m0)

    def cast_weights(self, name, src_ap_2d, rows, cols):
        t = self.dscr(name, [rows, cols], BF16)
        step = 512
        for r0 in range(0, rows, step):
            r1 = min(rows, r0 + step)
            self.S.dma("pool", t.ap()[r0:r1, :], src_ap_2d[r0:r1, :], writes=[name])
        return t

    def cast_moe(self, l):
        I = self.I
        experts = []
        for e in range(self.ne_cast):
            experts.append((self.cast_weights("wg%d_%d" % (l, e), I["moe_w_gate"].ap()[0, e], D, DFFE),
                            self.cast_weights("wu%d_%d" % (l, e), I["moe_w_up"].ap()[0, e], D, DFFE),
                            self.cast_weights("wd%d_%d" % (l, e), I["moe_w_down"].ap()[0, e], DFFE, D), e))
        self._moe_experts = experts

    def phaseC(self, l, Xsrc, Xdst, L, tag, bl, moe, final):
        S, A, I = self.S, self.arena, self.I
        u = self.key("pC")
        NT = NS * L
        GT = min(512, L)
        TG = GT // 128
        Ym = self.scr["Ym" + tag]
        md = self.scr["modD%d" % l]
        if moe:
            if self._moe_experts is None:
                self.cast_moe(l)
            experts = self._moe_experts[:self.ne_comp]
            dff, FBW = DFFE, 512
        else:
            if ("wgd%d" % l) not in self.scr:
                self.cast_weights("wgd%d" % l, I["ffn_w_gate"].ap()[l // 2], D, DFF)
                self.cast_weights("wud%d" % l, I["ffn_w_up"].ap()[l // 2], D, DFF)
                self.cast_weights("wdd%d" % l, I["ffn_w_down"].ap()[l // 2], DFF, D)
            experts = [(self.scr["wgd%d" % l], self.scr["wud%d" % l], self.scr["wdd%d" % l], None)]
            dff, FBW = DFF, 256
        NCH = dff // 128
        FB = FBW // 128
        NBLK = dff // FBW
        DB = 4 if moe else 11
        NWD = 4 if moe else 2
        m0 = A.mark()
        self._nt_ss = A.alloc([4], F32)
        self._nt_junk = A.alloc([D], F32)
        wout = A.alloc([KC, D], BF16)
        grow = A.alloc([len(bl), 2, D], F32)
        S.dma("pool", wout, I["w_out"].ap()[l].rearrange("(kc p) n -> p kc n", p=128), writes=[u + "wout"])
        for bi, b in enumerate(bl):
            S.dma("sp", grow[:, bi, 0, :], md.ap()[b, 2 * D:3 * D].partition_broadcast(128), reads=["modD%d" % l], writes=[u + "grow"])
            S.dma("sp", grow[:, bi, 1, :], md.ap()[b, 5 * D:6 * D].partition_broadcast(128), reads=["modD%d" % l], writes=[u + "grow"])
        if final:
            fg = A.alloc([D], F32)
            S.dma("sp", fg, I["final_g"].ap().partition_broadcast(128), writes=[u + "fg"])
        if moe:
            rw = A.alloc([KC, NE], F32)
            rb = A.alloc([NE], F32)
            if SKIPR < 2 or SKIPR == 3:
                S.dma("sp", rw, I["moe_router_w"].ap()[0].rearrange("(kc p) n -> p kc n", p=128), writes=[u + "rw"])
                S.dma("sp", rb, I["moe_router_b"].ap()[0].partition_broadcast(128), writes=[u + "rb"])
            h32 = A.alloc([KC, 128], F32)
            comb = A.alloc([TG, NE], F32)
            zb = A.alloc([1], F32)
            tmpc = [A.alloc([512], F32) for _ in range(2)]
            S.op("pool", lambda e: e.memset(zb, 0.0), writes=[u + "zb"])
            rt = A.alloc([6, NE], F32)
        x1t = A.alloc([TG, D], F32)
        acc = A.alloc([TG, D], F32)
        ymg = A.alloc([KC, GT], BF16)
        hfT = A.alloc([KC, GT], BF16)
        actT = A.alloc([NCH, GT], BF16)
        wgb = [A.alloc([KC, FBW], BF16) for _ in range(2)]
        wub = [A.alloc([KC, FBW], BF16) for _ in range(2)]
        wdb = [A.alloc([DB, 512], BF16) for _ in range(NWD)]
        sgt = [A.alloc([GT], F32) for _ in range(2)]
        xin = [A.alloc([D], F32) for _ in range(2)]
        wc = 0
        dc = 0
        pc = 0
        ec = 0
        for g in range(NT // GT):
            s_ = (g * GT) // L
            bi = s_ if len(bl) > 1 else 0
            b = bl[bi]
            S.dma("sp", ymg, Ym.ap()[:, g * GT:(g + 1) * GT].rearrange("(kc p) t -> p kc t", p=128), reads=["Ym" + tag], writes=[u + "ymg"], partial=False)
            for ti in range(TG):
                t = g * TG + ti
                xt, xk = xin[t % 2], u + "xin%d" % (t % 2)
                S.dma("sp", xt, Xsrc.ap()[t * 128:(t + 1) * 128, :], reads=[self.nm(Xsrc)], writes=[xk], partial=False)
                for half in range(2):
                    pst, pk = self.ps[half], self.psk[half]

                    def mmf(e, pst=pst, ti=ti, half=half):
                        ins = None
                        for kc in range(KC):
                            ins = e.matmul(pst, lhsT=ymg[:, kc, ti * 128:(ti + 1) * 128], rhs=wout[:, kc, half * 512:(half + 1) * 512], start=(kc == 0), stop=(kc == KC - 1))
                        return ins
                    S.op("pe", mmf, reads=[u + "ymg", u + "wout"], writes=[pk])
                    hs = slice(half * 512, (half + 1) * 512)
                    S.op("dve", lambda e, pst=pst, hs=hs, ti=ti, bi=bi: e.tensor_tensor(out=x1t[:, ti, hs], in0=pst, in1=grow[:, bi, 0, hs], op=ALU.mult),
                         reads=[pk, u + "grow"], writes=[u + "x1t%d" % ti], partial=True)
                S.op("dve", lambda e, ti=ti, xt=xt: e.tensor_tensor(out=x1t[:, ti, :], in0=x1t[:, ti, :], in1=xt, op=ALU.add),
                     reads=[u + "x1t%d" % ti, xk], writes=[u + "x1t%d" % ti])
            for ti in range(TG):
                self.norm_transpose(x1t[:, ti, :], u + "x1t%d" % ti, hfT, u + "hfT", ti * 128, 1, b, hT32=((h32, u + "h32") if (moe and SKIPR < 2) else None), single32=True)
                if moe and SKIPR:
                    pass
                if moe and SKIPR:
                    S.op("pool", lambda e, ti=ti: e.memset(comb[:, ti, :], 0.5), reads=[u + "h32"], writes=[u + "comb"])
                elif moe:
                    pst, pk = self.ps[2], self.psk[2]

                    def mmr(e, pst=pst):
                        ins = None
                        for kc in range(KC):
                            ins = e.matmul(pst[:, 0:NE], lhsT=h32[:, kc, :], rhs=rw[:, kc, :], start=(kc == 0), stop=(kc == KC - 1))
                        return ins
                    S.op("pe", mmr, reads=[u + "h32", u + "rw"], writes=[pk])
                    lg, m1, k1, l2, m2, k2 = (rt[:, i, :] for i in range(6))
                    rk_ = u + "rt"
                    dv_ = lambda fn, r=(), w=(rk_,): S.op("dve", fn, reads=[rk_] + list(r), writes=list(w))
                    dv_(lambda e, pst=pst: e.tensor_tensor(out=lg, in0=pst[:, 0:NE], in1=rb, op=ALU.add), [pk, u + "rb"])
                    dv_(lambda e: e.tensor_reduce(out=m1[:, 0:1], in_=lg, axis=AX.X, op=ALU.max))
                    dv_(lambda e: e.tensor_scalar(out=k1, in0=lg, scalar1=m1[:, 0:1], scalar2=None, op0=ALU.is_equal))
                    dv_(lambda e: e.scalar_tensor_tensor(out=l2, in0=k1, scalar=-1e30, in1=lg, op0=ALU.mult, op1=ALU.add))
                    dv_(lambda e: e.tensor_reduce(out=m2[:, 0:1], in_=l2, axis=AX.X, op=ALU.max))
                    dv_(lambda e: e.tensor_scalar(out=k2, in0=l2, scalar1=m2[:, 0:1], scalar2=None, op0=ALU.is_equal))
                    dv_(lambda e: e.tensor_tensor(out=m2[:, 1:2], in0=m2[:, 0:1], in1=m1[:, 0:1], op=ALU.subtract))
                    S.op("act", lambda e: e.activation(out=m2[:, 2:3], in_=m2[:, 1:2], func=AF.Exp), reads=[rk_], writes=[rk_])
                    dv_(lambda e: e.tensor_scalar(out=m2[:, 3:4], in0=m2[:, 2:3], scalar1=1.0, scalar2=None, op0=ALU.add))
                    dv_(lambda e: e.reciprocal(out=m2[:, 3:4], in_=m2[:, 3:4]))
                    dv_(lambda e: e.tensor_tensor(out=m2[:, 4:5], in0=m2[:, 2:3], in1=m2[:, 3:4], op=ALU.mult))
                    dv_(lambda e: e.tensor_scalar(out=k1, in0=k1, scalar1=m2[:, 3:4], scalar2=None, op0=ALU.mult))
                    dv_(lambda e, ti=ti: e.scalar_tensor_tensor(out=comb[:, ti, :], in0=k2, scalar=m2[:, 4:5], in1=k1, op0=ALU.mult, op1=ALU.add), w=(rk_, u + "comb"))
            for xi, (wg, wu, wd, eidx) in enumerate(experts):
                for fb in range(NBLK):
                    wc += 1
                    wgt, wut = wgb[wc % 2], wub[wc % 2]
                    wgk, wuk = u + "wg%d" % (wc % 2), u + "wu%d" % (wc % 2)
                    S.dma("sp", wgt, wg.ap()[:, fb * FBW:(fb + 1) * FBW].rearrange("(kc p) n -> p kc n", p=128), reads=[self.nm(wg)], writes=[wgk], partial=False)
                    S.dma("sp", wut, wu.ap()[:, fb * FBW:(fb + 1) * FBW].rearrange("(kc p) n -> p kc n", p=128), reads=[self.nm(wu)], writes=[wuk], partial=False)
                    for fc in range(FB):
                        pc += 1
                        pg, pgk = self.ps[(pc % 2) * 2], self.psk[(pc % 2) * 2]
                        pu_, puk = self.ps[(pc % 2) * 2 + 1], self.psk[(pc % 2) * 2 + 1]
                        for (pst, pk, wt, wk) in ((pg, pgk, wgt, wgk), (pu_, puk, wut, wuk)):
                            def mmf(e, pst=pst, wt=wt, fc=fc):
                                ins = None
                                for kc in range(KC):
                                    ins = e.matmul(pst[:, 0:GT], lhsT=wt[:, kc, fc * 128:(fc + 1) * 128], rhs=hfT[:, kc, :], start=(kc == 0), stop=(kc == KC - 1))
                                return ins
                            S.op("pe", mmf, reads=[wk, u + "hfT"], writes=[pk])
                        sg_, sgk = sgt[pc % 2], u + "sgt%d" % (pc % 2)
                        S.op("act", lambda e, sg_=sg_, pg=pg: e.activation(out=sg_, in_=pg[:, 0:GT], func=AF.Silu), reads=[pgk], writes=[sgk])
                        S.op("dve", lambda e, sg_=sg_, pu_=pu_, ch=fb * FB + fc: e.tensor_tensor(out=actT[:, ch, :], in0=sg_, in1=pu_[:, 0:GT], op=ALU.mult),
                             reads=[sgk, puk], writes=[u + "actT"], partial=True)
                for half in range(2):
                    hs = slice(half * 512, (half + 1) * 512)
                    for db in range(NCH // DB):
                        dc += 1
                        wdt, wdk = wdb[dc % NWD], u + "wd%d" % (dc % NWD)
                        S.dma("sp", wdt, wd.ap()[db * DB * 128:(db + 1) * DB * 128, hs].rearrange("(c p) n -> p c n", p=128), reads=[self.nm(wd)], writes=[wdk], partial=False)

                        def mmd(e, wdt=wdt, db=db):
                            ins = None
                            for cc in range(DB):
                                ch = db * DB + cc
                                for ti in range(TG):
                                    ins = e.matmul(self.ps[4 + ti], lhsT=actT[:, ch, ti * 128:(ti + 1) * 128], rhs=wdt[:, cc, :], start=(ch == 0), stop=(ch == NCH - 1))
                            return ins
                        S.op("pe", mmd, reads=[wdk, u + "actT"], writes=[self.psk[4 + ti] for ti in range(TG)], partial=(db > 0))
                    for ti in range(TG):
                        if eidx is None or SKIPC:
                            S.op("dve", lambda e, ti=ti, hs=hs: e.tensor_copy(out=acc[:, ti, hs], in_=self.ps[4 + ti]), reads=[self.psk[4 + ti]], writes=[u + "acc%d" % ti], partial=True)
                        elif xi == 0:
                            S.op("act", lambda e, ti=ti, hs=hs, eidx=eidx: e.activation(out=acc[:, ti, hs], in_=self.ps[4 + ti], func=AF.Identity,
                                                                                      scale=comb[:, ti, eidx:eidx + 1], bias=zb[:, 0:1]),
                                 reads=[self.psk[4 + ti], u + "comb", u + "zb"], writes=[u + "acc%d" % ti], partial=True)
                        else:
                            ec += 1
                            tmp, tk = tmpc[ec % 2], u + "tmpc%d" % (ec % 2)
                            S.op("act", lambda e, ti=ti, tmp=tmp, eidx=eidx: e.activation(out=tmp, in_=self.ps[4 + ti], func=AF.Identity,
                                                                                        scale=comb[:, ti, eidx:eidx + 1], bias=zb[:, 0:1]),
                                 reads=[self.psk[4 + ti], u + "comb", u + "zb"], writes=[tk])
                            S.op("dve", lambda e, ti=ti, hs=hs, tmp=tmp: e.tensor_tensor(out=acc[:, ti, hs], in0=acc[:, ti, hs], in1=tmp, op=ALU.add),
                                 reads=[tk, u + "acc%d" % ti], writes=[u + "acc%d" % ti])
            for ti in range(TG):
                t = g * TG + ti
                ak = u + "acc%d" % ti
                S.op("dve", lambda e, ti=ti, bi=bi: e.tensor_tensor(out=acc[:, ti, :], in0=acc[:, ti, :], in1=grow[:, bi, 1, :], op=ALU.mult), reads=[ak, u + "grow"], writes=[ak])
                S.op("dve", lambda e, ti=ti: e.tensor_tensor(out=acc[:, ti, :], in0=acc[:, ti, :], in1=x1t[:, ti, :], op=ALU.add), reads=[ak, u + "x1t%d" % ti], writes=[ak])
                if final:
                    ss = self._nt_ss
                    S.op("act", lambda e, ti=ti: e.activation(out=self._nt_junk, in_=acc[:, ti, :], func=AF.Square, accum_out=ss[:, 0:1]), reads=[ak], writes=["nt_ss", "nt_junk"])
                    S.op("act", lambda e: e.activation(out=ss[:, 1:2], in_=ss[:, 0:1], func=AF.Sqrt, scale=1.0 / D, bias=self.epsb[:, 0:1]), reads=["nt_ss", "epsb"], writes=["nt_ss"])
                    S.op("dve", lambda e: e.reciprocal(out=ss[:, 2:3], in_=ss[:, 1:2]), reads=["nt_ss"], writes=["nt_rs"])
                    S.op("dve", lambda e, ti=ti: e.scalar_tensor_tensor(out=acc[:, ti, :], in0=acc[:, ti, :], scalar=ss[:, 2:3], in1=fg, op0=ALU.mult, op1=ALU.mult),
                         reads=[ak, "nt_rs", u + "fg"], writes=[ak])
                S.dma("sp", Xdst.ap()[t * 128:(t + 1) * 128, :], acc[:, ti, :], reads=[ak], writes=[self.nm(Xdst)])
        self.S.barrier()
        A.release(m0)

    def program(self):
        S = self.S
        stop = self.stop_after
        xres = self.dscr("xres", [NS * L_LAT, D], F32)
        cres = self.dscr("cres", [NS * L_CTX, D], F32)
        for l in range(DEPTH):
            last = (l == DEPTH - 1)
            S.phase = "prep%d" % l
            self.prep_mod(l)
            self.prep_fnet(l)
            S.barrier()
            csrc = self.I["ctx"] if l == 0 else cres
            S.phase = "ctx%d" % l
            self.phaseA(l, csrc, L_CTX, False, lambda s: 2, "c")
            if not last:
                self.fnet(l, L_CTX, "c")
                self.hyena_filters(l, L_CTX, "c")
                self.hyena(l, L_CTX, "c")
            ms = self.arena.mark()
            P = self.s5_prep(l)
            self.s5_main(l, L_CTX, "c", P, False, not last)
            S.op("act", lambda e: e.copy(out=self.s0, in_=self.s0n), reads=["s0n"], writes=["s0"])
            S.barrier()
            self.arena.release(ms)
            if not last:
                self.phaseC(l, csrc, cres, L_CTX, "c", [2], False, False)
            if stop == "C%d" % l:
                break
            xsrc = self.I["x"] if l == 0 else xres
            xdst = self.out if last else xres
            S.phase = "xA%d" % l
            self.phaseA(l, xsrc, L_LAT, True, lambda s: s, "x")
            if stop == "xA%d" % l:
                break
            S.phase = "xF%d" % l
            self.fnet(l, L_LAT, "x")
            if stop == "xF%d" % l:
                break
            S.phase = "xG%d" % l
            self.hyena_filters(l, L_LAT, "x")
            if stop == "xG%d" % l:
                break
            S.phase = "xH%d" % l
            self.hyena(l, L_LAT, "x")
            if stop == "xH%d" % l:
                break
            ms = self.arena.mark()
            S.phase = "xS%d" % l
            P = self.s5_prep(l)
            if l == 0 and DEPTH > 1 and stop is None:
                self.cast_moe(1)
            self.s5_main(l, L_LAT, "x", P, True, True)
            S.barrier()
            self.arena.release(ms)
            if stop == "M%d" % l:
                break
            S.phase = "xC%d" % l
            self.phaseC(l, xsrc, xdst, L_LAT, "x", [0, 1], last, last)
            if stop == "L%d" % l:
                break
        self.finish()

    def finish(self):
        S = self.S
        keys = [k for k in S.res.keys() if k in self.debug or k == "out"]
        toks = []
        for k in list(S.res.keys()):
            r = S.res[k]
            toks += r["w"] + r["r"]
        waits = S._waits("sp", toks)
        S.q["sp"].append((waits, None, None, 0, S.phase))
        S.emit()
        self.st.close()


_CACHE = {}


def make_in_maps(inputs, cores=range(NCORES)):
    cst = _CACHE.get("consts")
    if cst is None:
        cst = _consts()
        _CACHE["consts"] = cst
    maps = []
    f32 = lambda a: np.ascontiguousarray(np.asarray(a, dtype=np.float32))
    shared = {}
    for name, shape in IN_SPECS:
        if name in ("x", "ctx", "cvec"):
            continue
        shared[name] = f32(inputs[name]).reshape(shape)
    x = np.asarray(inputs["x"])
    ctx = np.asarray(inputs["ctx"])
    c = np.asarray(inputs["c"])
    c_ctx = np.asarray(inputs["c_ctx"])
    for ci in cores:
        m = dict(shared)
        m.update(cst)
        m["x"] = f32(x[ci * NS:(ci + 1) * NS]).reshape(NS * L_LAT, D)
        m["ctx"] = f32(ctx[ci * NS:(ci + 1) * NS]).reshape(NS * L_CTX, D)
        cc = np.stack([c[ci * NS], c[ci * NS + 1], c_ctx], axis=0)
        m["cvec"] = f32(cc.T.reshape(KC, 128, 3).transpose(1, 0, 2))
        maps.append(m)
    return maps


def kernel(**inputs):
    B = _CACHE.get("B")
    if B is None:
        B = Builder()
        B.program()
        _CACHE["B"] = B
    maps = make_in_maps(inputs)
    res = run_bass_kernel_spmd(B.nc, maps, core_ids=list(range(NCORES)))
    out = np.concatenate([r["out"].reshape(NS, L_LAT, D) for r in res.results], axis=0)
    return out.astype(np.float32)
```

```python
import math
from contextlib import ExitStack
import numpy as np
import ml_dtypes
import concourse.bass as bass
import concourse.mybir as mybir
from concourse.bass_utils import run_bass_kernel_spmd

F32 = mybir.dt.float32
BF16 = mybir.dt.bfloat16
I32 = mybir.dt.int32
AF = mybir.ActivationFunctionType
ALU = mybir.AluOpType
AX = mybir.AxisListType

import os
SKIPR = int(os.environ.get('SKIPR', 0))
SKIPC = int(os.environ.get('SKIPC', 0))
SCOPES = int(os.environ.get('SCOPES', 0))
NCORES = 8
NS = 2
D = 1024
KC = 8
L_LAT = 4096
L_CTX = 256
DEPTH = 2
D_IN = 1792
HY_OFF = 256
S5_OFF = 1408
DFF = 2816
DFFE = 3584
NE = 8
EPS = 1e-6
TWO_PI = 2.0 * math.pi


class Sched:
    ENG = ("pe", "act", "dve", "pool", "sp")

    def __init__(self, nc):
        self.nc = nc
        self.q = {e: [] for e in self.ENG}
        self.ecount = {e: 0 for e in self.ENG}
        self.seen = {e: {} for e in self.ENG}
        self.res = {}
        self.dcount = {}
        self.semnames = ["c_pe", "c_act", "c_dve", "c_pool"]
        self.keysem = {}
        self.free_sems = []
        self.phase = "init"

    def _r(self, k):
        r = self.res.get(k)
        if r is None:
            r = dict(w=[], r=[], pw=[], pr=[], partial=False)
            self.res[k] = r
        return r

    def _deps(self, reads, writes, partial):
        deps = []
        for k in reads:
            deps += self._r(k)["w"]
        joins = []
        for k in writes:
            r = self._r(k)
            if partial and r["partial"] and not r["r"] and r["w"]:
                deps += r["pw"] + r["pr"]
                joins.append(k)
            else:
                deps += r["w"] + r["r"]
        return deps, joins

    def _commit(self, tok, reads, writes, partial, joins):
        for k in reads:
            rr = self._r(k)["r"]
            rr.append(tok)
            if len(rr) > 64:
                mx = {}
                for (s, v) in rr:
                    if mx.get(s, 0) < v:
                        mx[s] = v
                rr[:] = list(mx.items())
        for k in writes:
            r = self._r(k)
            if k in joins:
                r["w"].append(tok)
                if len(r["w"]) > 64:
                    mx = {}
                    for (s, v) in r["w"]:
                        if mx.get(s, 0) < v:
                            mx[s] = v
                    r["w"][:] = list(mx.items())
            else:
                r["pw"], r["pr"] = r["w"], r["r"]
                r["w"], r["r"] = [tok], []
                r["partial"] = partial

    def _waits(self, eng, deps, skip_self=False):
        need = {}
        for (s, v) in deps:
            if skip_self and s == "c_" + eng:
                continue
            if self.seen[eng].get(s, 0) >= v:
                continue
            if need.get(s, 0) < v:
                need[s] = v
        for s, v in need.items():
            self.seen[eng][s] = v
        return list(need.items())

    def op(self, eng, fn, reads=(), writes=(), partial=False):
        deps, joins = self._deps(reads, writes, partial)
        waits = self._waits(eng, deps, skip_self=(eng == "pe"))
        self.ecount[eng] += 1
        tok = ("c_" + eng, self.ecount[eng])
        self.q[eng].append((waits, fn, tok, 1, self.phase))
        self._commit(tok, reads, writes, partial, joins)
        return tok

    def dma(self, eng, out, in_, reads=(), writes=(), partial=True, **kw):
        assert len(writes) == 1
        k = writes[0]
        deps, joins = self._deps(reads, writes, partial)
        waits = self._waits(eng, deps)
        sname = self.keysem.get(k)
        if sname is None:
            if self.free_sems:
                sname = self.free_sems.pop()
            else:
                sname = "d_%d" % len(self.dcount)
                self.dcount[sname] = 0
                self.semnames.append(sname)
            self.keysem[k] = sname
        self.dcount[sname] += 16
        tok = (sname, self.dcount[sname])

        def fn(e, out=out, in_=in_, kw=kw):
            return e.dma_start(out=out, in_=in_, **kw)

        self.q[eng].append((waits, fn, tok, 16, self.phase))
        self._commit(tok, reads, writes, partial, joins)
        return tok

    def barrier(self):
        toks = []
        for e in ("pe", "act", "dve", "pool"):
            if self.ecount[e]:
                toks.append(("c_" + e, self.ecount[e]))
        for s, v in self.dcount.items():
            if v:
                toks.append((s, v))
        for e in self.ENG:
            waits = self._waits(e, toks)
            if waits:
                self.q[e].append((waits, None, None, 0, self.phase))
        self.keysem = {}
        self.free_sems = list(self.dcount.keys())

    def emit(self):
        nc = self.nc
        with ExitStack() as st:
            sems = {}
            for i, s in enumerate(self.semnames):
                sems[s] = st.enter_context(nc.semaphore("s%d" % i))
            block = st.enter_context(nc.Block())
            engs = {"pe": block.tensor, "act": block.scalar, "dve": block.vector,
                    "pool": block.gpsimd, "sp": block.sync}
            for ename, deco in engs.items():
                items = self.q[ename]

                def body(e, items=items):
                    cur = None
                    cm = None
                    for (waits, fn, tok, inc, ph) in items:
                        if SCOPES and ph != cur:
                            if cm is not None:
                                cm.__exit__(None, None, None)
                            cm = nc.named_scope(ph)
                            cm.__enter__()
                            cur = ph
                        for (s, v) in waits:
                            e.wait_ge(sems[s], v)
                        if fn is None:
                            continue
                        ins = fn(e)
                        ins.then_inc(sems[tok[0]], inc)
                    if cm is not None:
                        cm.__exit__(None, None, None)
                deco(body)


def _dsize(dt):
    return {F32: 4, BF16: 2, I32: 4}[dt]


class Arena:
    def __init__(self, nc, st, nbytes):
        self.t = st.enter_context(nc.sbuf_tensor("arena", [128, nbytes // 4], F32))
        self.nbytes = nbytes
        self.off = 0
        self.uid = 0

    def mark(self):
        return self.off

    def release(self, m):
        self.off = m

    def alloc(self, free_shape, dt, parts=128):
        n = int(np.prod(free_shape))
        size = (n * _dsize(dt) + 31) // 32 * 32
        assert self.off + size <= self.nbytes, ("arena overflow", self.off, size)
        a = self.t[0:parts, self.off // 4:(self.off + size) // 4]
        if dt != F32:
            a = a.bitcast(dt)
        a = a[:, 0:n]
        if len(free_shape) == 2:
            a = a.rearrange("p (a b) -> p a b", a=free_shape[0])
        elif len(free_shape) == 3:
            a = a.rearrange("p (a b c) -> p a b c", a=free_shape[0], b=free_shape[1])
        self.off += size
        self.uid += 1
        return a


def _consts():
    c = {}
    for L in (L_LAT, L_CTX):
        t = np.arange(L, dtype=np.int64)
        tf = (np.outer(t, t) % L).astype(np.float64) * (2.0 * np.pi / L)
        c["dftc%d" % L] = (np.cos(tf) / np.sqrt(L)).astype(ml_dtypes.bfloat16)
        c["dfts%d" % L] = (-np.sin(tf) / np.sqrt(L)).astype(ml_dtypes.bfloat16)
        tt = np.linspace(0.0, 1.0, L, dtype=np.float32)[:, None]
        w = (np.float32(2.0 * math.pi / L) * np.arange(L, dtype=np.float32))[:, None]
        bands = np.linspace(1e-4, 15, 16, dtype=np.float32)[None, :]
        z = np.concatenate([tt, np.cos(bands * w), -np.sin(bands * w)], axis=-1).astype(np.float32)
        c["zpos%d" % L] = np.ascontiguousarray(z.T)
        max_decay = math.log(0.01) / 0.3
        min_decay = math.log(0.01) / 1.5
        deltas = np.abs(np.linspace(min_decay, max_decay, 384, dtype=np.float32))
        c["dec%d" % L] = np.exp(-tt.T.astype(np.float32) * deltas[:, None]).astype(np.float32)
    k = np.arange(64)
    a = np.outer(k, k) * (2.0 * np.pi / 64)
    c64 = np.cos(a) / 8.0
    s64 = np.sin(a) / 8.0
    z64 = np.zeros((64, 64))
    c["c64bd"] = np.block([[c64, z64], [z64, c64]]).astype(np.float32)
    c["s64bd"] = np.block([[s64, z64], [z64, s64]]).astype(np.float32)
    ii = np.arange(128) // 16
    c["mask_le"] = (ii[:, None] <= ii[None, :]).astype(np.float32)
    c["mask_ge"] = (ii[:, None] >= ii[None, :]).astype(np.float32)
    c["ident"] = np.eye(128, dtype=np.float32)
    pw = list(range(-7, 9)) + [8 * (2 ** k) for k in range(10)]
    c["s5pw"] = np.tile(np.array(pw, dtype=np.float32)[None, :], (128, 1))
    return c


S5PW = list(range(-7, 9)) + [8 * (2 ** k) for k in range(10)]


def PWI(n):
    return S5PW.index(n)


IN_SPECS = [
    ("x", [NS * L_LAT, D]), ("ctx", [NS * L_CTX, D]), ("cvec", [128, KC, 3]),
    ("ada_w", [DEPTH, D, 6 * D]), ("ada_b", [DEPTH, 6 * D]),
    ("norm_mix_g", [DEPTH, D]), ("norm_ffn_g", [DEPTH, D]),
    ("w_in", [DEPTH, D, D_IN]), ("w_out", [DEPTH, D, D]),
    ("fnet_w", [DEPTH, 4, 64, 64]),
    ("hy_conv_w", [DEPTH, 3, 1152]), ("hy_conv_b", [DEPTH, 1152]),
    ("hy_fw1", [DEPTH, 33, 64]), ("hy_fb1", [DEPTH, 64]), ("hy_freq1", [DEPTH, 64]),
    ("hy_fw2", [DEPTH, 64, 64]), ("hy_fb2", [DEPTH, 64]), ("hy_freq2", [DEPTH, 64]),
    ("hy_fw3", [DEPTH, 64, 1536]), ("hy_bias", [DEPTH, 2, 384]),
    ("s5_a_re", [DEPTH, 2, 24, 64]), ("s5_a_im", [DEPTH, 2, 24, 64]), ("s5_log_dt", [DEPTH, 2, 24]),
    ("s5_b_re", [DEPTH, 2, 24, 64, 16]), ("s5_b_im", [DEPTH, 2, 24, 64, 16]),
    ("s5_c_re", [DEPTH, 2, 24, 16, 64]), ("s5_c_im", [DEPTH, 2, 24, 16, 64]),
    ("s5_d", [DEPTH, 384]), ("s5_glu_w", [DEPTH, 384, 384]), ("s5_glu_b", [DEPTH, 384]),
    ("ffn_w_gate", [1, D, DFF]), ("ffn_w_up", [1, D, DFF]), ("ffn_w_down", [1, DFF, D]),
    ("moe_router_w", [1, D, NE]), ("moe_router_b", [1, NE]),
    ("moe_w_gate", [1, NE, D, DFFE]), ("moe_w_up", [1, NE, D, DFFE]), ("moe_w_down", [1, NE, DFFE, D]),
    ("final_g", [D]),
]
CONST_SPECS = [
    ("dftc4096", [4096, 4096], BF16), ("dfts4096", [4096, 4096], BF16),
    ("dftc256", [256, 256], BF16), ("dfts256", [256, 256], BF16),
    ("zpos4096", [33, 4096], F32), ("zpos256", [33, 256], F32),
    ("dec4096", [384, 4096], F32), ("dec256", [384, 256], F32),
    ("c64bd", [128, 128], F32), ("s64bd", [128, 128], F32),
    ("mask_le", [128, 128], F32), ("mask_ge", [128, 128], F32),
    ("ident", [128, 128], F32), ("s5pw", [128, 26], F32),
]


class Builder:
    def __init__(self, debug=(), stop_after=None, ne_cast=NE, ne_comp=NE):
        self.ne_cast = ne_cast
        self.ne_comp = ne_comp
        self.debug = set(debug)
        self.stop_after = stop_after
        nc = bass.Bass("TRN2", target_bir_lowering=False)
        self.nc = nc
        self.S = Sched(nc)
        self.st = ExitStack()
        self.I = {}
        for name, shape in IN_SPECS:
            self.I[name] = nc.dram_tensor(name, shape, F32, kind="ExternalInput")
        for name, shape, dt in CONST_SPECS:
            self.I[name] = nc.dram_tensor(name, shape, dt, kind="ExternalInput")
        self.out = nc.dram_tensor("out", [NS * L_LAT, D], F32, kind="ExternalOutput")
        self.scr = {}
        self.names = {id(self.out): "out"}
        self.arena = Arena(nc, self.st, 192 * 1024)
        self.ps = [self.st.enter_context(nc.psum_tensor("psb%d" % i, [128, 512], F32))[:] for i in range(8)]
        self.psk = ["ps%d" % i for i in range(8)]
        self.uid = 0
        self._moe_experts = None
        A = self.arena
        self.ident = A.alloc([128], F32)
        self.identb = A.alloc([128], BF16)
        self.ones = A.alloc([128], F32)
        self.epsb = A.alloc([1], F32)
        self.Am = [A.alloc([KC, 3], F32) for _ in range(2)]
        self.Bm = [A.alloc([KC, 3], F32) for _ in range(2)]
        self.s0 = A.alloc([48, NS], F32)
        self.RHSf = A.alloc([2, 256], BF16)
        self.s0n = A.alloc([48, NS], F32)
        S = self.S
        S.dma("sp", self.ident, self.I["ident"].ap(), writes=["ident"])
        S.op("dve", lambda e: e.tensor_copy(out=self.identb, in_=self.ident), reads=["ident"], writes=["identb"])
        S.op("pool", lambda e: e.memset(self.ones, 1.0), writes=["ones"])
        S.op("pool", lambda e: e.memset(self.epsb, EPS), writes=["epsb"])

    def dscr(self, name, shape, dt):
        kind = "ExternalOutput" if name in self.debug else "Internal"
        t = self.nc.dram_tensor(name, shape, dt, kind=kind)
        self.scr[name] = t
        self.names[id(t)] = name
        return t

    def nm(self, t):
        return self.names.get(id(t), "input")

    def dump(self, name, ap, key, shape, dt):
        if name not in self.debug:
            return
        t = self.dscr(name, shape, dt)
        self.S.dma("sp", t.ap(), ap, reads=[key], writes=["dbg_" + name])

    def key(self, base):
        self.uid += 1
        return "%s#%d" % (base, self.uid)

    def range_reduce_sin(self, eng, out, ang, tmp_i, tmp_f, keys_r, key_w, key_i, key_f, shift=0.0):
        S = self.S
        S.op(eng, lambda e: e.tensor_scalar(out=tmp_i, in0=ang, scalar1=shift, scalar2=1.0 / TWO_PI, op0=ALU.add, op1=ALU.mult),
             reads=keys_r, writes=[key_i])
        S.op(eng, lambda e: e.tensor_copy(out=tmp_f, in_=tmp_i), reads=[key_i], writes=[key_f])
        S.op(eng, lambda e: e.scalar_tensor_tensor(out=tmp_f, in0=tmp_f, scalar=-TWO_PI, in1=ang, op0=ALU.mult, op1=ALU.add),
             reads=[key_f] + list(keys_r), writes=[key_f])
        S.op(eng, lambda e: e.tensor_scalar(out=tmp_f, in0=tmp_f, scalar1=shift, scalar2=math.pi, op0=ALU.add, op1=ALU.min),
             reads=[key_f], writes=[key_f])
        S.op(eng, lambda e: e.tensor_scalar(out=tmp_f, in0=tmp_f, scalar1=-math.pi, scalar2=None, op0=ALU.max),
             reads=[key_f], writes=[key_f])
        S.op("act", lambda e: e.activation(out=out, in_=tmp_f, func=AF.Sin), reads=[key_f], writes=[key_w], partial=True)

    def prep_mod(self, l):
        S, A, I = self.S, self.arena, self.I
        m0 = A.mark()
        cv = A.alloc([KC, 3], F32)
        adab = A.alloc([48], F32)
        gmix = A.alloc([KC], F32)
        gffn = A.alloc([KC], F32)
        modT = A.alloc([48, 3], F32)
        wt = [A.alloc([KC, 512], F32) for _ in range(2)]
        u = self.key("pm")
        S.dma("sp", cv, I["cvec"].ap(), writes=[u + "cv"])
        S.op("act", lambda e: e.activation(out=cv, in_=cv, func=AF.Silu), reads=[u + "cv"], writes=[u + "cv"])
        S.dma("sp", adab, I["ada_b"].ap()[l].rearrange("(f p) -> p f", p=128), writes=[u + "adab"], allow_slow_non_contiguous=True)
        S.dma("sp", gmix, I["norm_mix_g"].ap()[l].rearrange("(f p) -> p f", p=128), writes=[u + "gmix"], allow_slow_non_contiguous=True)
        S.dma("sp", gffn, I["norm_ffn_g"].ap()[l].rearrange("(f p) -> p f", p=128), writes=[u + "gffn"], allow_slow_non_contiguous=True)
        for blk in range(12):
            w = wt[blk % 2]
            wk = u + "wt%d" % (blk % 2)
            S.dma("sp", w, I["ada_w"].ap()[l][:, blk * 512:(blk + 1) * 512].rearrange("(kc p) n -> p kc n", p=128), writes=[wk])
            pst = self.ps[blk % 2]
            pk = self.psk[blk % 2]

            def mmf(e, w=w, pst=pst):
                ins = None
                for m in range(4):
                    for kc in range(KC):
                        ins = e.matmul(pst[:, m * 3:(m + 1) * 3], lhsT=w[:, kc, m * 128:(m + 1) * 128], rhs=cv[:, kc, :],
                                       start=(kc == 0), stop=(kc == KC - 1))
                return ins
            S.op("pe", mmf, reads=[wk, u + "cv"], writes=[pk])
            S.op("dve", lambda e, pst=pst, blk=blk: e.tensor_tensor(
                out=modT[:, blk * 4:(blk + 1) * 4, :], in0=pst[:, 0:12].rearrange("p (m b) -> p m b", m=4),
                in1=adab[:, blk * 4:(blk + 1) * 4].unsqueeze(2).to_broadcast([128, 4, 3]), op=ALU.add),
                reads=[pk, u + "adab"], writes=[u + "modT"], partial=True)
        for wi, (gv, gk, so, ho) in enumerate(((gmix, u + "gmix", 8, 0), (gffn, u + "gffn", 32, 24))):
            S.op("dve", lambda e, wi=wi, so=so: e.tensor_scalar(out=self.Am[wi], in0=modT[:, so:so + 8, :], scalar1=1.0, scalar2=None, op0=ALU.add),
                 reads=[u + "modT"], writes=["Am%d" % wi])
            S.op("dve", lambda e, wi=wi, gv=gv: e.tensor_tensor(out=self.Am[wi], in0=self.Am[wi], in1=gv.unsqueeze(2).to_broadcast([128, 8, 3]), op=ALU.mult),
                 reads=["Am%d" % wi, gk], writes=["Am%d" % wi])
            S.op("dve", lambda e, wi=wi, ho=ho: e.tensor_copy(out=self.Bm[wi], in_=modT[:, ho:ho + 8, :]),
                 reads=[u + "modT"], writes=["Bm%d" % wi])
        md = self.scr.get("modD%d" % l) or self.dscr("modD%d" % l, [3, 6 * D], F32)
        for b in range(3):
            S.dma("sp", md.ap()[b].rearrange("(f p) -> p f", p=128), modT[:, :, b], reads=[u + "modT"], writes=["modD%d" % l],
                  allow_slow_non_contiguous=True)
        self.S.barrier()
        A.release(m0)

    def norm_transpose(self, xt, xk, hT, hk, col0, which, b, hT32=None, single32=False):
        S = self.S
        u = self.key("nt")
        ss = self._nt_ss
        junk = self._nt_junk
        S.op("act", lambda e: e.activation(out=junk, in_=xt, func=AF.Square, accum_out=ss[:, 0:1]), reads=[xk], writes=["nt_ss", "nt_junk"])
        S.op("act", lambda e: e.activation(out=ss[:, 1:2], in_=ss[:, 0:1], func=AF.Sqrt, scale=1.0 / D, bias=self.epsb[:, 0:1]),
             reads=["nt_ss", "epsb"], writes=["nt_ss"])
        S.op("dve", lambda e: e.reciprocal(out=ss[:, 2:3], in_=ss[:, 1:2]), reads=["nt_ss"], writes=["nt_rs"])
        S.op("dve", lambda e: e.tensor_scalar(out=junk, in0=xt, scalar1=ss[:, 2:3], scalar2=None, op0=ALU.mult),
             reads=[xk, "nt_rs"], writes=["nt_junk"])
        for half in range(2):
            pst = self.ps[6 + half]
            pk = self.psk[6 + half]

            def tr(e, half=half, pst=pst):
                ins = None
                for q in range(4):
                    kc = half * 4 + q
                    ins = e.transpose(pst[:, q * 128:(q + 1) * 128], junk[:, kc * 128:(kc + 1) * 128], self.ident)
                return ins
            S.op("pe", tr, reads=["nt_junk", "ident"], writes=[pk])
            for q in range(4):
                kc = half * 4 + q
                S.op("act", lambda e, q=q, kc=kc, pst=pst: e.activation(
                    out=hT[:, kc, col0:col0 + 128], in_=pst[:, q * 128:(q + 1) * 128], func=AF.Identity,
                    scale=self.Am[which][:, kc, b:b + 1], bias=self.Bm[which][:, kc, b:b + 1]),
                    reads=[pk, "Am%d" % which, "Bm%d" % which], writes=[hk], partial=True)
                if hT32 is not None:
                    S.op("act", lambda e, q=q, kc=kc, pst=pst: e.activation(
                        out=(hT32[0][:, kc, :] if single32 else hT32[0][:, kc, col0:col0 + 128]), in_=pst[:, q * 128:(q + 1) * 128],
                        func=AF.Identity, scale=self.Am[which][:, kc, b:b + 1], bias=self.Bm[which][:, kc, b:b + 1]),
                        reads=[pk, "Am%d" % which, "Bm%d" % which], writes=[hT32[1]], partial=True)

    def prep_fnet(self, l):
        S, A, I = self.S, self.arena, self.I
        u = self.key("pf")
        m0 = A.mark()
        cbd = A.alloc([128], F32)
        sbd = A.alloc([128], F32)
        wbd = A.alloc([2, 128], F32)
        S.dma("sp", cbd, I["c64bd"].ap(), writes=[u + "cbd"])
        S.dma("sp", sbd, I["s64bd"].ap(), writes=[u + "sbd"])
        S.op("pool", lambda e: e.memset(wbd, 0.0), writes=[u + "wbd"])
        for h in range(4):
            j, hh = h // 2, h % 2
            S.dma("sp", wbd[hh * 64:(hh + 1) * 64, j, hh * 64:(hh + 1) * 64], I["fnet_w"].ap()[l, h], reads=[], writes=[u + "wbd"])
        for j in range(2):
            pst = self.ps[j]
            pk = self.psk[j]

            def mmf(e, j=j, pst=pst):
                e.matmul(pst[:, 0:128], lhsT=cbd, rhs=wbd[:, j, :], start=True, stop=True)
                return e.matmul(pst[:, 128:256], lhsT=sbd, rhs=wbd[:, j, :], start=True, stop=True)
            S.op("pe", mmf, reads=[u + "cbd", u + "sbd", u + "wbd"], writes=[pk])
            S.op("dve", lambda e, j=j, pst=pst: e.tensor_copy(out=self.RHSf[:, j, :], in_=pst[:, 0:256]), reads=[pk], writes=["RHSf"], partial=True)
        self.S.barrier()
        A.release(m0)

    def phaseA(self, l, Xd, L, grid, bfun, tag):
        S, A, I = self.S, self.arena, self.I
        u = self.key("pA")
        NT = NS * L
        GT = min(512, L)
        TG = GT // 128
        R = 64 if grid else L
        NBk = L // 8
        ABd = self.scr.get("AB" + tag) or self.dscr("AB" + tag, [NT, 512], BF16)
        Hq = self.scr.get("Hq" + tag) or self.dscr("Hq" + tag, [3, NT, 384], BF16)
        Xs = self.scr.get("Xs" + tag) or self.dscr("Xs" + tag, [8, 384, NS, NBk], BF16)
        m0 = A.mark()
        self._nt_ss = A.alloc([4], F32)
        self._nt_junk = A.alloc([D], F32)
        win = A.alloc([KC, 256 + 384], BF16)
        wtap = A.alloc([KC, 3, 1152], BF16)
        cwb = A.alloc([3, 1152], F32)
        cbb = A.alloc([1152], F32)
        S.dma("pool", win[:, :, 0:256], I["w_in"].ap()[l][:, 0:256].rearrange("(kc p) n -> p kc n", p=128), writes=[u + "win"])
        S.dma("pool", win[:, :, 256:640], I["w_in"].ap()[l][:, S5_OFF:D_IN].rearrange("(kc p) n -> p kc n", p=128), writes=[u + "win"])
        S.dma("sp", cwb, I["hy_conv_w"].ap()[l].rearrange("t n -> (t n)").partition_broadcast(128), writes=[u + "cwb"])
        S.dma("sp", cbb, I["hy_conv_b"].ap()[l].partition_broadcast(128), writes=[u + "cbb"])
        m1 = A.mark()
        wf32 = A.alloc([1152], F32)
        for kc in range(KC):
            S.dma("sp", wf32, I["w_in"].ap()[l][kc * 128:(kc + 1) * 128, HY_OFF:S5_OFF], writes=[u + "wf32"], partial=False)
            for tap in range(3):
                S.op("dve" if tap != 1 else "pool", lambda e, kc=kc, tap=tap: e.tensor_tensor(out=wtap[:, kc, tap, :], in0=wf32, in1=cwb[:, tap, :], op=ALU.mult),
                     reads=[u + "wf32", u + "cwb"], writes=[u + "wtap"], partial=True)
        self.dump("dbg_wtap", wtap[:, 0, :, :], u + "wtap", [128, 3, 1152], BF16)
        self.dump("dbg_cwb", cwb, u + "cwb", [128, 3, 1152], F32)
        self.dump("dbg_rhsf", self.RHSf, "RHSf", [128, 2, 256], BF16)
        self.S.barrier()
        A.release(m1)
        xts = [A.alloc([D], F32) for _ in range(2)]
        hT = [[A.alloc([KC, GT], BF16) for _ in range(3)] for _ in range(2)]
        hR = [A.alloc([KC, GT], BF16) for _ in range(3)]
        pfT = A.alloc([2, GT], BF16)
        abt = [A.alloc([512], BF16) for _ in range(2)]
        hqt = [A.alloc([384], BF16) for _ in range(2)]
        psd = [A.alloc([8, GT // 8], BF16) for _ in range(2)]
        for bi in range(2):
            for tp in (0, 2):
                S.op("pool", lambda e, bi=bi, tp=tp: e.memset(hT[bi][tp], 0.0), writes=[u + "hT%d_%d" % (bi, tp)])
        ngroups = NT // GT
        cnt = 0
        for g in range(ngroups):
            bi = g % 2
            s = (g * GT) // L
            b = bfun(s)
            hc, hm, hp = hT[bi][1], hT[bi][0], hT[bi][2]
            hck, hmk, hpk = [u + "hT%d_%d" % (bi, t) for t in (1, 0, 2)]
            for ti in range(TG):
                t = g * TG + ti
                xt = xts[t % 2]
                xk = u + "xt%d" % (t % 2)
                S.dma("sp", xt, Xd.ap()[t * 128:(t + 1) * 128, :], reads=[self.nm(Xd)], writes=[xk], partial=False)
                self.norm_transpose(xt, xk, hc, hck, ti * 128, 0, b)
            hcv = hc.rearrange("p k (r c) -> p (k r) c", c=R)
            hmv = hm.rearrange("p k (r c) -> p (k r) c", c=R)
            hpv = hp.rearrange("p k (r c) -> p (k r) c", c=R)
            S.op("pool", lambda e, hmv=hmv, hcv=hcv: e.tensor_copy(out=hmv[:, :, 1:R], in_=hcv[:, :, 0:R - 1]), reads=[hck], writes=[hmk])
            S.op("dve", lambda e, hpv=hpv, hcv=hcv: e.tensor_copy(out=hpv[:, :, 0:R - 1], in_=hcv[:, :, 1:R]), reads=[hck], writes=[hpk])
            for tp, (src, sk) in enumerate(((hm, hmk), (hc, hck), (hp, hpk))):
                S.op("pool" if tp == 1 else "dve", lambda e, tp=tp, src=src: e.tensor_copy(
                    out=hR[tp].rearrange("p k (t c) -> p (k t) c", c=128),
                    in_=src.rearrange("p k (t c) -> p (k t) c", c=128)[:, :, ::-1]), reads=[sk], writes=[u + "hR%d" % tp])
            for j in range(2):
                pst, pk = self.ps[j], self.psk[j]

                def mmf(e, j=j, pst=pst, hc=hc):
                    ins = None
                    for kc in range(KC):
                        ins = e.matmul(pst[:, 0:GT], lhsT=win[:, kc, j * 128:(j + 1) * 128], rhs=hc[:, kc, :], start=(kc == 0), stop=(kc == KC - 1))
                    return ins
                S.op("pe", mmf, reads=[u + "win", hck], writes=[pk])
                S.op("act", lambda e, j=j, pst=pst: e.copy(out=pfT[:, j, :], in_=pst[:, 0:GT]), reads=[pk], writes=[u + "pfT"], partial=True)
            for ti in range(TG):
                t = g * TG + ti
                pst, pk = self.ps[2 + (t % 2)], self.psk[2 + (t % 2)]

                def mmf(e, ti=ti, pst=pst):
                    e.matmul(pst[:, 0:256], lhsT=pfT[:, 0, ti * 128:(ti + 1) * 128], rhs=self.RHSf[:, 0, :], start=True, stop=True)
                    return e.matmul(pst[:, 256:512], lhsT=pfT[:, 1, ti * 128:(ti + 1) * 128], rhs=self.RHSf[:, 1, :], start=True, stop=True)
                S.op("pe", mmf, reads=[u + "pfT", "RHSf"], writes=[pk])
                ab = abt[t % 2]
                abk = u + "ab%d" % (t % 2)
                S.op("dve", lambda e, ab=ab, pst=pst: e.tensor_copy(out=ab, in_=pst), reads=[pk], writes=[abk])
                S.dma("sp", ABd.ap()[t * 128:(t + 1) * 128, :], ab, reads=[abk], writes=["AB" + tag])
            for ti in range(TG):
                t = g * TG + ti
                for part in range(3):
                    cnt += 1
                    pst, pk = self.ps[4 + (cnt % 2)], self.psk[4 + (cnt % 2)]

                    def mmf(e, ti=ti, part=part, pst=pst, hts=(hm, hc, hp)):
                        ins = None
                        n = 0
                        for tap in range(3):
                            for kc in range(KC):
                                lt = (hR[tap] if part == 1 else hts[tap])[:, kc, ti * 128:(ti + 1) * 128]
                                ins = e.matmul(pst[:, 0:384], lhsT=lt, rhs=wtap[:, kc, tap, part * 384:(part + 1) * 384],
                                               start=(n == 0), stop=(n == 23))
                                n += 1
                        return ins
                    S.op("pe", mmf, reads=[hck, hmk, hpk, u + "wtap", u + "hR0", u + "hR1", u + "hR2"], writes=[pk])
                    hq = hqt[cnt % 2]
                    hqk = u + "hq%d" % (cnt % 2)
                    S.op("dve", lambda e, hq=hq, pst=pst, part=part: e.tensor_tensor(out=hq, in0=pst[:, 0:384], in1=cbb[:, part * 384:(part + 1) * 384], op=ALU.add),
                         reads=[pk, u + "cbb"], writes=[hqk])
                    S.dma("sp", Hq.ap()[part, t * 128:(t + 1) * 128, :], hq, reads=[hqk], writes=["Hq" + tag])
            b0 = ((g * GT) % L) // 8
            for m in range(3):
                cnt += 1
                pst, pk = self.ps[cnt % 2], self.psk[cnt % 2]

                def mmf(e, m=m, pst=pst, hc=hc):
                    ins = None
                    for kc in range(KC):
                        ins = e.matmul(pst[:, 0:GT], lhsT=win[:, kc, 256 + m * 128:256 + (m + 1) * 128], rhs=hc[:, kc, :], start=(kc == 0), stop=(kc == KC - 1))
                    return ins
                S.op("pe", mmf, reads=[u + "win", hck], writes=[pk])
                pd = psd[cnt % 2]
                pdk = u + "psd%d" % (cnt % 2)
                S.op("act", lambda e, pd=pd, pst=pst: e.copy(out=pd, in_=pst[:, 0:GT].rearrange("p (b i) -> p i b", i=8)), reads=[pk], writes=[pdk])
                S.dma("sp", Xs.ap()[:, m * 128:(m + 1) * 128, s, b0:b0 + GT // 8].rearrange("i p b -> p i b"), pd, reads=[pdk], writes=["Xs" + tag])
        self.S.barrier()
        A.release(m0)

    def fnet(self, l, L, tag):
        S, A, I = self.S, self.arena, self.I
        u = self.key("fn")
        NT = NS * L
        NTC = L // 128
        FT = min(512, L)
        ABd = self.scr["AB" + tag]
        Ym = self.scr.get("Ym" + tag) or self.dscr("Ym" + tag, [D, NT], BF16)
        m0 = A.mark()
        ABs = A.alloc([NS * NTC, 512], BF16)
        Cf = A.alloc([NTC, FT], BF16)
        Sf = A.alloc([NTC, FT], BF16)
        yf = [A.alloc([FT], BF16) for _ in range(2)]
        S.dma("sp", ABs, ABd.ap().rearrange("(c p) n -> p c n", p=128), reads=["AB" + tag], writes=[u + "ABs"])
        cnt = 0
        for ft in range(L // FT):
            S.dma("sp", Cf, I["dftc%d" % L].ap()[:, ft * FT:(ft + 1) * FT].rearrange("(c p) n -> p c n", p=128), writes=[u + "Cf"], partial=False)
            S.dma("sp", Sf, I["dfts%d" % L].ap()[:, ft * FT:(ft + 1) * FT].rearrange("(c p) n -> p c n", p=128), writes=[u + "Sf"], partial=False)
            for s in range(NS):
                for j in range(2):
                    cnt += 1
                    pst, pk = self.ps[cnt % 2], self.psk[cnt % 2]

                    def mmf(e, s=s, j=j, pst=pst):
                        ins = None
                        for tc in range(NTC):
                            e.matmul(pst[:, 0:FT], lhsT=ABs[:, s * NTC + tc, j * 256:j * 256 + 128], rhs=Cf[:, tc, :], start=(tc == 0), stop=False)
                            ins = e.matmul(pst[:, 0:FT], lhsT=ABs[:, s * NTC + tc, j * 256 + 128:j * 256 + 256], rhs=Sf[:, tc, :], start=False, stop=(tc == NTC - 1))
                        return ins
                    S.op("pe", mmf, reads=[u + "ABs", u + "Cf", u + "Sf"], writes=[pk])
                    y = yf[cnt % 2]
                    yk = u + "yf%d" % (cnt % 2)
                    S.op("act", lambda e, y=y, pst=pst: e.copy(out=y, in_=pst[:, 0:FT]), reads=[pk], writes=[yk])
                    S.dma("sp", Ym.ap()[j * 128:(j + 1) * 128, s * L + ft * FT:s * L + (ft + 1) * FT], y, reads=[yk], writes=["Ym" + tag])
        self.S.barrier()
        A.release(m0)

    def hyena_filters(self, l, L, tag):
        S, A, I = self.S, self.arena, self.I
        u = self.key("hf")
        Gd = self.scr.get("Gd" + tag) or self.dscr("Gd" + tag, [2, 384, 2 * L], BF16)
        CT = min(512, L)
        m0 = A.mark()
        zT = A.alloc([L], F32)
        H1 = A.alloc([L], F32)
        H2n = A.alloc([L], F32)
        H2r = A.alloc([L], F32)
        fw1 = A.alloc([64], F32)
        fw2 = A.alloc([64], F32)
        fw3 = A.alloc([1536], F32)
        vec = A.alloc([4], F32)
        hb = A.alloc([6], F32)
        dec = A.alloc([3, L], F32)
        G = A.alloc([2 * L], F32)
        Gb = A.alloc([2 * L], BF16)
        tf = A.alloc([CT], F32)
        tf2 = A.alloc([CT], F32)
        ti = A.alloc([CT], I32)
        nrm = A.alloc([4], F32)
        S.dma("sp", zT[0:33, :], I["zpos%d" % L].ap(), writes=[u + "zT"])
        S.dma("sp", fw1[0:33, :], I["hy_fw1"].ap()[l], writes=[u + "fw1"])
        S.dma("sp", fw2[0:64, :], I["hy_fw2"].ap()[l], writes=[u + "fw2"])
        S.dma("sp", fw3[0:64, :], I["hy_fw3"].ap()[l], writes=[u + "fw3"])
        for i, nm in enumerate(("hy_fb1", "hy_freq1", "hy_fb2", "hy_freq2")):
            S.dma("sp", vec[0:64, i:i + 1], I[nm].ap()[l].rearrange("(p o) -> p o", o=1), writes=[u + "vec"])
        S.dma("sp", hb, I["hy_bias"].ap()[l].rearrange("o (c p) -> p (o c)", p=128), writes=[u + "hb"], allow_slow_non_contiguous=True)
        S.dma("sp", dec, I["dec%d" % L].ap().rearrange("(c p) n -> p c n", p=128), writes=[u + "dec"])
        for (src, sk, wgt, wk, kdim, dst, dk, vi) in ((zT, u + "zT", fw1, u + "fw1", 33, H1, u + "H1", 0), (H1, u + "H1", fw2, u + "fw2", 64, H2n, u + "H2n", 2)):
            for ct in range(L // CT):
                pst, pk = self.ps[ct % 2], self.psk[ct % 2]
                S.op("pe", lambda e, pst=pst, src=src, wgt=wgt, kdim=kdim, ct=ct: e.matmul(
                    pst[0:64, 0:CT], lhsT=wgt[0:kdim, 0:64], rhs=src[0:kdim, ct * CT:(ct + 1) * CT], start=True, stop=True),
                    reads=[sk, wk], writes=[pk])
                S.op("dve", lambda e, pst=pst, vi=vi: e.tensor_scalar(out=tf[0:64, :], in0=pst[0:64, 0:CT], scalar1=vec[0:64, vi:vi + 1], scalar2=vec[0:64, vi + 1:vi + 2],
                                                            op0=ALU.add, op1=ALU.mult), reads=[pk, u + "vec"], writes=[u + "tf"])
                self.range_reduce_sin("dve", dst[0:64, ct * CT:(ct + 1) * CT], tf[0:64, :], ti[0:64, :], tf2[0:64, :], [u + "tf"], dk, u + "ti", u + "tf2")
        S.op("dve", lambda e: e.tensor_copy(out=H2r[0:64, :], in_=H2n[0:64, ::-1]), reads=[u + "H2n"], writes=[u + "H2r"])
        cnt = 0
        for o in range(2):
            for cc in range(3):
                if o == 0:
                    segs = [(0, L, 0, "r", 0), (L, 2 * L - 1, 1, "n", 1)]
                else:
                    segs = [(0, L - 1, 1, "r", 0), (L - 1, 2 * L - 1, 0, "n", 0)]
                gk = u + "G"
                S.op("pool", lambda e: e.memset(G[:, 2 * L - 1:2 * L], 0.0), writes=[gk])
                for (a0, a1, dr, wh, po) in segs:
                    col = dr * 768 + o * 384 + cc * 128
                    srcH = H2r if wh == "r" else H2n
                    p = a0
                    while p < a1:
                        n = min(CT, a1 - p)
                        sp = p - a0 + po
                        cnt += 1
                        pst, pk = self.ps[cnt % 2], self.psk[cnt % 2]
                        S.op("pe", lambda e, pst=pst, srcH=srcH, col=col, sp=sp, n=n: e.matmul(
                            pst[:, 0:n], lhsT=fw3[0:64, col:col + 128], rhs=srcH[0:64, sp:sp + n], start=True, stop=True),
                            reads=[u + "H2n", u + "H2r", u + "fw3"], writes=[pk])
                        if wh == "r":
                            dsl = dec[:, cc, L - sp - n:L - sp][:, ::-1]
                        else:
                            dsl = dec[:, cc, sp:sp + n]
                        S.op("dve", lambda e, pst=pst, p=p, n=n, dsl=dsl: e.tensor_tensor(out=G[:, p:p + n], in0=pst[:, 0:n], in1=dsl, op=ALU.mult),
                             reads=[pk, u + "dec"], writes=[gk], partial=True)
                        p += n
                S.op("dve", lambda e: e.tensor_reduce(out=nrm[:, 0:1], in_=G, axis=AX.X, op=ALU.add, apply_absolute_value=True), reads=[gk], writes=[u + "nrm"])
                S.op("dve", lambda e: e.reciprocal(out=nrm[:, 1:2], in_=nrm[:, 0:1]), reads=[u + "nrm"], writes=[u + "nrm"])
                S.op("dve", lambda e: e.tensor_scalar(out=G, in0=G, scalar1=nrm[:, 1:2], scalar2=None, op0=ALU.mult), reads=[gk, u + "nrm"], writes=[gk])
                S.op("dve", lambda e, o=o, cc=cc: e.tensor_scalar(out=G[:, L - 1:L], in0=G[:, L - 1:L], scalar1=hb[:, o * 3 + cc:o * 3 + cc + 1], scalar2=None, op0=ALU.add),
                     reads=[gk, u + "hb"], writes=[gk])
                S.op("act", lambda e: e.copy(out=Gb, in_=G), reads=[gk], writes=[u + "Gb"])
                S.dma("sp", Gd.ap()[o, cc * 128:(cc + 1) * 128, :], Gb, reads=[u + "Gb"], writes=["Gd" + tag])
        self.S.barrier()
        A.release(m0)

    def hyena(self, l, L, tag):
        S, A, I = self.S, self.arena, self.I
        u = self.key("hy")
        NB = L // 128
        NC = NS * NB
        W = 128 * (2 * NB - 1)
        NT = NS * L
        Hq = self.scr["Hq" + tag]
        Gd = self.scr["Gd" + tag]
        Ym = self.scr.get("Ym" + tag) or self.dscr("Ym" + tag, [D, NT], BF16)
        HC = 192
        CG = max(1, min(8, 512 // NC))
        m0 = A.mark()
        Z2 = A.alloc([NC, 384], BF16)
        Ub = A.alloc([NC, HC], BF16)
        Xb = A.alloc([NC, HC], BF16)
        Z1 = A.alloc([NC, HC], BF16)
        RG = 1
        for cand in (24, 12, 8, 6, 4, 3, 2):
            if cand * W <= 9216 and HC % cand == 0:
                RG = cand
                break
        Rb = [A.alloc([RG, W], BF16) for _ in range(2)]
        yT = [A.alloc([3, 128], BF16) for _ in range(2)]
        rc = 0
        bc = 0
        dl = [0] + [d for d in range(-(NB - 1), NB) if d != 0]
        for half in range(2):
            h0 = half * HC
            for o in range(2):
                if o == 0:
                    S.dma("sp", Ub, Hq.ap()[0, :, h0:h0 + HC].rearrange("(c p) n -> p c n", p=128), reads=["Hq" + tag], writes=[u + "Ub"], partial=False)
                    S.dma("sp", Xb, Hq.ap()[1, :, h0:h0 + HC].rearrange("(c p) n -> p c n", p=128), reads=["Hq" + tag], writes=[u + "Xb"], partial=False)
                    src, srck, dst, dstk = Ub, u + "Ub", Z1, u + "Z1"
                else:
                    S.dma("sp", Xb, Hq.ap()[2, :, h0:h0 + HC].rearrange("(c p) n -> p c n", p=128), reads=["Hq" + tag], writes=[u + "Xb"], partial=False)
                    src, srck, dst, dstk = Z1, u + "Z1", Z2, u + "Z2"
                srcv = src.rearrange("p (s b) n -> p s b n", s=NS)
                for c8 in range(HC // CG):
                    bc += 1
                    pst, pk = self.ps[bc % 4], self.psk[bc % 4]
                    for ci in range(CG):
                        cl = c8 * CG + ci
                        c = h0 + cl
                        if cl % RG == 0:
                            rc += 1
                            Rg = Rb[rc % 2]
                            rk = u + "R%d" % (rc % 2)
                            if RG == 1:
                                S.dma("sp", Rg[:, 0, :], bass.AP(Gd, (o * 384 + c) * 2 * L, [[1, 128], [1, W]]), reads=["Gd" + tag], writes=[rk], partial=False)
                            else:
                                S.dma("sp", Rg, bass.AP(Gd, (o * 384 + c) * 2 * L, [[1, 128], [2 * L, RG], [1, W]]), reads=["Gd" + tag], writes=[rk], partial=False)
                        Rt = Rg[:, cl % RG, :]
                        pv = pst[:, ci * NC:(ci + 1) * NC].rearrange("p (s b) -> p s b", s=NS)

                        def mmf(e, Rt=Rt, pv=pv, srcv=srcv, cl=cl, o=o):
                            ins = None
                            for k, d in enumerate(dl):
                                i0 = max(0, d)
                                i1 = min(NB - 1, NB - 1 + d)
                                nb = i1 - i0 + 1
                                j0 = i0 - d
                                xd = 128 * (NB - 1 - d) if o == 0 else 128 * (d + NB - 1)
                                ins = e.matmul(pv[:, :, i0:i0 + nb], lhsT=Rt[:, xd:xd + 128], rhs=srcv[:, :, j0:j0 + nb, cl],
                                               start=(k == 0), stop=(k == len(dl) - 1))
                            return ins
                        S.op("pe", mmf, reads=[rk, srck], writes=[pk], partial=True)
                    cl0 = c8 * CG
                    oc0 = cl0 if o == 0 else h0 + cl0
                    S.op("dve", lambda e, pst=pst, dst=dst, cl0=cl0, oc0=oc0: e.tensor_tensor(
                        out=dst[:, :, oc0:oc0 + CG], in0=pst[:, 0:CG * NC].rearrange("p (c n) -> p n c", c=CG),
                        in1=Xb[:, :, cl0:cl0 + CG], op=ALU.mult), reads=[pk, u + "Xb"], writes=[dstk], partial=True)
        for col in range(NC):
            pst, pk = self.ps[4 + col % 2], self.psk[4 + col % 2]
            pb = pst.bitcast(BF16)

            def trf(e, col=col, pb=pb):
                ins = None
                for cc in range(3):
                    ins = e.transpose(pb[:, cc * 128:(cc + 1) * 128], Z2[:, col, cc * 128:(cc + 1) * 128], self.identb)
                return ins
            S.op("pe", trf, reads=[u + "Z2", "identb"], writes=[pk])
            y = yT[col % 2]
            yk = u + "yT%d" % (col % 2)
            S.op("act", lambda e, y=y, pb=pb: e.copy(out=y, in_=pb[:, 0:384].rearrange("p (c t) -> p c t", c=3)), reads=[pk], writes=[yk])
            S.dma("sp", Ym.ap()[256:640, col * 128:(col + 1) * 128].rearrange("(c p) t -> p c t", p=128), y, reads=[yk], writes=["Ym" + tag])
        self.S.barrier()
        A.release(m0)

    def s5_prep(self, l):
        S, A, I = self.S, self.arena, self.I
        u = self.key("sp")
        P = {}
        P["pr"] = A.alloc([24, 26], F32)
        P["pi"] = A.alloc([24, 26], F32)
        P["npi"] = A.alloc([24, 26], F32)
        P["WBT"] = A.alloc([48, 128], BF16)
        P["Mpad"] = A.alloc([24, 8, 32], BF16)
        P["Gpad"] = A.alloc([48, 8, 32], BF16)
        P["gluw"] = A.alloc([4, 384], BF16)
        P["glub"] = A.alloc([4], F32)
        P["u"] = u
        m0 = A.mark()
        lr = A.alloc([24], F32); li = A.alloc([24], F32); dt = A.alloc([24], F32)
        Bre = A.alloc([24, 16], F32); Bim = A.alloc([24, 16], F32)
        Cre = A.alloc([24, 16], F32); Cim = A.alloc([24, 16], F32)
        pwc = A.alloc([26], F32)
        EX = A.alloc([24, 26], F32); ANG = A.alloc([24, 26], F32)
        tI = A.alloc([24, 26], I32); tF = A.alloc([24, 26], F32)
        t1 = A.alloc([24], F32); t2 = A.alloc([24], F32); cr = A.alloc([24], F32); ci = A.alloc([24], F32)
        bbr = A.alloc([24, 16], F32); bbi = A.alloc([24, 16], F32)
        T1 = A.alloc([12, 8, 16], F32); T2 = A.alloc([12, 8, 16], F32)
        Wn = A.alloc([48, 128], F32)
        Pp = A.alloc([48, 128], BF16)
        Qq = A.alloc([48, 128], BF16)
        mle = A.alloc([128], F32); mge = A.alloc([128], F32)
        dv = A.alloc([24], F32)
        Mf = A.alloc([128], F32)
        S.dma("sp", lr, I["s5_a_re"].ap()[l].rearrange("d (q m) s -> (m s) (d q)", m=2), writes=[u + "lr"], allow_slow_non_contiguous=True)
        S.dma("sp", li, I["s5_a_im"].ap()[l].rearrange("d (q m) s -> (m s) (d q)", m=2), writes=[u + "li"], allow_slow_non_contiguous=True)
        for m in range(2):
            S.dma("sp", dt[m * 64:(m + 1) * 64, :], I["s5_log_dt"].ap()[l].rearrange("d (q m) -> m (d q)", m=2)[m].partition_broadcast(64),
                  writes=[u + "dt"], allow_slow_non_contiguous=True)
        Csrc = A.alloc([6, 128], F32)
        CT_ = A.alloc([768], F32)
        for (dst, nm, kk) in ((Cre, "s5_c_re", u + "Cre"), (Cim, "s5_c_im", u + "Cim")):
            for hh in range(2):
                S.dma("sp", Csrc[:, :, hh * 64:(hh + 1) * 64], I[nm].ap()[l].rearrange("d g o s -> (d g o) s").rearrange("(k p) s -> p k s", p=128),
                      writes=[u + "Csrc"])
            for k in range(6):
                pst, pk = self.ps[k % 2], self.psk[k % 2]
                S.op("pe", lambda e, k=k, pst=pst: e.transpose(pst[:, 0:128], Csrc[:, k, :], self.ident), reads=[u + "Csrc", "ident"], writes=[pk])
                S.op("act", lambda e, k=k, pst=pst: e.copy(out=CT_[:, k * 128:(k + 1) * 128], in_=pst[:, 0:128]), reads=[pk], writes=[u + "CT"], partial=True)
            CTv = CT_.rearrange("p (d q m o) -> p d q m o", d=2, q=12, m=2)
            for m in range(2):
                S.op("dve", lambda e, m=m, dst=dst, CTv=CTv: e.tensor_copy(out=dst[m * 64:(m + 1) * 64].rearrange("p (d q) o -> p d q o", d=2), in_=CTv[m * 64:(m + 1) * 64, :, :, m, :]),
                     reads=[u + "CT"], writes=[kk], partial=True)
        S.dma("sp", Bre, I["s5_b_re"].ap()[l].rearrange("d (q m) s c -> (m s) (d q) c", m=2), writes=[u + "Bre"])
        S.dma("sp", Bim, I["s5_b_im"].ap()[l].rearrange("d (q m) s c -> (m s) (d q) c", m=2), writes=[u + "Bim"])
        S.dma("sp", pwc, I["s5pw"].ap(), writes=[u + "pwc"])
        S.dma("sp", mle, I["mask_le"].ap(), writes=[u + "mle"])
        S.dma("sp", mge, I["mask_ge"].ap(), writes=[u + "mge"])
        for i in range(8):
            S.dma("sp", dv[i * 16:(i + 1) * 16, :], I["s5_d"].ap()[l].rearrange("(g c) -> c g", c=16), writes=[u + "dv"], allow_slow_non_contiguous=True)
        S.dma("pool", P["gluw"][0:96], I["s5_glu_w"].ap()[l].rearrange("(c p) n -> p c n", p=96), writes=[u + "gluw"])
        S.dma("sp", P["glub"][0:96], I["s5_glu_b"].ap()[l].rearrange("(c p) -> p c", p=96), writes=[u + "glub"], allow_slow_non_contiguous=True)
        V = lambda e: e
        dve = lambda fn, r, w, **k: S.op("dve", fn, reads=r, writes=w, **k)
        dve(lambda e: e.tensor_copy(out=dt, in_=dt), [u + "dt"], [u + "dt"])
        S.op("act", lambda e: e.activation(out=dt, in_=dt, func=AF.Exp), reads=[u + "dt"], writes=[u + "dt"])
        dve(lambda e: e.tensor_tensor(out=t1, in0=lr, in1=dt, op=ALU.mult), [u + "lr", u + "dt"], [u + "t1"])
        dve(lambda e: e.tensor_tensor(out=t2, in0=li, in1=dt, op=ALU.mult), [u + "li", u + "dt"], [u + "t2"])
        bc3 = lambda a: a.unsqueeze(2).to_broadcast([128, 24, 26])
        pw3 = pwc.unsqueeze(1).to_broadcast([128, 24, 26])
        dve(lambda e: e.tensor_tensor(out=EX, in0=bc3(t1), in1=pw3, op=ALU.mult), [u + "t1", u + "pwc"], [u + "EX"])
        dve(lambda e: e.tensor_tensor(out=ANG, in0=bc3(t2), in1=pw3, op=ALU.mult), [u + "t2", u + "pwc"], [u + "ANG"])
        S.op("act", lambda e: e.activation(out=EX, in_=EX, func=AF.Exp), reads=[u + "EX"], writes=[u + "EX"])
        self.range_reduce_sin("dve", P["pi"], ANG, tI, tF, [u + "ANG"], u + "pi", u + "tI", u + "tF")
        self.range_reduce_sin("dve", P["pr"], ANG, tI, tF, [u + "ANG"], u + "pr", u + "tI", u + "tF", shift=math.pi / 2)
        dve(lambda e: e.tensor_tensor(out=P["pi"], in0=P["pi"], in1=EX, op=ALU.mult), [u + "pi", u + "EX"], [u + "pi"])
        dve(lambda e: e.tensor_tensor(out=P["pr"], in0=P["pr"], in1=EX, op=ALU.mult), [u + "pr", u + "EX"], [u + "pr"])
        dve(lambda e: e.tensor_scalar(out=P["npi"], in0=P["pi"], scalar1=-1.0, scalar2=None, op0=ALU.mult), [u + "pi"], [u + "npi"])
        pk_ = [u + "pr", u + "pi", u + "npi"]
        i1 = PWI(1)
        ar = P["pr"][:, :, i1]; ai = P["pi"][:, :, i1]
        dve(lambda e: e.tensor_tensor(out=t1, in0=lr, in1=lr, op=ALU.mult), [u + "lr", u + "EX"], [u + "t1"])
        dve(lambda e: e.tensor_tensor(out=t2, in0=li, in1=li, op=ALU.mult), [u + "li", u + "ANG"], [u + "t2"])
        dve(lambda e: e.tensor_tensor(out=t1, in0=t1, in1=t2, op=ALU.add), [u + "t1", u + "t2"], [u + "t1"])
        dve(lambda e: e.reciprocal(out=t1, in_=t1), [u + "t1"], [u + "t1"])
        dve(lambda e: e.tensor_scalar(out=t2, in0=ar, scalar1=-1.0, scalar2=None, op0=ALU.add), pk_, [u + "t2"])
        dve(lambda e: e.tensor_tensor(out=cr, in0=t2, in1=lr, op=ALU.mult), [u + "t2", u + "lr"], [u + "cr"])
        dve(lambda e: e.tensor_tensor(out=ci, in0=ai, in1=li, op=ALU.mult), pk_ + [u + "li"], [u + "ci"])
        dve(lambda e: e.tensor_tensor(out=cr, in0=cr, in1=ci, op=ALU.add), [u + "cr", u + "ci"], [u + "cr"])
        dve(lambda e: e.tensor_tensor(out=cr, in0=cr, in1=t1, op=ALU.mult), [u + "cr", u + "t1"], [u + "cr"])
        dve(lambda e: e.tensor_tensor(out=ci, in0=ai, in1=lr, op=ALU.mult), pk_ + [u + "lr", u + "cr"], [u + "ci"])
        dve(lambda e: e.tensor_tensor(out=t2, in0=t2, in1=li, op=ALU.mult), [u + "t2", u + "li"], [u + "t2"])
        dve(lambda e: e.tensor_tensor(out=ci, in0=ci, in1=t2, op=ALU.subtract), [u + "ci", u + "t2"], [u + "ci"])
        dve(lambda e: e.tensor_tensor(out=ci, in0=ci, in1=t1, op=ALU.mult), [u + "ci", u + "t1"], [u + "ci"])
        b16 = lambda a: a.unsqueeze(2).to_broadcast([128, 24, 16])
        dve(lambda e: e.tensor_scalar(out=Cim, in0=Cim, scalar1=-1.0, scalar2=None, op0=ALU.mult), [u + "Cim"], [u + "Cim"])

        def cmul(outr, outi, xr_, xi_, yr_, yi_, rk, wkr, wki, ta, tb, shape_sel=None, neg_im=False):
            dve(lambda e: e.tensor_tensor(out=ta, in0=xr_, in1=yr_, op=ALU.mult), rk, [u + "ta"])
            dve(lambda e: e.tensor_tensor(out=tb, in0=xi_, in1=yi_, op=ALU.mult), rk, [u + "tb"])
            dve(lambda e: e.tensor_tensor(out=outr, in0=ta, in1=tb, op=(ALU.add if neg_im else ALU.subtract)), [u + "ta", u + "tb"], [wkr], partial=True)
            dve(lambda e: e.tensor_tensor(out=ta, in0=xr_, in1=yi_, op=ALU.mult), rk + [wkr], [u + "ta"])
            dve(lambda e: e.tensor_tensor(out=tb, in0=xi_, in1=yr_, op=ALU.mult), rk + [wkr], [u + "tb"])
            dve(lambda e: e.tensor_tensor(out=outi, in0=ta, in1=tb, op=(ALU.subtract if neg_im else ALU.add)), [u + "ta", u + "tb"], [wki], partial=True)
        ta24 = T1.rearrange("p a b c -> p (a b c)")[:, 0:384].rearrange("p (a c) -> p a c", a=24)
        tb24 = T2.rearrange("p a b c -> p (a b c)")[:, 0:384].rearrange("p (a c) -> p a c", a=24)
        cmul(bbr, bbi, b16(cr), b16(ci), Bre, Bim, [u + "cr", u + "ci", u + "Bre", u + "Bim"], u + "bbr", u + "bbi", ta24, tb24)
        def pslice(arr, d, n0, step):
            i0 = PWI(n0)
            if step > 0:
                sl = arr[:, d * 12:(d + 1) * 12, i0:i0 + 8]
            else:
                sl = arr[:, d * 12:(d + 1) * 12, i0 - 7:i0 + 1][:, :, ::-1]
            return sl.unsqueeze(3).to_broadcast([128, 12, 8, 16])
        v16 = lambda a, d: a[:, d * 12:(d + 1) * 12, :].unsqueeze(2).to_broadcast([128, 12, 8, 16])
        Wnv = Wn.rearrange("p (d q r) (i c) -> p d q r i c", d=2, q=12, r=2, i=8)
        Ppv = Pp.rearrange("p (d q r) (i c) -> p d q r i c", d=2, q=12, r=2, i=8)
        Qqv = Qq.rearrange("p (d q r) (i c) -> p d q r i c", d=2, q=12, r=2, i=8)
        bk = [u + "bbr", u + "bbi"] + pk_
        ck = [u + "Cre", u + "Cim"] + pk_
        for d in range(2):
            n0, stp = ((7, -1), (0, 1))[d]
            cmul(Wnv[:, d, :, 0], Wnv[:, d, :, 1], pslice(P["pr"], d, n0, stp), pslice(P["pi"], d, n0, stp), v16(bbr, d), v16(bbi, d),
                 bk, u + "Wn", u + "Wn", T1, T2)
            n0, stp = ((0, -1), (0, 1))[d]
            cmul(Ppv[:, d, :, 0], Ppv[:, d, :, 1], pslice(P["pr"], d, n0, stp), pslice(P["pi"], d, n0, stp), v16(bbr, d), v16(bbi, d),
                 bk, u + "Pp", u + "Pp", T1, T2)
            n0, stp = ((0, 1), (0, -1))[d]
            cmul(Qqv[:, d, :, 0], Qqv[:, d, :, 1], pslice(P["pr"], d, n0, stp), pslice(P["pi"], d, n0, stp), v16(Cre, d), v16(Cim, d),
                 ck, u + "Qq", u + "Qq", T1, T2, neg_im=True)
        S.op("pool", lambda e: e.memset(P["Gpad"], 0.0), writes=[u + "Gpad"])
        Gv = P["Gpad"].rearrange("p (d q r) j n -> p d q r j n", d=2, q=12, r=2)
        for d in range(2):
            n0, stp = ((1, 1), (8, -1))[d]
            for m in range(2):
                hs = slice(m * 64, (m + 1) * 64)
                h = lambda a: a[hs]
                cmul(Gv[hs, d, :, 0, :, m * 16:(m + 1) * 16], Gv[hs, d, :, 1, :, m * 16:(m + 1) * 16],
                     h(pslice(P["pr"], d, n0, stp)), h(pslice(P["pi"], d, n0, stp)), h(v16(Cre, d)), h(v16(Cim, d)),
                     ck + [u + "Gpad"], u + "Gpad", u + "Gpad", T1[hs], T2[hs], neg_im=True)
        for k in range(48):
            pst, pk = self.ps[k % 2], self.psk[k % 2]
            S.op("pe", lambda e, k=k, pst=pst: e.transpose(pst[:, 0:128], Wn[:, k, :], self.ident), reads=[u + "Wn", "ident"], writes=[pk])
            S.op("act", lambda e, k=k, pst=pst: e.copy(out=P["WBT"][:, k, :], in_=pst[:, 0:128]), reads=[pk], writes=[u + "WBT"], partial=True)
        S.op("pool", lambda e: e.memset(P["Mpad"], 0.0), writes=[u + "Mpad"])
        for g in range(24):
            q, m = g // 2, g % 2
            hs = slice(m * 64, (m + 1) * 64)
            psf, pkf = self.ps[2 + (g % 2) * 2], self.psk[2 + (g % 2) * 2]
            psb, pkb = self.ps[3 + (g % 2) * 2], self.psk[3 + (g % 2) * 2]
            for d, pst, pk in ((0, psf, pkf), (1, psb, pkb)):
                kre = (d * 12 + q) * 2
                def mmf(e, pst=pst, kre=kre, hs=hs):
                    e.matmul(pst[:, 0:128], lhsT=Pp[hs, kre, :], rhs=Qq[hs, kre, :], start=True, stop=False)
                    return e.matmul(pst[:, 0:128], lhsT=Pp[hs, kre + 1, :], rhs=Qq[hs, kre + 1, :], start=False, stop=True)
                S.op("pe", mmf, reads=[u + "Pp", u + "Qq"], writes=[pk])
            dve(lambda e, psf=psf: e.tensor_tensor(out=Mf, in0=psf[:, 0:128], in1=mle, op=ALU.mult), [pkf, u + "mle"], [u + "Mf"])
            dve(lambda e, psb=psb: e.tensor_tensor(out=T1.rearrange("p a b c -> p (a b c)")[:, 0:128], in0=psb[:, 0:128], in1=mge, op=ALU.mult), [pkb, u + "mge"], [u + "ta"])
            dve(lambda e: e.tensor_tensor(out=Mf, in0=Mf, in1=T1.rearrange("p a b c -> p (a b c)")[:, 0:128], op=ALU.add), [u + "Mf", u + "ta"], [u + "Mf"])
            dve(lambda e, g=g: e.scalar_tensor_tensor(out=Mf, in0=self.ident, scalar=dv[:, g:g + 1], in1=Mf, op0=ALU.mult, op1=ALU.add), [u + "Mf", u + "dv", "ident"], [u + "Mf"])
            dve(lambda e, g=g, m=m: e.tensor_copy(out=P["Mpad"][:, g, :, m * 16:(m + 1) * 16], in_=Mf.rearrange("p (j o) -> p j o", j=8)), [u + "Mf"], [u + "Mpad"], partial=True)
        self.S.barrier()
        A.release(m0)
        return P

    def s5_main(self, l, L, tag, P, use_s0, readout):
        S, A, I = self.S, self.arena, self.I
        u = self.key("s5")
        pu = P["u"]
        NBk = L // 8
        E = NBk + 1
        NT = NS * L
        Xs = self.scr["Xs" + tag]
        Ym = self.scr.get("Ym" + tag) or self.dscr("Ym" + tag, [D, NT], BF16)
        nlev = 0
        while (1 << nlev) < E:
            nlev += 1
        if nlev % 2:
            nlev += 1
        m0 = A.mark()
        eA = A.alloc([4, NS, E], F32)
        eB = A.alloc([4, NS, E], F32)
        ebf = A.alloc([4, NS, E], BF16)
        Xg = [A.alloc([NS * NBk], BF16) for _ in range(2)]
        if readout:
            YS = A.alloc([NS * L], F32)
            PC = min(2048, NS * L)
            tq = A.alloc([PC], F32)
            ygp = [A.alloc([PC], BF16) for _ in range(2)]
            Ygd = self.scr.get("Yg" + tag) or self.dscr("Yg" + tag, [384, NT], BF16)
        pk_ = [pu + "pr", pu + "pi", pu + "npi"]
        bc = 0
        for q in range(12):
            for m in range(2):
                g = 2 * q + m
                for i in range(8):
                    S.dma("sp", Xg[m][i * 16:(i + 1) * 16, :], Xs.ap()[i, g * 16:(g + 1) * 16].rearrange("c s b -> c (s b)"),
                          reads=["Xs" + tag], writes=[u + "Xg%d" % m])
            for d in range(2):
                col = 0 if d == 0 else NBk
                for ri in range(2):
                    k = d * 2 + ri
                    if use_s0:
                        S.op("act", lambda e, k=k, col=col, d=d, ri=ri, q=q: e.copy(out=eA[:, k, :, col], in_=self.s0[:, (d * 12 + q) * 2 + ri, :]),
                             reads=["s0"], writes=[u + "eA%d" % d], partial=True)
                    else:
                        S.op("pool", lambda e, k=k, col=col: e.memset(eA[:, k, :, col], 0.0), writes=[u + "eA%d" % d], partial=True)
            for d in range(2):
                off = 1 if d == 0 else 0
                for ri in range(2):
                    for sq in range(NS):
                        bc += 1
                        pst, pk = self.ps[bc % 2], self.psk[bc % 2]
                        kk = (d * 12 + q) * 2 + ri

                        def mmf(e, pst=pst, kk=kk, sq=sq):
                            e.matmul(pst[0:64, 0:NBk], lhsT=P["WBT"][:, kk, 0:64], rhs=Xg[0][:, sq * NBk:(sq + 1) * NBk], start=True, stop=True)
                            return e.matmul(pst[64:128, 0:NBk], lhsT=P["WBT"][:, kk, 64:128], rhs=Xg[1][:, sq * NBk:(sq + 1) * NBk], start=True, stop=True)
                        S.op("pe", mmf, reads=[pu + "WBT", u + "Xg0", u + "Xg1"], writes=[pk])
                        S.op("act", lambda e, pst=pst, d=d, ri=ri, sq=sq, off=off: e.copy(out=eA[:, d * 2 + ri, sq, off:off + NBk], in_=pst[:, 0:NBk]),
                             reads=[pk], writes=[u + "eA%d" % d], partial=True)
            for d, eng in ((0, "dve"), (1, "dve")):
                cur, nxt = eA, eB
                ck_, nk_ = u + "eA%d" % d, u + "eB%d" % d
                for lev in range(nlev):
                    dist = 1 << lev
                    ix = PWI(8 * dist)
                    ar = P["pr"][:, d * 12 + q, ix:ix + 1]
                    ai = P["pi"][:, d * 12 + q, ix:ix + 1]
                    nai = P["npi"][:, d * 12 + q, ix:ix + 1]
                    cr_, ci_ = cur[:, d * 2 + 0], cur[:, d * 2 + 1]
                    nr_, ni_ = nxt[:, d * 2 + 0], nxt[:, d * 2 + 1]
                    if dist < E:
                        n = E - dist
                        if d == 0:
                            srcs, dsts = slice(0, n), slice(dist, E)
                            keep = slice(0, dist)
                        else:
                            srcs, dsts = slice(dist, E), slice(0, n)
                            keep = slice(n, E)
                        S.op(eng, lambda e, cr_=cr_, nr_=nr_, ar=ar, srcs=srcs, dsts=dsts: e.scalar_tensor_tensor(
                            out=nr_[:, :, dsts], in0=cr_[:, :, srcs], scalar=ar, in1=cr_[:, :, dsts], op0=ALU.mult, op1=ALU.add),
                            reads=[ck_] + pk_, writes=[nk_], partial=True)
                        S.op(eng, lambda e, ci_=ci_, nr_=nr_, nai=nai, srcs=srcs, dsts=dsts: e.scalar_tensor_tensor(
                            out=nr_[:, :, dsts], in0=ci_[:, :, srcs], scalar=nai, in1=nr_[:, :, dsts], op0=ALU.mult, op1=ALU.add),
                            reads=[ck_, nk_] + pk_, writes=[nk_], partial=True)
                        S.op(eng, lambda e, ci_=ci_, ni_=ni_, ar=ar, srcs=srcs, dsts=dsts: e.scalar_tensor_tensor(
                            out=ni_[:, :, dsts], in0=ci_[:, :, srcs], scalar=ar, in1=ci_[:, :, dsts], op0=ALU.mult, op1=ALU.add),
                            reads=[ck_] + pk_, writes=[nk_], partial=True)
                        S.op(eng, lambda e, cr_=cr_, ni_=ni_, ai=ai, srcs=srcs, dsts=dsts: e.scalar_tensor_tensor(
                            out=ni_[:, :, dsts], in0=cr_[:, :, srcs], scalar=ai, in1=ni_[:, :, dsts], op0=ALU.mult, op1=ALU.add),
                            reads=[ck_, nk_] + pk_, writes=[nk_], partial=True)
                    else:
                        keep = slice(0, E)
                    S.op("act", lambda e, cur=cur, nxt=nxt, d=d, keep=keep: e.copy(out=nxt[:, d * 2:d * 2 + 2, :, keep], in_=cur[:, d * 2:d * 2 + 2, :, keep]),
                         reads=[ck_], writes=[nk_], partial=True)
                    cur, nxt = nxt, cur
                    ck_, nk_ = nk_, ck_
            if not readout or True:
                for d in range(2):
                    col = NBk if d == 0 else 0
                    for ri in range(2):
                        S.op("act", lambda e, d=d, ri=ri, col=col, q=q: e.copy(out=self.s0n[:, (d * 12 + q) * 2 + ri, :], in_=eA[:, d * 2 + ri, :, col]),
                             reads=[u + "eA%d" % d], writes=["s0n"], partial=True)
            if not readout:
                continue
            S.op("act", lambda e: e.copy(out=ebf, in_=eA), reads=[u + "eA0", u + "eA1"], writes=[u + "ebf"])
            qq = q % 3
            gc = q // 3
            YSv = YS.rearrange("p (s b j) -> p s b j", s=NS, j=8)
            for j in range(8):
                for sq in range(NS):
                    bc += 1
                    pst, pk = self.ps[2 + bc % 4], self.psk[2 + bc % 4]

                    def mmf(e, pst=pst, j=j, sq=sq, q=q, qq=qq):
                        o = pst[qq * 32:(qq + 1) * 32, 0:NBk]
                        e.matmul(o, lhsT=P["Mpad"][:, 2 * q, j, :], rhs=Xg[0][:, sq * NBk:(sq + 1) * NBk], start=True, stop=False)
                        e.matmul(o, lhsT=P["Mpad"][:, 2 * q + 1, j, :], rhs=Xg[1][:, sq * NBk:(sq + 1) * NBk], start=False, stop=False)
                        ins = None
                        for d in range(2):
                            c0 = 0 if d == 0 else 1
                            for ri in range(2):
                                ins = e.matmul(o, lhsT=P["Gpad"][:, (d * 12 + q) * 2 + ri, j, :], rhs=ebf[:, d * 2 + ri, sq, c0:c0 + NBk],
                                               start=False, stop=(d == 1 and ri == 1))
                        return ins
                    S.op("pe", mmf, reads=[pu + "Mpad", pu + "Gpad", u + "Xg0", u + "Xg1", u + "ebf"], writes=[pk])
                    S.op("dve", lambda e, pst=pst, j=j, sq=sq, qq=qq: e.tensor_copy(out=YSv[qq * 32:(qq + 1) * 32, sq, :, j], in_=pst[qq * 32:(qq + 1) * 32, 0:NBk]),
                         reads=[pk], writes=[u + "YS"], partial=True)
            if qq == 2:
                for pc in range(NS * L // PC):
                    cs = slice(pc * PC, (pc + 1) * PC)
                    yp, ypk = ygp[pc % 2], u + "ygp%d" % (pc % 2)
                    S.op("act", lambda e, cs=cs: e.activation(out=tq[0:96], in_=YS[0:96, cs], func=AF.Square), reads=[u + "YS"], writes=[u + "tq"])
                    S.op("dve", lambda e: e.tensor_scalar(out=tq[0:96], in0=tq[0:96], scalar1=0.044715, scalar2=1.0, op0=ALU.mult, op1=ALU.add), reads=[u + "tq"], writes=[u + "tq"])
                    S.op("dve", lambda e, cs=cs: e.tensor_tensor(out=tq[0:96], in0=tq[0:96], in1=YS[0:96, cs], op=ALU.mult), reads=[u + "tq", u + "YS"], writes=[u + "tq"])
                    S.op("act", lambda e: e.activation(out=tq[0:96], in_=tq[0:96], func=AF.Sigmoid, scale=1.5957691216057308), reads=[u + "tq"], writes=[u + "tq"])
                    S.op("dve", lambda e, cs=cs, yp=yp: e.tensor_tensor(out=yp[0:96], in0=tq[0:96], in1=YS[0:96, cs], op=ALU.mult), reads=[u + "tq", u + "YS"], writes=[ypk])
                    S.dma("sp", Ygd.ap()[gc * 96:(gc + 1) * 96, cs], yp[0:96], reads=[ypk], writes=["Yg" + tag])
        if readout:
            TT = min(512, NT)
            yo = [A.alloc([TT], BF16) for _ in range(2)]
            sg = [A.alloc([TT], F32) for _ in range(2)]
            ygt = [A.alloc([4, TT], BF16) for _ in range(2)]
            for tt in range(NT // TT):
                yt, ytk = ygt[tt % 2], u + "ygt%d" % (tt % 2)
                S.dma("sp", yt[0:96], Ygd.ap()[:, tt * TT:(tt + 1) * TT].rearrange("(c p) t -> p c t", p=96), reads=["Yg" + tag], writes=[ytk], partial=False)
                for mo in range(4):
                    bc += 1
                    pst, pk = self.ps[bc % 2], self.psk[bc % 2]

                    def mmf(e, pst=pst, yt=yt, mo=mo):
                        ins = None
                        for cc in range(4):
                            ins = e.matmul(pst[0:96, 0:TT], lhsT=P["gluw"][0:96, cc, mo * 96:(mo + 1) * 96], rhs=yt[0:96, cc, :], start=(cc == 0), stop=(cc == 3))
                        return ins
                    S.op("pe", mmf, reads=[pu + "gluw", ytk], writes=[pk])
                    sgt, sk = sg[bc % 2], u + "sg%d" % (bc % 2)
                    S.op("act", lambda e, pst=pst, sgt=sgt, mo=mo: e.activation(out=sgt[0:96], in_=pst[0:96, 0:TT], func=AF.Sigmoid, bias=P["glub"][0:96, mo:mo + 1], scale=1.0),
                         reads=[pk, pu + "glub"], writes=[sk])
                    yot, yk = yo[bc % 2], u + "yo%d" % (bc % 2)
                    S.op("dve", lambda e, yot=yot, sgt=sgt, mo=mo, yt=yt: e.tensor_tensor(out=yot[0:96], in0=sgt[0:96], in1=yt[0:96, mo, :], op=ALU.mult),
                         reads=[sk, ytk], writes=[yk])
                    S.dma("sp", Ym.ap()[640 + mo * 96:640 + (mo + 1) * 96, tt * TT:(tt + 1) * TT], yot[0:96], reads=[yk], writes=["Ym" + tag])
        self.S.barrier()
        A.release(m0)

    def cast_weights(self, name, src_ap_2d, rows, cols):
        t = self.dscr(name, [rows, cols], BF16)
        step = 512
        for r0 in range(0, rows, step):
            r1 = min(rows, r0 + step)
            self.S.dma("pool", t.ap()[r0:r1, :], src_ap_2d[r0:r1, :], writes=[name])
        return t

    def cast_moe(self, l):
        I = self.I
        experts = []
        for e in range(self.ne_cast):
            experts.append((self.cast_weights("wg%d_%d" % (l, e), I["moe_w_gate"].ap()[0, e], D, DFFE),
                            self.cast_weights("wu%d_%d" % (l, e), I["moe_w_up"].ap()[0, e], D, DFFE),
                            self.cast_weights("wd%d_%d" % (l, e), I["moe_w_down"].ap()[0, e], DFFE, D), e))
        self._moe_experts = experts

    def phaseC(self, l, Xsrc, Xdst, L, tag, bl, moe, final):
        S, A, I = self.S, self.arena, self.I
        u = self.key("pC")
        NT = NS * L
        GT = min(512, L)
        TG = GT // 128
        Ym = self.scr["Ym" + tag]
        md = self.scr["modD%d" % l]
        if moe:
            if self._moe_experts is None:
                self.cast_moe(l)
            experts = self._moe_experts[:self.ne_comp]
            dff, FBW = DFFE, 512
        else:
            if ("wgd%d" % l) not in self.scr:
                self.cast_weights("wgd%d" % l, I["ffn_w_gate"].ap()[l // 2], D, DFF)
                self.cast_weights("wud%d" % l, I["ffn_w_up"].ap()[l // 2], D, DFF)
                self.cast_weights("wdd%d" % l, I["ffn_w_down"].ap()[l // 2], DFF, D)
            experts = [(self.scr["wgd%d" % l], self.scr["wud%d" % l], self.scr["wdd%d" % l], None)]
            dff, FBW = DFF, 256
        NCH = dff // 128
        FB = FBW // 128
        NBLK = dff // FBW
        DB = 4 if moe else 11
        NWD = 4 if moe else 2
        m0 = A.mark()
        self._nt_ss = A.alloc([4], F32)
        self._nt_junk = A.alloc([D], F32)
        wout = A.alloc([KC, D], BF16)
        grow = A.alloc([len(bl), 2, D], F32)
        S.dma("pool", wout, I["w_out"].ap()[l].rearrange("(kc p) n -> p kc n", p=128), writes=[u + "wout"])
        for bi, b in enumerate(bl):
            S.dma("sp", grow[:, bi, 0, :], md.ap()[b, 2 * D:3 * D].partition_broadcast(128), reads=["modD%d" % l], writes=[u + "grow"])
            S.dma("sp", grow[:, bi, 1, :], md.ap()[b, 5 * D:6 * D].partition_broadcast(128), reads=["modD%d" % l], writes=[u + "grow"])
        if final:
            fg = A.alloc([D], F32)
            S.dma("sp", fg, I["final_g"].ap().partition_broadcast(128), writes=[u + "fg"])
        if moe:
            rw = A.alloc([KC, NE], F32)
            rb = A.alloc([NE], F32)
            if SKIPR < 2 or SKIPR == 3:
                S.dma("sp", rw, I["moe_router_w"].ap()[0].rearrange("(kc p) n -> p kc n", p=128), writes=[u + "rw"])
                S.dma("sp", rb, I["moe_router_b"].ap()[0].partition_broadcast(128), writes=[u + "rb"])
            h32 = A.alloc([KC, 128], F32)
            comb = A.alloc([TG, NE], F32)
            zb = A.alloc([1], F32)
            tmpc = [A.alloc([512], F32) for _ in range(2)]
            S.op("pool", lambda e: e.memset(zb, 0.0), writes=[u + "zb"])
            rt = A.alloc([6, NE], F32)
        x1t = A.alloc([TG, D], F32)
        acc = A.alloc([TG, D], F32)
        ymg = A.alloc([KC, GT], BF16)
        hfT = A.alloc([KC, GT], BF16)
        actT = A.alloc([NCH, GT], BF16)
        NWB = 2 if moe else 4
        wgb = [A.alloc([KC, FBW], BF16) for _ in range(NWB)]
        wub = [A.alloc([KC, FBW], BF16) for _ in range(NWB)]
        wdb = [A.alloc([DB, 512], BF16) for _ in range(NWD)]
        sgt = [A.alloc([GT], F32) for _ in range(2)]
        xin = [A.alloc([D], F32) for _ in range(2)]
        wc = 0
        dc = 0
        pc = 0
        ec = 0
        for g in range(NT // GT):
            s_ = (g * GT) // L
            bi = s_ if len(bl) > 1 else 0
            b = bl[bi]
            S.dma("sp", ymg, Ym.ap()[:, g * GT:(g + 1) * GT].rearrange("(kc p) t -> p kc t", p=128), reads=["Ym" + tag], writes=[u + "ymg"], partial=False)
            for ti in range(TG):
                t = g * TG + ti
                xt, xk = xin[t % 2], u + "xin%d" % (t % 2)
                S.dma("sp", xt, Xsrc.ap()[t * 128:(t + 1) * 128, :], reads=[self.nm(Xsrc)], writes=[xk], partial=False)
                for half in range(2):
                    pst, pk = self.ps[half], self.psk[half]

                    def mmf(e, pst=pst, ti=ti, half=half):
                        ins = None
                        for kc in range(KC):
                            ins = e.matmul(pst, lhsT=ymg[:, kc, ti * 128:(ti + 1) * 128], rhs=wout[:, kc, half * 512:(half + 1) * 512], start=(kc == 0), stop=(kc == KC - 1))
                        return ins
                    S.op("pe", mmf, reads=[u + "ymg", u + "wout"], writes=[pk])
                    hs = slice(half * 512, (half + 1) * 512)
                    S.op("dve", lambda e, pst=pst, hs=hs, ti=ti, bi=bi: e.tensor_tensor(out=x1t[:, ti, hs], in0=pst, in1=grow[:, bi, 0, hs], op=ALU.mult),
                         reads=[pk, u + "grow"], writes=[u + "x1t%d" % ti], partial=True)
                S.op("dve", lambda e, ti=ti, xt=xt: e.tensor_tensor(out=x1t[:, ti, :], in0=x1t[:, ti, :], in1=xt, op=ALU.add),
                     reads=[u + "x1t%d" % ti, xk], writes=[u + "x1t%d" % ti])
            for ti in range(TG):
                self.norm_transpose(x1t[:, ti, :], u + "x1t%d" % ti, hfT, u + "hfT", ti * 128, 1, b, hT32=((h32, u + "h32") if (moe and SKIPR < 2) else None), single32=True)
                if moe and SKIPR:
                    pass
                if moe and SKIPR:
                    S.op("pool", lambda e, ti=ti: e.memset(comb[:, ti, :], 0.5), reads=[u + "h32"], writes=[u + "comb"])
                elif moe:
                    pst, pk = self.ps[2], self.psk[2]

                    def mmr(e, pst=pst):
                        ins = None
                        for kc in range(KC):
                            ins = e.matmul(pst[:, 0:NE], lhsT=h32[:, kc, :], rhs=rw[:, kc, :], start=(kc == 0), stop=(kc == KC - 1))
                        return ins
                    S.op("pe", mmr, reads=[u + "h32", u + "rw"], writes=[pk])
                    lg, m1, k1, l2, m2, k2 = (rt[:, i, :] for i in range(6))
                    rk_ = u + "rt"
                    dv_ = lambda fn, r=(), w=(rk_,): S.op("dve", fn, reads=[rk_] + list(r), writes=list(w))
                    dv_(lambda e, pst=pst: e.tensor_tensor(out=lg, in0=pst[:, 0:NE], in1=rb, op=ALU.add), [pk, u + "rb"])
                    dv_(lambda e: e.tensor_reduce(out=m1[:, 0:1], in_=lg, axis=AX.X, op=ALU.max))
                    dv_(lambda e: e.tensor_scalar(out=k1, in0=lg, scalar1=m1[:, 0:1], scalar2=None, op0=ALU.is_equal))
                    dv_(lambda e: e.scalar_tensor_tensor(out=l2, in0=k1, scalar=-1e30, in1=lg, op0=ALU.mult, op1=ALU.add))
                    dv_(lambda e: e.tensor_reduce(out=m2[:, 0:1], in_=l2, axis=AX.X, op=ALU.max))
                    dv_(lambda e: e.tensor_scalar(out=k2, in0=l2, scalar1=m2[:, 0:1], scalar2=None, op0=ALU.is_equal))
                    dv_(lambda e: e.tensor_tensor(out=m2[:, 1:2], in0=m2[:, 0:1], in1=m1[:, 0:1], op=ALU.subtract))
                    S.op("act", lambda e: e.activation(out=m2[:, 2:3], in_=m2[:, 1:2], func=AF.Exp), reads=[rk_], writes=[rk_])
                    dv_(lambda e: e.tensor_scalar(out=m2[:, 3:4], in0=m2[:, 2:3], scalar1=1.0, scalar2=None, op0=ALU.add))
                    dv_(lambda e: e.reciprocal(out=m2[:, 3:4], in_=m2[:, 3:4]))
                    dv_(lambda e: e.tensor_tensor(out=m2[:, 4:5], in0=m2[:, 2:3], in1=m2[:, 3:4], op=ALU.mult))
                    dv_(lambda e: e.tensor_scalar(out=k1, in0=k1, scalar1=m2[:, 3:4], scalar2=None, op0=ALU.mult))
                    dv_(lambda e, ti=ti: e.scalar_tensor_tensor(out=comb[:, ti, :], in0=k2, scalar=m2[:, 4:5], in1=k1, op0=ALU.mult, op1=ALU.add), w=(rk_, u + "comb"))
            for xi, (wg, wu, wd, eidx) in enumerate(experts):
                for fb in range(NBLK):
                    wc += 1
                    wgt, wut = wgb[wc % NWB], wub[wc % NWB]
                    wgk, wuk = u + "wg%d" % (wc % NWB), u + "wu%d" % (wc % NWB)
                    S.dma("sp", wgt, wg.ap()[:, fb * FBW:(fb + 1) * FBW].rearrange("(kc p) n -> p kc n", p=128), reads=[self.nm(wg)], writes=[wgk], partial=False)
                    S.dma("sp", wut, wu.ap()[:, fb * FBW:(fb + 1) * FBW].rearrange("(kc p) n -> p kc n", p=128), reads=[self.nm(wu)], writes=[wuk], partial=False)
                    for fc in range(FB):
                        pc += 1
                        pg, pgk = self.ps[(pc % 2) * 2], self.psk[(pc % 2) * 2]
                        pu_, puk = self.ps[(pc % 2) * 2 + 1], self.psk[(pc % 2) * 2 + 1]
                        for (pst, pk, wt, wk) in ((pg, pgk, wgt, wgk), (pu_, puk, wut, wuk)):
                            def mmf(e, pst=pst, wt=wt, fc=fc):
                                ins = None
                                for kc in range(KC):
                                    ins = e.matmul(pst[:, 0:GT], lhsT=wt[:, kc, fc * 128:(fc + 1) * 128], rhs=hfT[:, kc, :], start=(kc == 0), stop=(kc == KC - 1))
                                return ins
                            S.op("pe", mmf, reads=[wk, u + "hfT"], writes=[pk])
                        sg_, sgk = sgt[pc % 2], u + "sgt%d" % (pc % 2)
                        S.op("act", lambda e, sg_=sg_, pg=pg: e.activation(out=sg_, in_=pg[:, 0:GT], func=AF.Silu), reads=[pgk], writes=[sgk])
                        S.op("dve", lambda e, sg_=sg_, pu_=pu_, ch=fb * FB + fc: e.tensor_tensor(out=actT[:, ch, :], in0=sg_, in1=pu_[:, 0:GT], op=ALU.mult),
                             reads=[sgk, puk], writes=[u + "actT"], partial=True)
                for half in range(2):
                    hs = slice(half * 512, (half + 1) * 512)
                    for db in range(NCH // DB):
                        dc += 1
                        wdt, wdk = wdb[dc % NWD], u + "wd%d" % (dc % NWD)
                        S.dma("sp", wdt, wd.ap()[db * DB * 128:(db + 1) * DB * 128, hs].rearrange("(c p) n -> p c n", p=128), reads=[self.nm(wd)], writes=[wdk], partial=False)

                        def mmd(e, wdt=wdt, db=db):
                            ins = None
                            for cc in range(DB):
                                ch = db * DB + cc
                                for ti in range(TG):
                                    ins = e.matmul(self.ps[4 + ti], lhsT=actT[:, ch, ti * 128:(ti + 1) * 128], rhs=wdt[:, cc, :], start=(ch == 0), stop=(ch == NCH - 1))
                            return ins
                        S.op("pe", mmd, reads=[wdk, u + "actT"], writes=[self.psk[4 + ti] for ti in range(TG)], partial=(db > 0))
                    for ti in range(TG):
                        if eidx is None or SKIPC:
                            S.op("dve", lambda e, ti=ti, hs=hs: e.tensor_copy(out=acc[:, ti, hs], in_=self.ps[4 + ti]), reads=[self.psk[4 + ti]], writes=[u + "acc%d" % ti], partial=True)
                        elif xi == 0:
                            S.op("act", lambda e, ti=ti, hs=hs, eidx=eidx: e.activation(out=acc[:, ti, hs], in_=self.ps[4 + ti], func=AF.Identity,
                                                                                      scale=comb[:, ti, eidx:eidx + 1], bias=zb[:, 0:1]),
                                 reads=[self.psk[4 + ti], u + "comb", u + "zb"], writes=[u + "acc%d" % ti], partial=True)
                        else:
                            ec += 1
                            tmp, tk = tmpc[ec % 2], u + "tmpc%d" % (ec % 2)
                            S.op("act", lambda e, ti=ti, tmp=tmp, eidx=eidx: e.activation(out=tmp, in_=self.ps[4 + ti], func=AF.Identity,
                                                                                        scale=comb[:, ti, eidx:eidx + 1], bias=zb[:, 0:1]),
                                 reads=[self.psk[4 + ti], u + "comb", u + "zb"], writes=[tk])
                            S.op("dve", lambda e, ti=ti, hs=hs, tmp=tmp: e.tensor_tensor(out=acc[:, ti, hs], in0=acc[:, ti, hs], in1=tmp, op=ALU.add),
                                 reads=[tk, u + "acc%d" % ti], writes=[u + "acc%d" % ti])
            for ti in range(TG):
                t = g * TG + ti
                ak = u + "acc%d" % ti
                S.op("dve", lambda e, ti=ti, bi=bi: e.tensor_tensor(out=acc[:, ti, :], in0=acc[:, ti, :], in1=grow[:, bi, 1, :], op=ALU.mult), reads=[ak, u + "grow"], writes=[ak])
                S.op("dve", lambda e, ti=ti: e.tensor_tensor(out=acc[:, ti, :], in0=acc[:, ti, :], in1=x1t[:, ti, :], op=ALU.add), reads=[ak, u + "x1t%d" % ti], writes=[ak])
                if final:
                    ss = self._nt_ss
                    S.op("act", lambda e, ti=ti: e.activation(out=self._nt_junk, in_=acc[:, ti, :], func=AF.Square, accum_out=ss[:, 0:1]), reads=[ak], writes=["nt_ss", "nt_junk"])
                    S.op("act", lambda e: e.activation(out=ss[:, 1:2], in_=ss[:, 0:1], func=AF.Sqrt, scale=1.0 / D, bias=self.epsb[:, 0:1]), reads=["nt_ss", "epsb"], writes=["nt_ss"])
                    S.op("dve", lambda e: e.reciprocal(out=ss[:, 2:3], in_=ss[:, 1:2]), reads=["nt_ss"], writes=["nt_rs"])
                    S.op("dve", lambda e, ti=ti: e.scalar_tensor_tensor(out=acc[:, ti, :], in0=acc[:, ti, :], scalar=ss[:, 2:3], in1=fg, op0=ALU.mult, op1=ALU.mult),
                         reads=[ak, "nt_rs", u + "fg"], writes=[ak])
                S.dma("sp", Xdst.ap()[t * 128:(t + 1) * 128, :], acc[:, ti, :], reads=[ak], writes=[self.nm(Xdst)])
        self.S.barrier()
        A.release(m0)

    def program(self):
        S = self.S
        stop = self.stop_after
        xres = self.dscr("xres", [NS * L_LAT, D], F32)
        cres = self.dscr("cres", [NS * L_CTX, D], F32)
        for l in range(DEPTH):
            last = (l == DEPTH - 1)
            S.phase = "prep%d" % l
            self.prep_mod(l)
            self.prep_fnet(l)
            S.barrier()
            csrc = self.I["ctx"] if l == 0 else cres
            S.phase = "ctx%d" % l
            self.phaseA(l, csrc, L_CTX, False, lambda s: 2, "c")
            if not last:
                self.fnet(l, L_CTX, "c")
                self.hyena_filters(l, L_CTX, "c")
                self.hyena(l, L_CTX, "c")
            ms = self.arena.mark()
            P = self.s5_prep(l)
            self.s5_main(l, L_CTX, "c", P, False, not last)
            S.op("act", lambda e: e.copy(out=self.s0, in_=self.s0n), reads=["s0n"], writes=["s0"])
            S.barrier()
            self.arena.release(ms)
            if not last:
                self.phaseC(l, csrc, cres, L_CTX, "c", [2], False, False)
            if stop == "C%d" % l:
                break
            xsrc = self.I["x"] if l == 0 else xres
            xdst = self.out if last else xres
            S.phase = "xA%d" % l
            self.phaseA(l, xsrc, L_LAT, True, lambda s: s, "x")
            if stop == "xA%d" % l:
                break
            S.phase = "xF%d" % l
            self.fnet(l, L_LAT, "x")
            if stop == "xF%d" % l:
                break
            S.phase = "xG%d" % l
            self.hyena_filters(l, L_LAT, "x")
            if stop == "xG%d" % l:
                break
            S.phase = "xH%d" % l
            self.hyena(l, L_LAT, "x")
            if stop == "xH%d" % l:
                break
            ms = self.arena.mark()
            S.phase = "xS%d" % l
            P = self.s5_prep(l)
            if l == 0 and DEPTH > 1 and stop is None:
                self.cast_moe(1)
            self.s5_main(l, L_LAT, "x", P, True, True)
            S.barrier()
            self.arena.release(ms)
            if stop == "M%d" % l:
                break
            S.phase = "xC%d" % l
            self.phaseC(l, xsrc, xdst, L_LAT, "x", [0, 1], last, last)
            if stop == "L%d" % l:
                break
        self.finish()

    def finish(self):
        S = self.S
        keys = [k for k in S.res.keys() if k in self.debug or k == "out"]
        toks = []
        for k in list(S.res.keys()):
            r = S.res[k]
            toks += r["w"] + r["r"]
        waits = S._waits("sp", toks)
        S.q["sp"].append((waits, None, None, 0, S.phase))
        S.emit()
        self.st.close()


_CACHE = {}


def make_in_maps(inputs, cores=range(NCORES)):
    cst = _CACHE.get("consts")
    if cst is None:
        cst = _consts()
        _CACHE["consts"] = cst
    maps = []
    f32 = lambda a: np.ascontiguousarray(np.asarray(a, dtype=np.float32))
    shared = {}
    for name, shape in IN_SPECS:
        if name in ("x", "ctx", "cvec"):
            continue
        shared[name] = f32(inputs[name]).reshape(shape)
    x = np.asarray(inputs["x"])
    ctx = np.asarray(inputs["ctx"])
    c = np.asarray(inputs["c"])
    c_ctx = np.asarray(inputs["c_ctx"])
    for ci in cores:
        m = dict(shared)
        m.update(cst)
        m["x"] = f32(x[ci * NS:(ci + 1) * NS]).reshape(NS * L_LAT, D)
        m["ctx"] = f32(ctx[ci * NS:(ci + 1) * NS]).reshape(NS * L_CTX, D)
        cc = np.stack([c[ci * NS], c[ci * NS + 1], c_ctx], axis=0)
        m["cvec"] = f32(cc.T.reshape(KC, 128, 3).transpose(1, 0, 2))
        maps.append(m)
    return maps


def kernel(**inputs):
    B = _CACHE.get("B")
    if B is None:
        B = Builder()
        B.program()
        _CACHE["B"] = B
    maps = make_in_maps(inputs)
    res = run_bass_kernel_spmd(B.nc, maps, core_ids=list(range(NCORES)))
    out = np.concatenate([r["out"].reshape(NS, L_LAT, D) for r in res.results], axis=0)
    return out.astype(np.float32)
```

```python
import math
from contextlib import ExitStack
import numpy as np
import ml_dtypes
import concourse.bass as bass
import concourse.mybir as mybir
from concourse.bass_utils import run_bass_kernel_spmd

F32 = mybir.dt.float32
BF16 = mybir.dt.bfloat16
I32 = mybir.dt.int32
AF = mybir.ActivationFunctionType
ALU = mybir.AluOpType
AX = mybir.AxisListType

import os
SKIPR = int(os.environ.get('SKIPR', 0))
SKIPC = int(os.environ.get('SKIPC', 0))
SCOPES = int(os.environ.get('SCOPES', 0))
NCORES = 8
NS = 2
D = 1024
KC = 8
L_LAT = 4096
L_CTX = 256
DEPTH = 2
D_IN = 1792
HY_OFF = 256
S5_OFF = 1408
DFF = 2816
DFFE = 3584
NE = 8
EPS = 1e-6
TWO_PI = 2.0 * math.pi


class Sched:
    ENG = ("pe", "act", "dve", "pool", "sp")

    def __init__(self, nc):
        self.nc = nc
        self.q = {e: [] for e in self.ENG}
        self.ecount = {e: 0 for e in self.ENG}
        self.seen = {e: {} for e in self.ENG}
        self.res = {}
        self.dcount = {}
        self.semnames = ["c_pe", "c_act", "c_dve", "c_pool"]
        self.keysem = {}
        self.free_sems = []
        self.phase = "init"

    def _r(self, k):
        r = self.res.get(k)
        if r is None:
            r = dict(w=[], r=[], pw=[], pr=[], partial=False)
            self.res[k] = r
        return r

    def _deps(self, reads, writes, partial):
        deps = []
        for k in reads:
            deps += self._r(k)["w"]
        joins = []
        for k in writes:
            r = self._r(k)
            if partial and r["partial"] and not r["r"] and r["w"]:
                deps += r["pw"] + r["pr"]
                joins.append(k)
            else:
                deps += r["w"] + r["r"]
        return deps, joins

    def _commit(self, tok, reads, writes, partial, joins):
        for k in reads:
            rr = self._r(k)["r"]
            rr.append(tok)
            if len(rr) > 64:
                mx = {}
                for (s, v) in rr:
                    if mx.get(s, 0) < v:
                        mx[s] = v
                rr[:] = list(mx.items())
        for k in writes:
            r = self._r(k)
            if k in joins:
                r["w"].append(tok)
                if len(r["w"]) > 64:
                    mx = {}
                    for (s, v) in r["w"]:
                        if mx.get(s, 0) < v:
                            mx[s] = v
                    r["w"][:] = list(mx.items())
            else:
                r["pw"], r["pr"] = r["w"], r["r"]
                r["w"], r["r"] = [tok], []
                r["partial"] = partial

    def _waits(self, eng, deps, skip_self=False):
        need = {}
        for (s, v) in deps:
            if skip_self and s == "c_" + eng:
                continue
            if self.seen[eng].get(s, 0) >= v:
                continue
            if need.get(s, 0) < v:
                need[s] = v
        for s, v in need.items():
            self.seen[eng][s] = v
        return list(need.items())

    def op(self, eng, fn, reads=(), writes=(), partial=False):
        deps, joins = self._deps(reads, writes, partial)
        waits = self._waits(eng, deps, skip_self=(eng == "pe"))
        self.ecount[eng] += 1
        tok = ("c_" + eng, self.ecount[eng])
        self.q[eng].append((waits, fn, tok, 1, self.phase))
        self._commit(tok, reads, writes, partial, joins)
        return tok

    def dma(self, eng, out, in_, reads=(), writes=(), partial=True, **kw):
        assert len(writes) == 1
        k = writes[0]
        deps, joins = self._deps(reads, writes, partial)
        waits = self._waits(eng, deps)
        sname = self.keysem.get(k)
        if sname is None:
            if self.free_sems:
                sname = self.free_sems.pop()
            else:
                sname = "d_%d" % len(self.dcount)
                self.dcount[sname] = 0
                self.semnames.append(sname)
            self.keysem[k] = sname
        self.dcount[sname] += 16
        tok = (sname, self.dcount[sname])

        def fn(e, out=out, in_=in_, kw=kw):
            return e.dma_start(out=out, in_=in_, **kw)

        self.q[eng].append((waits, fn, tok, 16, self.phase))
        self._commit(tok, reads, writes, partial, joins)
        return tok

    def barrier(self):
        toks = []
        for e in ("pe", "act", "dve", "pool"):
            if self.ecount[e]:
                toks.append(("c_" + e, self.ecount[e]))
        for s, v in self.dcount.items():
            if v:
                toks.append((s, v))
        for e in self.ENG:
            waits = self._waits(e, toks)
            if waits:
                self.q[e].append((waits, None, None, 0, self.phase))
        self.keysem = {}
        self.free_sems = list(self.dcount.keys())

    def emit(self):
        nc = self.nc
        with ExitStack() as st:
            sems = {}
            for i, s in enumerate(self.semnames):
                sems[s] = st.enter_context(nc.semaphore("s%d" % i))
            block = st.enter_context(nc.Block())
            engs = {"pe": block.tensor, "act": block.scalar, "dve": block.vector,
                    "pool": block.gpsimd, "sp": block.sync}
            for ename, deco in engs.items():
                items = self.q[ename]

                def body(e, items=items):
                    cur = None
                    cm = None
                    for (waits, fn, tok, inc, ph) in items:
                        if SCOPES and ph != cur:
                            if cm is not None:
                                cm.__exit__(None, None, None)
                            cm = nc.named_scope(ph)
                            cm.__enter__()
                            cur = ph
                        for (s, v) in waits:
                            e.wait_ge(sems[s], v)
                        if fn is None:
                            continue
                        ins = fn(e)
                        ins.then_inc(sems[tok[0]], inc)
                    if cm is not None:
                        cm.__exit__(None, None, None)
                deco(body)


def _dsize(dt):
    return {F32: 4, BF16: 2, I32: 4}[dt]


class Arena:
    def __init__(self, nc, st, nbytes):
        self.t = st.enter_context(nc.sbuf_tensor("arena", [128, nbytes // 4], F32))
        self.nbytes = nbytes
        self.off = 0
        self.uid = 0

    def mark(self):
        return self.off

    def release(self, m):
        self.off = m

    def alloc(self, free_shape, dt, parts=128):
        n = int(np.prod(free_shape))
        size = (n * _dsize(dt) + 31) // 32 * 32
        assert self.off + size <= self.nbytes, ("arena overflow", self.off, size)
        a = self.t[0:parts, self.off // 4:(self.off + size) // 4]
        if dt != F32:
            a = a.bitcast(dt)
        a = a[:, 0:n]
        if len(free_shape) == 2:
            a = a.rearrange("p (a b) -> p a b", a=free_shape[0])
        elif len(free_shape) == 3:
            a = a.rearrange("p (a b c) -> p a b c", a=free_shape[0], b=free_shape[1])
        self.off += size
        self.uid += 1
        return a


def _consts():
    c = {}
    for L in (L_LAT, L_CTX):
        t = np.arange(L, dtype=np.int64)
        tf = (np.outer(t, t) % L).astype(np.float64) * (2.0 * np.pi / L)
        c["dftc%d" % L] = (np.cos(tf) / np.sqrt(L)).astype(ml_dtypes.bfloat16)
        c["dfts%d" % L] = (-np.sin(tf) / np.sqrt(L)).astype(ml_dtypes.bfloat16)
        tt = np.linspace(0.0, 1.0, L, dtype=np.float32)[:, None]
        w = (np.float32(2.0 * math.pi / L) * np.arange(L, dtype=np.float32))[:, None]
        bands = np.linspace(1e-4, 15, 16, dtype=np.float32)[None, :]
        z = np.concatenate([tt, np.cos(bands * w), -np.sin(bands * w)], axis=-1).astype(np.float32)
        c["zpos%d" % L] = np.ascontiguousarray(z.T)
        max_decay = math.log(0.01) / 0.3
        min_decay = math.log(0.01) / 1.5
        deltas = np.abs(np.linspace(min_decay, max_decay, 384, dtype=np.float32))
        c["dec%d" % L] = np.exp(-tt.T.astype(np.float32) * deltas[:, None]).astype(np.float32)
    k = np.arange(64)
    a = np.outer(k, k) * (2.0 * np.pi / 64)
    c64 = np.cos(a) / 8.0
    s64 = np.sin(a) / 8.0
    z64 = np.zeros((64, 64))
    c["c64bd"] = np.block([[c64, z64], [z64, c64]]).astype(np.float32)
    c["s64bd"] = np.block([[s64, z64], [z64, s64]]).astype(np.float32)
    ii = np.arange(128) // 16
    c["mask_le"] = (ii[:, None] <= ii[None, :]).astype(np.float32)
    c["mask_ge"] = (ii[:, None] >= ii[None, :]).astype(np.float32)
    c["ident"] = np.eye(128, dtype=np.float32)
    pw = list(range(-7, 9)) + [8 * (2 ** k) for k in range(10)]
    c["s5pw"] = np.tile(np.array(pw, dtype=np.float32)[None, :], (128, 1))
    return c


S5PW = list(range(-7, 9)) + [8 * (2 ** k) for k in range(10)]


def PWI(n):
    return S5PW.index(n)


IN_SPECS = [
    ("x", [NS * L_LAT, D]), ("ctx", [NS * L_CTX, D]), ("cvec", [128, KC, 3]),
    ("ada_w", [DEPTH, D, 6 * D]), ("ada_b", [DEPTH, 6 * D]),
    ("norm_mix_g", [DEPTH, D]), ("norm_ffn_g", [DEPTH, D]),
    ("w_in", [DEPTH, D, D_IN]), ("w_out", [DEPTH, D, D]),
    ("fnet_w", [DEPTH, 4, 64, 64]),
    ("hy_conv_w", [DEPTH, 3, 1152]), ("hy_conv_b", [DEPTH, 1152]),
    ("hy_fw1", [DEPTH, 33, 64]), ("hy_fb1", [DEPTH, 64]), ("hy_freq1", [DEPTH, 64]),
    ("hy_fw2", [DEPTH, 64, 64]), ("hy_fb2", [DEPTH, 64]), ("hy_freq2", [DEPTH, 64]),
    ("hy_fw3", [DEPTH, 64, 1536]), ("hy_bias", [DEPTH, 2, 384]),
    ("s5_a_re", [DEPTH, 2, 24, 64]), ("s5_a_im", [DEPTH, 2, 24, 64]), ("s5_log_dt", [DEPTH, 2, 24]),
    ("s5_b_re", [DEPTH, 2, 24, 64, 16]), ("s5_b_im", [DEPTH, 2, 24, 64, 16]),
    ("s5_c_re", [DEPTH, 2, 24, 16, 64]), ("s5_c_im", [DEPTH, 2, 24, 16, 64]),
    ("s5_d", [DEPTH, 384]), ("s5_glu_w", [DEPTH, 384, 384]), ("s5_glu_b", [DEPTH, 384]),
    ("ffn_w_gate", [1, D, DFF]), ("ffn_w_up", [1, D, DFF]), ("ffn_w_down", [1, DFF, D]),
    ("moe_router_w", [1, D, NE]), ("moe_router_b", [1, NE]),
    ("moe_w_gate", [1, NE, D, DFFE]), ("moe_w_up", [1, NE, D, DFFE]), ("moe_w_down", [1, NE, DFFE, D]),
    ("final_g", [D]),
]
CONST_SPECS = [
    ("dftc4096", [4096, 4096], BF16), ("dfts4096", [4096, 4096], BF16),
    ("dftc256", [256, 256], BF16), ("dfts256", [256, 256], BF16),
    ("zpos4096", [33, 4096], F32), ("zpos256", [33, 256], F32),
    ("dec4096", [384, 4096], F32), ("dec256", [384, 256], F32),
    ("c64bd", [128, 128], F32), ("s64bd", [128, 128], F32),
    ("mask_le", [128, 128], F32), ("mask_ge", [128, 128], F32),
    ("ident", [128, 128], F32), ("s5pw", [128, 26], F32),
]


class Builder:
    def __init__(self, debug=(), stop_after=None, ne_cast=NE, ne_comp=NE):
        self.ne_cast = ne_cast
        self.ne_comp = ne_comp
        self.debug = set(debug)
        self.stop_after = stop_after
        nc = bass.Bass("TRN2", target_bir_lowering=False)
        self.nc = nc
        self.S = Sched(nc)
        self.st = ExitStack()
        self.I = {}
        for name, shape in IN_SPECS:
            self.I[name] = nc.dram_tensor(name, shape, F32, kind="ExternalInput")
        for name, shape, dt in CONST_SPECS:
            self.I[name] = nc.dram_tensor(name, shape, dt, kind="ExternalInput")
        self.out = nc.dram_tensor("out", [NS * L_LAT, D], F32, kind="ExternalOutput")
        self.scr = {}
        self.names = {id(self.out): "out"}
        self.arena = Arena(nc, self.st, 192 * 1024)
        self.ps = [self.st.enter_context(nc.psum_tensor("psb%d" % i, [128, 512], F32))[:] for i in range(8)]
        self.psk = ["ps%d" % i for i in range(8)]
        self.uid = 0
        self._moe_experts = None
        A = self.arena
        self.ident = A.alloc([128], F32)
        self.identb = A.alloc([128], BF16)
        self.ones = A.alloc([128], F32)
        self.epsb = A.alloc([1], F32)
        self.Am = [A.alloc([KC, 3], F32) for _ in range(2)]
        self.Bm = [A.alloc([KC, 3], F32) for _ in range(2)]
        self.s0 = A.alloc([48, NS], F32)
        self.RHSf = A.alloc([2, 256], BF16)
        self.s0n = A.alloc([48, NS], F32)
        S = self.S
        S.dma("sp", self.ident, self.I["ident"].ap(), writes=["ident"])
        S.op("dve", lambda e: e.tensor_copy(out=self.identb, in_=self.ident), reads=["ident"], writes=["identb"])
        S.op("pool", lambda e: e.memset(self.ones, 1.0), writes=["ones"])
        S.op("pool", lambda e: e.memset(self.epsb, EPS), writes=["epsb"])

    def dscr(self, name, shape, dt):
        kind = "ExternalOutput" if name in self.debug else "Internal"
        t = self.nc.dram_tensor(name, shape, dt, kind=kind)
        self.scr[name] = t
        self.names[id(t)] = name
        return t

    def nm(self, t):
        return self.names.get(id(t), "input")

    def dump(self, name, ap, key, shape, dt):
        if name not in self.debug:
            return
        t = self.dscr(name, shape, dt)
        self.S.dma("sp", t.ap(), ap, reads=[key], writes=["dbg_" + name])

    def key(self, base):
        self.uid += 1
        return "%s#%d" % (base, self.uid)

    def range_reduce_sin(self, eng, out, ang, tmp_i, tmp_f, keys_r, key_w, key_i, key_f, shift=0.0):
        S = self.S
        S.op(eng, lambda e: e.tensor_scalar(out=tmp_i, in0=ang, scalar1=shift, scalar2=1.0 / TWO_PI, op0=ALU.add, op1=ALU.mult),
             reads=keys_r, writes=[key_i])
        S.op(eng, lambda e: e.tensor_copy(out=tmp_f, in_=tmp_i), reads=[key_i], writes=[key_f])
        S.op(eng, lambda e: e.scalar_tensor_tensor(out=tmp_f, in0=tmp_f, scalar=-TWO_PI, in1=ang, op0=ALU.mult, op1=ALU.add),
             reads=[key_f] + list(keys_r), writes=[key_f])
        S.op(eng, lambda e: e.tensor_scalar(out=tmp_f, in0=tmp_f, scalar1=shift, scalar2=math.pi, op0=ALU.add, op1=ALU.min),
             reads=[key_f], writes=[key_f])
        S.op(eng, lambda e: e.tensor_scalar(out=tmp_f, in0=tmp_f, scalar1=-math.pi, scalar2=None, op0=ALU.max),
             reads=[key_f], writes=[key_f])
        S.op("act", lambda e: e.activation(out=out, in_=tmp_f, func=AF.Sin), reads=[key_f], writes=[key_w], partial=True)

    def prep_mod(self, l):
        S, A, I = self.S, self.arena, self.I
        m0 = A.mark()
        cv = A.alloc([KC, 3], F32)
        adab = A.alloc([48], F32)
        gmix = A.alloc([KC], F32)
        gffn = A.alloc([KC], F32)
        modT = A.alloc([48, 3], F32)
        wt = [A.alloc([KC, 512], F32) for _ in range(2)]
        u = self.key("pm")
        S.dma("sp", cv, I["cvec"].ap(), writes=[u + "cv"])
        S.op("act", lambda e: e.activation(out=cv, in_=cv, func=AF.Silu), reads=[u + "cv"], writes=[u + "cv"])
        S.dma("sp", adab, I["ada_b"].ap()[l].rearrange("(f p) -> p f", p=128), writes=[u + "adab"], allow_slow_non_contiguous=True)
        S.dma("sp", gmix, I["norm_mix_g"].ap()[l].rearrange("(f p) -> p f", p=128), writes=[u + "gmix"], allow_slow_non_contiguous=True)
        S.dma("sp", gffn, I["norm_ffn_g"].ap()[l].rearrange("(f p) -> p f", p=128), writes=[u + "gffn"], allow_slow_non_contiguous=True)
        for blk in range(12):
            w = wt[blk % 2]
            wk = u + "wt%d" % (blk % 2)
            S.dma("sp", w, I["ada_w"].ap()[l][:, blk * 512:(blk + 1) * 512].rearrange("(kc p) n -> p kc n", p=128), writes=[wk])
            pst = self.ps[blk % 2]
            pk = self.psk[blk % 2]

            def mmf(e, w=w, pst=pst):
                ins = None
                for m in range(4):
                    for kc in range(KC):
                        ins = e.matmul(pst[:, m * 3:(m + 1) * 3], lhsT=w[:, kc, m * 128:(m + 1) * 128], rhs=cv[:, kc, :],
                                       start=(kc == 0), stop=(kc == KC - 1))
                return ins
            S.op("pe", mmf, reads=[wk, u + "cv"], writes=[pk])
            S.op("dve", lambda e, pst=pst, blk=blk: e.tensor_tensor(
                out=modT[:, blk * 4:(blk + 1) * 4, :], in0=pst[:, 0:12].rearrange("p (m b) -> p m b", m=4),
                in1=adab[:, blk * 4:(blk + 1) * 4].unsqueeze(2).to_broadcast([128, 4, 3]), op=ALU.add),
                reads=[pk, u + "adab"], writes=[u + "modT"], partial=True)
        for wi, (gv, gk, so, ho) in enumerate(((gmix, u + "gmix", 8, 0), (gffn, u + "gffn", 32, 24))):
            S.op("dve", lambda e, wi=wi, so=so: e.tensor_scalar(out=self.Am[wi], in0=modT[:, so:so + 8, :], scalar1=1.0, scalar2=None, op0=ALU.add),
                 reads=[u + "modT"], writes=["Am%d" % wi])
            S.op("dve", lambda e, wi=wi, gv=gv: e.tensor_tensor(out=self.Am[wi], in0=self.Am[wi], in1=gv.unsqueeze(2).to_broadcast([128, 8, 3]), op=ALU.mult),
                 reads=["Am%d" % wi, gk], writes=["Am%d" % wi])
            S.op("dve", lambda e, wi=wi, ho=ho: e.tensor_copy(out=self.Bm[wi], in_=modT[:, ho:ho + 8, :]),
                 reads=[u + "modT"], writes=["Bm%d" % wi])
        md = self.scr.get("modD%d" % l) or self.dscr("modD%d" % l, [3, 6 * D], F32)
        for b in range(3):
            S.dma("sp", md.ap()[b].rearrange("(f p) -> p f", p=128), modT[:, :, b], reads=[u + "modT"], writes=["modD%d" % l],
                  allow_slow_non_contiguous=True)
        self.S.barrier()
        A.release(m0)

    def norm_transpose(self, xt, xk, hT, hk, col0, which, b, hT32=None, single32=False):
        S = self.S
        u = self.key("nt")
        ss = self._nt_ss
        junk = self._nt_junk
        S.op("act", lambda e: e.activation(out=junk, in_=xt, func=AF.Square, accum_out=ss[:, 0:1]), reads=[xk], writes=["nt_ss", "nt_junk"])
        S.op("act", lambda e: e.activation(out=ss[:, 1:2], in_=ss[:, 0:1], func=AF.Sqrt, scale=1.0 / D, bias=self.epsb[:, 0:1]),
             reads=["nt_ss", "epsb"], writes=["nt_ss"])
        S.op("dve", lambda e: e.reciprocal(out=ss[:, 2:3], in_=ss[:, 1:2]), reads=["nt_ss"], writes=["nt_rs"])
        S.op("dve", lambda e: e.tensor_scalar(out=junk, in0=xt, scalar1=ss[:, 2:3], scalar2=None, op0=ALU.mult),
             reads=[xk, "nt_rs"], writes=["nt_junk"])
        for half in range(2):
            pst = self.ps[6 + half]
            pk = self.psk[6 + half]

            def tr(e, half=half, pst=pst):
                ins = None
                for q in range(4):
                    kc = half * 4 + q
                    ins = e.transpose(pst[:, q * 128:(q + 1) * 128], junk[:, kc * 128:(kc + 1) * 128], self.ident)
                return ins
            S.op("pe", tr, reads=["nt_junk", "ident"], writes=[pk])
            for q in range(4):
                kc = half * 4 + q
                S.op("act", lambda e, q=q, kc=kc, pst=pst: e.activation(
                    out=hT[:, kc, col0:col0 + 128], in_=pst[:, q * 128:(q + 1) * 128], func=AF.Identity,
                    scale=self.Am[which][:, kc, b:b + 1], bias=self.Bm[which][:, kc, b:b + 1]),
                    reads=[pk, "Am%d" % which, "Bm%d" % which], writes=[hk], partial=True)
                if hT32 is not None:
                    S.op("act", lambda e, q=q, kc=kc, pst=pst: e.activation(
                        out=(hT32[0][:, kc, :] if single32 else hT32[0][:, kc, col0:col0 + 128]), in_=pst[:, q * 128:(q + 1) * 128],
                        func=AF.Identity, scale=self.Am[which][:, kc, b:b + 1], bias=self.Bm[which][:, kc, b:b + 1]),
                        reads=[pk, "Am%d" % which, "Bm%d" % which], writes=[hT32[1]], partial=True)

    def prep_fnet(self, l):
        S, A, I = self.S, self.arena, self.I
        u = self.key("pf")
        m0 = A.mark()
        cbd = A.alloc([128], F32)
        sbd = A.alloc([128], F32)
        wbd = A.alloc([2, 128], F32)
        S.dma("sp", cbd, I["c64bd"].ap(), writes=[u + "cbd"])
        S.dma("sp", sbd, I["s64bd"].ap(), writes=[u + "sbd"])
        S.op("pool", lambda e: e.memset(wbd, 0.0), writes=[u + "wbd"])
        for h in range(4):
            j, hh = h // 2, h % 2
            S.dma("sp", wbd[hh * 64:(hh + 1) * 64, j, hh * 64:(hh + 1) * 64], I["fnet_w"].ap()[l, h], reads=[], writes=[u + "wbd"])
        for j in range(2):
            pst = self.ps[j]
            pk = self.psk[j]

            def mmf(e, j=j, pst=pst):
                e.matmul(pst[:, 0:128], lhsT=cbd, rhs=wbd[:, j, :], start=True, stop=True)
                return e.matmul(pst[:, 128:256], lhsT=sbd, rhs=wbd[:, j, :], start=True, stop=True)
            S.op("pe", mmf, reads=[u + "cbd", u + "sbd", u + "wbd"], writes=[pk])
            S.op("dve", lambda e, j=j, pst=pst: e.tensor_copy(out=self.RHSf[:, j, :], in_=pst[:, 0:256]), reads=[pk], writes=["RHSf"], partial=True)
        self.S.barrier()
        A.release(m0)

    def phaseA(self, l, Xd, L, grid, bfun, tag):
        S, A, I = self.S, self.arena, self.I
        u = self.key("pA")
        NT = NS * L
        GT = min(512, L)
        TG = GT // 128
        R = 64 if grid else L
        NBk = L // 8
        ABd = self.scr.get("AB" + tag) or self.dscr("AB" + tag, [NT, 512], BF16)
        Hq = self.scr.get("Hq" + tag) or self.dscr("Hq" + tag, [3, NT, 384], BF16)
        Xs = self.scr.get("Xs" + tag) or self.dscr("Xs" + tag, [8, 384, NS, NBk], BF16)
        m0 = A.mark()
        self._nt_ss = A.alloc([4], F32)
        self._nt_junk = A.alloc([D], F32)
        win = A.alloc([KC, 256 + 384], BF16)
        wtap = A.alloc([KC, 3, 1152], BF16)
        cwb = A.alloc([3, 1152], F32)
        cbb = A.alloc([1152], F32)
        S.dma("pool", win[:, :, 0:256], I["w_in"].ap()[l][:, 0:256].rearrange("(kc p) n -> p kc n", p=128), writes=[u + "win"])
        S.dma("pool", win[:, :, 256:640], I["w_in"].ap()[l][:, S5_OFF:D_IN].rearrange("(kc p) n -> p kc n", p=128), writes=[u + "win"])
        S.dma("sp", cwb, I["hy_conv_w"].ap()[l].rearrange("t n -> (t n)").partition_broadcast(128), writes=[u + "cwb"])
        S.dma("sp", cbb, I["hy_conv_b"].ap()[l].partition_broadcast(128), writes=[u + "cbb"])
        m1 = A.mark()
        wf32 = A.alloc([1152], F32)
        for kc in range(KC):
            S.dma("sp", wf32, I["w_in"].ap()[l][kc * 128:(kc + 1) * 128, HY_OFF:S5_OFF], writes=[u + "wf32"], partial=False)
            for tap in range(3):
                S.op("dve" if tap != 1 else "pool", lambda e, kc=kc, tap=tap: e.tensor_tensor(out=wtap[:, kc, tap, :], in0=wf32, in1=cwb[:, tap, :], op=ALU.mult),
                     reads=[u + "wf32", u + "cwb"], writes=[u + "wtap"], partial=True)
        self.dump("dbg_wtap", wtap[:, 0, :, :], u + "wtap", [128, 3, 1152], BF16)
        self.dump("dbg_cwb", cwb, u + "cwb", [128, 3, 1152], F32)
        self.dump("dbg_rhsf", self.RHSf, "RHSf", [128, 2, 256], BF16)
        self.S.barrier()
        A.release(m1)
        xts = [A.alloc([D], F32) for _ in range(2)]
        hT = [[A.alloc([KC, GT], BF16) for _ in range(3)] for _ in range(2)]
        hR = [A.alloc([KC, GT], BF16) for _ in range(3)]
        pfT = A.alloc([2, GT], BF16)
        abt = [A.alloc([512], BF16) for _ in range(4)]
        hqt = [A.alloc([384], BF16) for _ in range(4)]
        psd = [A.alloc([8, GT // 8], BF16) for _ in range(4)]
        for bi in range(2):
            for tp in (0, 2):
                S.op("pool", lambda e, bi=bi, tp=tp: e.memset(hT[bi][tp], 0.0), writes=[u + "hT%d_%d" % (bi, tp)])
        ngroups = NT // GT
        cnt = 0
        for g in range(ngroups):
            bi = g % 2
            s = (g * GT) // L
            b = bfun(s)
            hc, hm, hp = hT[bi][1], hT[bi][0], hT[bi][2]
            hck, hmk, hpk = [u + "hT%d_%d" % (bi, t) for t in (1, 0, 2)]
            for ti in range(TG):
                t = g * TG + ti
                xt = xts[t % 2]
                xk = u + "xt%d" % (t % 2)
                S.dma("sp", xt, Xd.ap()[t * 128:(t + 1) * 128, :], reads=[self.nm(Xd)], writes=[xk], partial=False)
                self.norm_transpose(xt, xk, hc, hck, ti * 128, 0, b)
            hcv = hc.rearrange("p k (r c) -> p (k r) c", c=R)
            hmv = hm.rearrange("p k (r c) -> p (k r) c", c=R)
            hpv = hp.rearrange("p k (r c) -> p (k r) c", c=R)
            S.op("dve", lambda e, hmv=hmv, hcv=hcv: e.tensor_copy(out=hmv[:, :, 1:R], in_=hcv[:, :, 0:R - 1]), reads=[hck], writes=[hmk])
            S.op("dve", lambda e, hpv=hpv, hcv=hcv: e.tensor_copy(out=hpv[:, :, 0:R - 1], in_=hcv[:, :, 1:R]), reads=[hck], writes=[hpk])
            for tp, (src, sk) in enumerate(((hm, hmk), (hc, hck), (hp, hpk))):
                S.op("pool" if tp == 1 else "dve", lambda e, tp=tp, src=src: e.tensor_copy(
                    out=hR[tp].rearrange("p k (t c) -> p (k t) c", c=128),
                    in_=src.rearrange("p k (t c) -> p (k t) c", c=128)[:, :, ::-1]), reads=[sk], writes=[u + "hR%d" % tp])
            for j in range(2):
                pst, pk = self.ps[j], self.psk[j]

                def mmf(e, j=j, pst=pst, hc=hc):
                    ins = None
                    for kc in range(KC):
                        ins = e.matmul(pst[:, 0:GT], lhsT=win[:, kc, j * 128:(j + 1) * 128], rhs=hc[:, kc, :], start=(kc == 0), stop=(kc == KC - 1))
                    return ins
                S.op("pe", mmf, reads=[u + "win", hck], writes=[pk])
                S.op("act", lambda e, j=j, pst=pst: e.copy(out=pfT[:, j, :], in_=pst[:, 0:GT]), reads=[pk], writes=[u + "pfT"], partial=True)
            for ti in range(TG):
                t = g * TG + ti
                pst, pk = self.ps[2 + (t % 2)], self.psk[2 + (t % 2)]

                def mmf(e, ti=ti, pst=pst):
                    e.matmul(pst[:, 0:256], lhsT=pfT[:, 0, ti * 128:(ti + 1) * 128], rhs=self.RHSf[:, 0, :], start=True, stop=True)
                    return e.matmul(pst[:, 256:512], lhsT=pfT[:, 1, ti * 128:(ti + 1) * 128], rhs=self.RHSf[:, 1, :], start=True, stop=True)
                S.op("pe", mmf, reads=[u + "pfT", "RHSf"], writes=[pk])
                ab = abt[t % 4]
                abk = u + "ab%d" % (t % 4)
                S.op("dve", lambda e, ab=ab, pst=pst: e.tensor_copy(out=ab, in_=pst), reads=[pk], writes=[abk])
                S.dma("sp", ABd.ap()[t * 128:(t + 1) * 128, :], ab, reads=[abk], writes=["AB" + tag])
            for ti in range(TG):
                t = g * TG + ti
                for part in (0, 2):
                    cnt += 1
                    pst, pk = self.ps[4 + (cnt % 2)], self.psk[4 + (cnt % 2)]

                    def mmf(e, ti=ti, part=part, pst=pst, hts=(hm, hc, hp)):
                        ins = None
                        n = 0
                        for tap in range(3):
                            for kc in range(KC):
                                lt = (hR[tap] if part == 1 else hts[tap])[:, kc, ti * 128:(ti + 1) * 128]
                                ins = e.matmul(pst[:, 0:384], lhsT=lt, rhs=wtap[:, kc, tap, part * 384:(part + 1) * 384],
                                               start=(n == 0), stop=(n == 23))
                                n += 1
                        return ins
                    S.op("pe", mmf, reads=[hck, hmk, hpk, u + "wtap"], writes=[pk])
                    hq = hqt[cnt % 4]
                    hqk = u + "hq%d" % (cnt % 4)
                    S.op("dve", lambda e, hq=hq, pst=pst, part=part: e.tensor_tensor(out=hq, in0=pst[:, 0:384], in1=cbb[:, part * 384:(part + 1) * 384], op=ALU.add),
                         reads=[pk, u + "cbb"], writes=[hqk])
                    S.dma("sp", Hq.ap()[part, t * 128:(t + 1) * 128, :], hq, reads=[hqk], writes=["Hq" + tag])
            b0 = ((g * GT) % L) // 8
            for m in range(3):
                cnt += 1
                pst, pk = self.ps[cnt % 2], self.psk[cnt % 2]

                def mmf(e, m=m, pst=pst, hc=hc):
                    ins = None
                    for kc in range(KC):
                        ins = e.matmul(pst[:, 0:GT], lhsT=win[:, kc, 256 + m * 128:256 + (m + 1) * 128], rhs=hc[:, kc, :], start=(kc == 0), stop=(kc == KC - 1))
                    return ins
                S.op("pe", mmf, reads=[u + "win", hck], writes=[pk])
                pd = psd[cnt % 4]
                pdk = u + "psd%d" % (cnt % 4)
                S.op("act", lambda e, pd=pd, pst=pst: e.copy(out=pd, in_=pst[:, 0:GT].rearrange("p (b i) -> p i b", i=8)), reads=[pk], writes=[pdk])
                S.dma("sp", Xs.ap()[:, m * 128:(m + 1) * 128, s, b0:b0 + GT // 8].rearrange("i p b -> p i b"), pd, reads=[pdk], writes=["Xs" + tag])
            for ti in range(TG):
                t = g * TG + ti
                for part in (1,):
                    cnt += 1
                    pst, pk = self.ps[4 + (cnt % 2)], self.psk[4 + (cnt % 2)]

                    def mmf(e, ti=ti, part=part, pst=pst, hts=(hm, hc, hp)):
                        ins = None
                        n = 0
                        for tap in range(3):
                            for kc in range(KC):
                                lt = (hR[tap] if part == 1 else hts[tap])[:, kc, ti * 128:(ti + 1) * 128]
                                ins = e.matmul(pst[:, 0:384], lhsT=lt, rhs=wtap[:, kc, tap, part * 384:(part + 1) * 384],
                                               start=(n == 0), stop=(n == 23))
                                n += 1
                        return ins
                    S.op("pe", mmf, reads=[u + "wtap", u + "hR0", u + "hR1", u + "hR2"], writes=[pk])
                    hq = hqt[cnt % 4]
                    hqk = u + "hq%d" % (cnt % 4)
                    S.op("dve", lambda e, hq=hq, pst=pst, part=part: e.tensor_tensor(out=hq, in0=pst[:, 0:384], in1=cbb[:, part * 384:(part + 1) * 384], op=ALU.add),
                         reads=[pk, u + "cbb"], writes=[hqk])
                    S.dma("sp", Hq.ap()[part, t * 128:(t + 1) * 128, :], hq, reads=[hqk], writes=["Hq" + tag])
        self.S.barrier()
        A.release(m0)

    def fnet(self, l, L, tag):
        S, A, I = self.S, self.arena, self.I
        u = self.key("fn")
        NT = NS * L
        NTC = L // 128
        FT = min(512, L)
        ABd = self.scr["AB" + tag]
        Ym = self.scr.get("Ym" + tag) or self.dscr("Ym" + tag, [D, NT], BF16)
        m0 = A.mark()
        ABs = A.alloc([NS * NTC, 512], BF16)
        Cf = A.alloc([NTC, FT], BF16)
        Sf = A.alloc([NTC, FT], BF16)
        yf = [A.alloc([FT], BF16) for _ in range(2)]
        S.dma("sp", ABs, ABd.ap().rearrange("(c p) n -> p c n", p=128), reads=["AB" + tag], writes=[u + "ABs"])
        cnt = 0
        for ft in range(L // FT):
            S.dma("sp", Cf, I["dftc%d" % L].ap()[:, ft * FT:(ft + 1) * FT].rearrange("(c p) n -> p c n", p=128), writes=[u + "Cf"], partial=False)
            S.dma("sp", Sf, I["dfts%d" % L].ap()[:, ft * FT:(ft + 1) * FT].rearrange("(c p) n -> p c n", p=128), writes=[u + "Sf"], partial=False)
            for s in range(NS):
                for j in range(2):
                    cnt += 1
                    pst, pk = self.ps[cnt % 2], self.psk[cnt % 2]

                    def mmf(e, s=s, j=j, pst=pst):
                        ins = None
                        for tc in range(NTC):
                            e.matmul(pst[:, 0:FT], lhsT=ABs[:, s * NTC + tc, j * 256:j * 256 + 128], rhs=Cf[:, tc, :], start=(tc == 0), stop=False)
                            ins = e.matmul(pst[:, 0:FT], lhsT=ABs[:, s * NTC + tc, j * 256 + 128:j * 256 + 256], rhs=Sf[:, tc, :], start=False, stop=(tc == NTC - 1))
                        return ins
                    S.op("pe", mmf, reads=[u + "ABs", u + "Cf", u + "Sf"], writes=[pk])
                    y = yf[cnt % 2]
                    yk = u + "yf%d" % (cnt % 2)
                    S.op("act", lambda e, y=y, pst=pst: e.copy(out=y, in_=pst[:, 0:FT]), reads=[pk], writes=[yk])
                    S.dma("sp", Ym.ap()[j * 128:(j + 1) * 128, s * L + ft * FT:s * L + (ft + 1) * FT], y, reads=[yk], writes=["Ym" + tag])
        self.S.barrier()
        A.release(m0)

    def hyena_filters(self, l, L, tag):
        S, A, I = self.S, self.arena, self.I
        u = self.key("hf")
        Gd = self.scr.get("Gd" + tag) or self.dscr("Gd" + tag, [2, 384, 2 * L], BF16)
        CT = min(512, L)
        m0 = A.mark()
        zT = A.alloc([L], F32)
        H1 = A.alloc([L], F32)
        H2n = A.alloc([L], F32)
        H2r = A.alloc([L], F32)
        fw1 = A.alloc([64], F32)
        fw2 = A.alloc([64], F32)
        fw3 = A.alloc([1536], F32)
        vec = A.alloc([4], F32)
        hb = A.alloc([6], F32)
        dec = A.alloc([3, L], F32)
        G = A.alloc([2 * L], F32)
        Gb = A.alloc([2 * L], BF16)
        tf = A.alloc([CT], F32)
        tf2 = A.alloc([CT], F32)
        ti = A.alloc([CT], I32)
        nrm = A.alloc([4], F32)
        S.dma("sp", zT[0:33, :], I["zpos%d" % L].ap(), writes=[u + "zT"])
        S.dma("sp", fw1[0:33, :], I["hy_fw1"].ap()[l], writes=[u + "fw1"])
        S.dma("sp", fw2[0:64, :], I["hy_fw2"].ap()[l], writes=[u + "fw2"])
        S.dma("sp", fw3[0:64, :], I["hy_fw3"].ap()[l], writes=[u + "fw3"])
        for i, nm in enumerate(("hy_fb1", "hy_freq1", "hy_fb2", "hy_freq2")):
            S.dma("sp", vec[0:64, i:i + 1], I[nm].ap()[l].rearrange("(p o) -> p o", o=1), writes=[u + "vec"])
        S.dma("sp", hb, I["hy_bias"].ap()[l].rearrange("o (c p) -> p (o c)", p=128), writes=[u + "hb"], allow_slow_non_contiguous=True)
        S.dma("sp", dec, I["dec%d" % L].ap().rearrange("(c p) n -> p c n", p=128), writes=[u + "dec"])
        for (src, sk, wgt, wk, kdim, dst, dk, vi) in ((zT, u + "zT", fw1, u + "fw1", 33, H1, u + "H1", 0), (H1, u + "H1", fw2, u + "fw2", 64, H2n, u + "H2n", 2)):
            for ct in range(L // CT):
                pst, pk = self.ps[ct % 2], self.psk[ct % 2]
                S.op("pe", lambda e, pst=pst, src=src, wgt=wgt, kdim=kdim, ct=ct: e.matmul(
                    pst[0:64, 0:CT], lhsT=wgt[0:kdim, 0:64], rhs=src[0:kdim, ct * CT:(ct + 1) * CT], start=True, stop=True),
                    reads=[sk, wk], writes=[pk])
                S.op("dve", lambda e, pst=pst, vi=vi: e.tensor_scalar(out=tf[0:64, :], in0=pst[0:64, 0:CT], scalar1=vec[0:64, vi:vi + 1], scalar2=vec[0:64, vi + 1:vi + 2],
                                                            op0=ALU.add, op1=ALU.mult), reads=[pk, u + "vec"], writes=[u + "tf"])
                self.range_reduce_sin("dve", dst[0:64, ct * CT:(ct + 1) * CT], tf[0:64, :], ti[0:64, :], tf2[0:64, :], [u + "tf"], dk, u + "ti", u + "tf2")
        S.op("dve", lambda e: e.tensor_copy(out=H2r[0:64, :], in_=H2n[0:64, ::-1]), reads=[u + "H2n"], writes=[u + "H2r"])
        cnt = 0
        for o in range(2):
            for cc in range(3):
                if o == 0:
                    segs = [(0, L, 0, "r", 0), (L, 2 * L - 1, 1, "n", 1)]
                else:
                    segs = [(0, L - 1, 1, "r", 0), (L - 1, 2 * L - 1, 0, "n", 0)]
                gk = u + "G"
                S.op("pool", lambda e: e.memset(G[:, 2 * L - 1:2 * L], 0.0), writes=[gk])
                for (a0, a1, dr, wh, po) in segs:
                    col = dr * 768 + o * 384 + cc * 128
                    srcH = H2r if wh == "r" else H2n
                    p = a0
                    while p < a1:
                        n = min(CT, a1 - p)
                        sp = p - a0 + po
                        cnt += 1
                        pst, pk = self.ps[cnt % 2], self.psk[cnt % 2]
                        S.op("pe", lambda e, pst=pst, srcH=srcH, col=col, sp=sp, n=n: e.matmul(
                            pst[:, 0:n], lhsT=fw3[0:64, col:col + 128], rhs=srcH[0:64, sp:sp + n], start=True, stop=True),
                            reads=[u + "H2n", u + "H2r", u + "fw3"], writes=[pk])
                        if wh == "r":
                            dsl = dec[:, cc, L - sp - n:L - sp][:, ::-1]
                        else:
                            dsl = dec[:, cc, sp:sp + n]
                        S.op("dve", lambda e, pst=pst, p=p, n=n, dsl=dsl: e.tensor_tensor(out=G[:, p:p + n], in0=pst[:, 0:n], in1=dsl, op=ALU.mult),
                             reads=[pk, u + "dec"], writes=[gk], partial=True)
                        p += n
                S.op("dve", lambda e: e.tensor_reduce(out=nrm[:, 0:1], in_=G, axis=AX.X, op=ALU.add, apply_absolute_value=True), reads=[gk], writes=[u + "nrm"])
                S.op("dve", lambda e: e.reciprocal(out=nrm[:, 1:2], in_=nrm[:, 0:1]), reads=[u + "nrm"], writes=[u + "nrm"])
                S.op("dve", lambda e: e.tensor_scalar(out=G, in0=G, scalar1=nrm[:, 1:2], scalar2=None, op0=ALU.mult), reads=[gk, u + "nrm"], writes=[gk])
                S.op("dve", lambda e, o=o, cc=cc: e.tensor_scalar(out=G[:, L - 1:L], in0=G[:, L - 1:L], scalar1=hb[:, o * 3 + cc:o * 3 + cc + 1], scalar2=None, op0=ALU.add),
                     reads=[gk, u + "hb"], writes=[gk])
                S.op("act", lambda e: e.copy(out=Gb, in_=G), reads=[gk], writes=[u + "Gb"])
                S.dma("sp", Gd.ap()[o, cc * 128:(cc + 1) * 128, :], Gb, reads=[u + "Gb"], writes=["Gd" + tag])
        self.S.barrier()
        A.release(m0)

    def hyena(self, l, L, tag):
        S, A, I = self.S, self.arena, self.I
        u = self.key("hy")
        NB = L // 128
        NC = NS * NB
        W = 128 * (2 * NB - 1)
        NT = NS * L
        Hq = self.scr["Hq" + tag]
        Gd = self.scr["Gd" + tag]
        Ym = self.scr.get("Ym" + tag) or self.dscr("Ym" + tag, [D, NT], BF16)
        HC = 192
        CG = max(1, min(8, 512 // NC))
        m0 = A.mark()
        Z2 = A.alloc([NC, 384], BF16)
        Ub = A.alloc([NC, HC], BF16)
        Xb = A.alloc([NC, HC], BF16)
        Z1 = A.alloc([NC, HC], BF16)
        RG = 1
        for cand in (24, 12, 8, 6, 4, 3, 2):
            if cand * W <= 9216 and HC % cand == 0:
                RG = cand
                break
        Rb = [A.alloc([RG, W], BF16) for _ in range(2)]
        yT = [A.alloc([3, 128], BF16) for _ in range(2)]
        rc = 0
        bc = 0
        dl = [0] + [d for d in range(-(NB - 1), NB) if d != 0]
        for half in range(2):
            h0 = half * HC
            for o in range(2):
                if o == 0:
                    S.dma("sp", Ub, Hq.ap()[0, :, h0:h0 + HC].rearrange("(c p) n -> p c n", p=128), reads=["Hq" + tag], writes=[u + "Ub"], partial=False)
                    S.dma("sp", Xb, Hq.ap()[1, :, h0:h0 + HC].rearrange("(c p) n -> p c n", p=128), reads=["Hq" + tag], writes=[u + "Xb"], partial=False)
                    src, srck, dst, dstk = Ub, u + "Ub", Z1, u + "Z1"
                else:
                    S.dma("sp", Xb, Hq.ap()[2, :, h0:h0 + HC].rearrange("(c p) n -> p c n", p=128), reads=["Hq" + tag], writes=[u + "Xb"], partial=False)
                    src, srck, dst, dstk = Z1, u + "Z1", Z2, u + "Z2"
                srcv = src.rearrange("p (s b) n -> p s b n", s=NS)
                for c8 in range(HC // CG):
                    bc += 1
                    pst, pk = self.ps[bc % 4], self.psk[bc % 4]
                    for ci in range(CG):
                        cl = c8 * CG + ci
                        c = h0 + cl
                        if cl % RG == 0:
                            rc += 1
                            Rg = Rb[rc % 2]
                            rk = u + "R%d" % (rc % 2)
                            if RG == 1:
                                S.dma("sp", Rg[:, 0, :], bass.AP(Gd, (o * 384 + c) * 2 * L, [[1, 128], [1, W]]), reads=["Gd" + tag], writes=[rk], partial=False)
                            else:
                                S.dma("sp", Rg, bass.AP(Gd, (o * 384 + c) * 2 * L, [[1, 128], [2 * L, RG], [1, W]]), reads=["Gd" + tag], writes=[rk], partial=False)
                        Rt = Rg[:, cl % RG, :]
                        pv = pst[:, ci * NC:(ci + 1) * NC].rearrange("p (s b) -> p s b", s=NS)

                        def mmf(e, Rt=Rt, pv=pv, srcv=srcv, cl=cl, o=o):
                            ins = None
                            for k, d in enumerate(dl):
                                i0 = max(0, d)
                                i1 = min(NB - 1, NB - 1 + d)
                                nb = i1 - i0 + 1
                                j0 = i0 - d
                                xd = 128 * (NB - 1 - d) if o == 0 else 128 * (d + NB - 1)
                                ins = e.matmul(pv[:, :, i0:i0 + nb], lhsT=Rt[:, xd:xd + 128], rhs=srcv[:, :, j0:j0 + nb, cl],
                                               start=(k == 0), stop=(k == len(dl) - 1))
                            return ins
                        S.op("pe", mmf, reads=[rk, srck], writes=[pk], partial=True)
                    cl0 = c8 * CG
                    oc0 = cl0 if o == 0 else h0 + cl0
                    S.op("dve", lambda e, pst=pst, dst=dst, cl0=cl0, oc0=oc0: e.tensor_tensor(
                        out=dst[:, :, oc0:oc0 + CG], in0=pst[:, 0:CG * NC].rearrange("p (c n) -> p n c", c=CG),
                        in1=Xb[:, :, cl0:cl0 + CG], op=ALU.mult), reads=[pk, u + "Xb"], writes=[dstk], partial=True)
        for col in range(NC):
            pst, pk = self.ps[4 + col % 2], self.psk[4 + col % 2]
            pb = pst.bitcast(BF16)

            def trf(e, col=col, pb=pb):
                ins = None
                for cc in range(3):
                    ins = e.transpose(pb[:, cc * 128:(cc + 1) * 128], Z2[:, col, cc * 128:(cc + 1) * 128], self.identb)
                return ins
            S.op("pe", trf, reads=[u + "Z2", "identb"], writes=[pk])
            y = yT[col % 2]
            yk = u + "yT%d" % (col % 2)
            S.op("act", lambda e, y=y, pb=pb: e.copy(out=y, in_=pb[:, 0:384].rearrange("p (c t) -> p c t", c=3)), reads=[pk], writes=[yk])
            S.dma("sp", Ym.ap()[256:640, col * 128:(col + 1) * 128].rearrange("(c p) t -> p c t", p=128), y, reads=[yk], writes=["Ym" + tag])
        self.S.barrier()
        A.release(m0)

    def s5_prep(self, l):
        S, A, I = self.S, self.arena, self.I
        u = self.key("sp")
        P = {}
        P["pr"] = A.alloc([24, 26], F32)
        P["pi"] = A.alloc([24, 26], F32)
        P["npi"] = A.alloc([24, 26], F32)
        P["WBT"] = A.alloc([48, 128], BF16)
        P["Mpad"] = A.alloc([24, 8, 32], BF16)
        P["Gpad"] = A.alloc([48, 8, 32], BF16)
        P["gluw"] = A.alloc([4, 384], BF16)
        P["glub"] = A.alloc([4], F32)
        P["u"] = u
        m0 = A.mark()
        lr = A.alloc([24], F32); li = A.alloc([24], F32); dt = A.alloc([24], F32)
        Bre = A.alloc([24, 16], F32); Bim = A.alloc([24, 16], F32)
        Cre = A.alloc([24, 16], F32); Cim = A.alloc([24, 16], F32)
        pwc = A.alloc([26], F32)
        EX = A.alloc([24, 26], F32); ANG = A.alloc([24, 26], F32)
        tI = A.alloc([24, 26], I32); tF = A.alloc([24, 26], F32)
        t1 = A.alloc([24], F32); t2 = A.alloc([24], F32); cr = A.alloc([24], F32); ci = A.alloc([24], F32)
        bbr = A.alloc([24, 16], F32); bbi = A.alloc([24, 16], F32)
        T1 = A.alloc([12, 8, 16], F32); T2 = A.alloc([12, 8, 16], F32)
        Wn = A.alloc([48, 128], F32)
        Pp = A.alloc([48, 128], BF16)
        Qq = A.alloc([48, 128], BF16)
        mle = A.alloc([128], F32); mge = A.alloc([128], F32)
        dv = A.alloc([24], F32)
        Mf = A.alloc([128], F32)
        S.dma("sp", lr, I["s5_a_re"].ap()[l].rearrange("d (q m) s -> (m s) (d q)", m=2), writes=[u + "lr"], allow_slow_non_contiguous=True)
        S.dma("sp", li, I["s5_a_im"].ap()[l].rearrange("d (q m) s -> (m s) (d q)", m=2), writes=[u + "li"], allow_slow_non_contiguous=True)
        for m in range(2):
            S.dma("sp", dt[m * 64:(m + 1) * 64, :], I["s5_log_dt"].ap()[l].rearrange("d (q m) -> m (d q)", m=2)[m].partition_broadcast(64),
                  writes=[u + "dt"], allow_slow_non_contiguous=True)
        Csrc = A.alloc([6, 128], F32)
        CT_ = A.alloc([768], F32)
        for (dst, nm, kk) in ((Cre, "s5_c_re", u + "Cre"), (Cim, "s5_c_im", u + "Cim")):
            for hh in range(2):
                S.dma("sp", Csrc[:, :, hh * 64:(hh + 1) * 64], I[nm].ap()[l].rearrange("d g o s -> (d g o) s").rearrange("(k p) s -> p k s", p=128),
                      writes=[u + "Csrc"])
            for k in range(6):
                pst, pk = self.ps[k % 2], self.psk[k % 2]
                S.op("pe", lambda e, k=k, pst=pst: e.transpose(pst[:, 0:128], Csrc[:, k, :], self.ident), reads=[u + "Csrc", "ident"], writes=[pk])
                S.op("act", lambda e, k=k, pst=pst: e.copy(out=CT_[:, k * 128:(k + 1) * 128], in_=pst[:, 0:128]), reads=[pk], writes=[u + "CT"], partial=True)
            CTv = CT_.rearrange("p (d q m o) -> p d q m o", d=2, q=12, m=2)
            for m in range(2):
                S.op("dve", lambda e, m=m, dst=dst, CTv=CTv: e.tensor_copy(out=dst[m * 64:(m + 1) * 64].rearrange("p (d q) o -> p d q o", d=2), in_=CTv[m * 64:(m + 1) * 64, :, :, m, :]),
                     reads=[u + "CT"], writes=[kk], partial=True)
        S.dma("sp", Bre, I["s5_b_re"].ap()[l].rearrange("d (q m) s c -> (m s) (d q) c", m=2), writes=[u + "Bre"])
        S.dma("sp", Bim, I["s5_b_im"].ap()[l].rearrange("d (q m) s c -> (m s) (d q) c", m=2), writes=[u + "Bim"])
        S.dma("sp", pwc, I["s5pw"].ap(), writes=[u + "pwc"])
        S.dma("sp", mle, I["mask_le"].ap(), writes=[u + "mle"])
        S.dma("sp", mge, I["mask_ge"].ap(), writes=[u + "mge"])
        for i in range(8):
            S.dma("sp", dv[i * 16:(i + 1) * 16, :], I["s5_d"].ap()[l].rearrange("(g c) -> c g", c=16), writes=[u + "dv"], allow_slow_non_contiguous=True)
        S.dma("pool", P["gluw"][0:96], I["s5_glu_w"].ap()[l].rearrange("(c p) n -> p c n", p=96), writes=[u + "gluw"])
        S.dma("sp", P["glub"][0:96], I["s5_glu_b"].ap()[l].rearrange("(c p) -> p c", p=96), writes=[u + "glub"], allow_slow_non_contiguous=True)
        V = lambda e: e
        dve = lambda fn, r, w, **k: S.op("dve", fn, reads=r, writes=w, **k)
        dve(lambda e: e.tensor_copy(out=dt, in_=dt), [u + "dt"], [u + "dt"])
        S.op("act", lambda e: e.activation(out=dt, in_=dt, func=AF.Exp), reads=[u + "dt"], writes=[u + "dt"])
        dve(lambda e: e.tensor_tensor(out=t1, in0=lr, in1=dt, op=ALU.mult), [u + "lr", u + "dt"], [u + "t1"])
        dve(lambda e: e.tensor_tensor(out=t2, in0=li, in1=dt, op=ALU.mult), [u + "li", u + "dt"], [u + "t2"])
        bc3 = lambda a: a.unsqueeze(2).to_broadcast([128, 24, 26])
        pw3 = pwc.unsqueeze(1).to_broadcast([128, 24, 26])
        dve(lambda e: e.tensor_tensor(out=EX, in0=bc3(t1), in1=pw3, op=ALU.mult), [u + "t1", u + "pwc"], [u + "EX"])
        dve(lambda e: e.tensor_tensor(out=ANG, in0=bc3(t2), in1=pw3, op=ALU.mult), [u + "t2", u + "pwc"], [u + "ANG"])
        S.op("act", lambda e: e.activation(out=EX, in_=EX, func=AF.Exp), reads=[u + "EX"], writes=[u + "EX"])
        self.range_reduce_sin("dve", P["pi"], ANG, tI, tF, [u + "ANG"], u + "pi", u + "tI", u + "tF")
        self.range_reduce_sin("dve", P["pr"], ANG, tI, tF, [u + "ANG"], u + "pr", u + "tI", u + "tF", shift=math.pi / 2)
        dve(lambda e: e.tensor_tensor(out=P["pi"], in0=P["pi"], in1=EX, op=ALU.mult), [u + "pi", u + "EX"], [u + "pi"])
        dve(lambda e: e.tensor_tensor(out=P["pr"], in0=P["pr"], in1=EX, op=ALU.mult), [u + "pr", u + "EX"], [u + "pr"])
        dve(lambda e: e.tensor_scalar(out=P["npi"], in0=P["pi"], scalar1=-1.0, scalar2=None, op0=ALU.mult), [u + "pi"], [u + "npi"])
        pk_ = [u + "pr", u + "pi", u + "npi"]
        i1 = PWI(1)
        ar = P["pr"][:, :, i1]; ai = P["pi"][:, :, i1]
        dve(lambda e: e.tensor_tensor(out=t1, in0=lr, in1=lr, op=ALU.mult), [u + "lr", u + "EX"], [u + "t1"])
        dve(lambda e: e.tensor_tensor(out=t2, in0=li, in1=li, op=ALU.mult), [u + "li", u + "ANG"], [u + "t2"])
        dve(lambda e: e.tensor_tensor(out=t1, in0=t1, in1=t2, op=ALU.add), [u + "t1", u + "t2"], [u + "t1"])
        dve(lambda e: e.reciprocal(out=t1, in_=t1), [u + "t1"], [u + "t1"])
        dve(lambda e: e.tensor_scalar(out=t2, in0=ar, scalar1=-1.0, scalar2=None, op0=ALU.add), pk_, [u + "t2"])
        dve(lambda e: e.tensor_tensor(out=cr, in0=t2, in1=lr, op=ALU.mult), [u + "t2", u + "lr"], [u + "cr"])
        dve(lambda e: e.tensor_tensor(out=ci, in0=ai, in1=li, op=ALU.mult), pk_ + [u + "li"], [u + "ci"])
        dve(lambda e: e.tensor_tensor(out=cr, in0=cr, in1=ci, op=ALU.add), [u + "cr", u + "ci"], [u + "cr"])
        dve(lambda e: e.tensor_tensor(out=cr, in0=cr, in1=t1, op=ALU.mult), [u + "cr", u + "t1"], [u + "cr"])
        dve(lambda e: e.tensor_tensor(out=ci, in0=ai, in1=lr, op=ALU.mult), pk_ + [u + "lr", u + "cr"], [u + "ci"])
        dve(lambda e: e.tensor_tensor(out=t2, in0=t2, in1=li, op=ALU.mult), [u + "t2", u + "li"], [u + "t2"])
        dve(lambda e: e.tensor_tensor(out=ci, in0=ci, in1=t2, op=ALU.subtract), [u + "ci", u + "t2"], [u + "ci"])
        dve(lambda e: e.tensor_tensor(out=ci, in0=ci, in1=t1, op=ALU.mult), [u + "ci", u + "t1"], [u + "ci"])
        b16 = lambda a: a.unsqueeze(2).to_broadcast([128, 24, 16])
        dve(lambda e: e.tensor_scalar(out=Cim, in0=Cim, scalar1=-1.0, scalar2=None, op0=ALU.mult), [u + "Cim"], [u + "Cim"])

        def cmul(outr, outi, xr_, xi_, yr_, yi_, rk, wkr, wki, ta, tb, shape_sel=None, neg_im=False):
            dve(lambda e: e.tensor_tensor(out=ta, in0=xr_, in1=yr_, op=ALU.mult), rk, [u + "ta"])
            dve(lambda e: e.tensor_tensor(out=tb, in0=xi_, in1=yi_, op=ALU.mult), rk, [u + "tb"])
            dve(lambda e: e.tensor_tensor(out=outr, in0=ta, in1=tb, op=(ALU.add if neg_im else ALU.subtract)), [u + "ta", u + "tb"], [wkr], partial=True)
            dve(lambda e: e.tensor_tensor(out=ta, in0=xr_, in1=yi_, op=ALU.mult), rk + [wkr], [u + "ta"])
            dve(lambda e: e.tensor_tensor(out=tb, in0=xi_, in1=yr_, op=ALU.mult), rk + [wkr], [u + "tb"])
            dve(lambda e: e.tensor_tensor(out=outi, in0=ta, in1=tb, op=(ALU.subtract if neg_im else ALU.add)), [u + "ta", u + "tb"], [wki], partial=True)
        ta24 = T1.rearrange("p a b c -> p (a b c)")[:, 0:384].rearrange("p (a c) -> p a c", a=24)
        tb24 = T2.rearrange("p a b c -> p (a b c)")[:, 0:384].rearrange("p (a c) -> p a c", a=24)
        cmul(bbr, bbi, b16(cr), b16(ci), Bre, Bim, [u + "cr", u + "ci", u + "Bre", u + "Bim"], u + "bbr", u + "bbi", ta24, tb24)
        def pslice(arr, d, n0, step):
            i0 = PWI(n0)
            if step > 0:
                sl = arr[:, d * 12:(d + 1) * 12, i0:i0 + 8]
            else:
                sl = arr[:, d * 12:(d + 1) * 12, i0 - 7:i0 + 1][:, :, ::-1]
            return sl.unsqueeze(3).to_broadcast([128, 12, 8, 16])
        v16 = lambda a, d: a[:, d * 12:(d + 1) * 12, :].unsqueeze(2).to_broadcast([128, 12, 8, 16])
        Wnv = Wn.rearrange("p (d q r) (i c) -> p d q r i c", d=2, q=12, r=2, i=8)
        Ppv = Pp.rearrange("p (d q r) (i c) -> p d q r i c", d=2, q=12, r=2, i=8)
        Qqv = Qq.rearrange("p (d q r) (i c) -> p d q r i c", d=2, q=12, r=2, i=8)
        bk = [u + "bbr", u + "bbi"] + pk_
        ck = [u + "Cre", u + "Cim"] + pk_
        for d in range(2):
            n0, stp = ((7, -1), (0, 1))[d]
            cmul(Wnv[:, d, :, 0], Wnv[:, d, :, 1], pslice(P["pr"], d, n0, stp), pslice(P["pi"], d, n0, stp), v16(bbr, d), v16(bbi, d),
                 bk, u + "Wn", u + "Wn", T1, T2)
            n0, stp = ((0, -1), (0, 1))[d]
            cmul(Ppv[:, d, :, 0], Ppv[:, d, :, 1], pslice(P["pr"], d, n0, stp), pslice(P["pi"], d, n0, stp), v16(bbr, d), v16(bbi, d),
                 bk, u + "Pp", u + "Pp", T1, T2)
            n0, stp = ((0, 1), (0, -1))[d]
            cmul(Qqv[:, d, :, 0], Qqv[:, d, :, 1], pslice(P["pr"], d, n0, stp), pslice(P["pi"], d, n0, stp), v16(Cre, d), v16(Cim, d),
                 ck, u + "Qq", u + "Qq", T1, T2, neg_im=True)
        S.op("pool", lambda e: e.memset(P["Gpad"], 0.0), writes=[u + "Gpad"])
        Gv = P["Gpad"].rearrange("p (d q r) j n -> p d q r j n", d=2, q=12, r=2)
        for d in range(2):
            n0, stp = ((1, 1), (8, -1))[d]
            for m in range(2):
                hs = slice(m * 64, (m + 1) * 64)
                h = lambda a: a[hs]
                cmul(Gv[hs, d, :, 0, :, m * 16:(m + 1) * 16], Gv[hs, d, :, 1, :, m * 16:(m + 1) * 16],
                     h(pslice(P["pr"], d, n0, stp)), h(pslice(P["pi"], d, n0, stp)), h(v16(Cre, d)), h(v16(Cim, d)),
                     ck + [u + "Gpad"], u + "Gpad", u + "Gpad", T1[hs], T2[hs], neg_im=True)
        for k in range(48):
            pst, pk = self.ps[k % 2], self.psk[k % 2]
            S.op("pe", lambda e, k=k, pst=pst: e.transpose(pst[:, 0:128], Wn[:, k, :], self.ident), reads=[u + "Wn", "ident"], writes=[pk])
            S.op("act", lambda e, k=k, pst=pst: e.copy(out=P["WBT"][:, k, :], in_=pst[:, 0:128]), reads=[pk], writes=[u + "WBT"], partial=True)
        S.op("pool", lambda e: e.memset(P["Mpad"], 0.0), writes=[u + "Mpad"])
        for g in range(24):
            q, m = g // 2, g % 2
            hs = slice(m * 64, (m + 1) * 64)
            psf, pkf = self.ps[2 + (g % 2) * 2], self.psk[2 + (g % 2) * 2]
            psb, pkb = self.ps[3 + (g % 2) * 2], self.psk[3 + (g % 2) * 2]
            for d, pst, pk in ((0, psf, pkf), (1, psb, pkb)):
                kre = (d * 12 + q) * 2
                def mmf(e, pst=pst, kre=kre, hs=hs):
                    e.matmul(pst[:, 0:128], lhsT=Pp[hs, kre, :], rhs=Qq[hs, kre, :], start=True, stop=False)
                    return e.matmul(pst[:, 0:128], lhsT=Pp[hs, kre + 1, :], rhs=Qq[hs, kre + 1, :], start=False, stop=True)
                S.op("pe", mmf, reads=[u + "Pp", u + "Qq"], writes=[pk])
            dve(lambda e, psf=psf: e.tensor_tensor(out=Mf, in0=psf[:, 0:128], in1=mle, op=ALU.mult), [pkf, u + "mle"], [u + "Mf"])
            dve(lambda e, psb=psb: e.tensor_tensor(out=T1.rearrange("p a b c -> p (a b c)")[:, 0:128], in0=psb[:, 0:128], in1=mge, op=ALU.mult), [pkb, u + "mge"], [u + "ta"])
            dve(lambda e: e.tensor_tensor(out=Mf, in0=Mf, in1=T1.rearrange("p a b c -> p (a b c)")[:, 0:128], op=ALU.add), [u + "Mf", u + "ta"], [u + "Mf"])
            dve(lambda e, g=g: e.scalar_tensor_tensor(out=Mf, in0=self.ident, scalar=dv[:, g:g + 1], in1=Mf, op0=ALU.mult, op1=ALU.add), [u + "Mf", u + "dv", "ident"], [u + "Mf"])
            dve(lambda e, g=g, m=m: e.tensor_copy(out=P["Mpad"][:, g, :, m * 16:(m + 1) * 16], in_=Mf.rearrange("p (j o) -> p j o", j=8)), [u + "Mf"], [u + "Mpad"], partial=True)
        self.S.barrier()
        A.release(m0)
        return P

    def s5_main(self, l, L, tag, P, use_s0, readout):
        S, A, I = self.S, self.arena, self.I
        u = self.key("s5")
        pu = P["u"]
        NBk = L // 8
        E = NBk + 1
        NT = NS * L
        Xs = self.scr["Xs" + tag]
        Ym = self.scr.get("Ym" + tag) or self.dscr("Ym" + tag, [D, NT], BF16)
        nlev = 0
        while (1 << nlev) < E:
            nlev += 1
        if nlev % 2:
            nlev += 1
        m0 = A.mark()
        eA = A.alloc([4, NS, E], F32)
        eB = A.alloc([4, NS, E], F32)
        ebf = A.alloc([4, NS, E], BF16)
        Xg = [A.alloc([NS * NBk], BF16) for _ in range(2)]
        if readout:
            YS = A.alloc([NS * L], F32)
            PC = min(2048, NS * L)
            tq = A.alloc([PC], F32)
            ygp = [A.alloc([PC], BF16) for _ in range(2)]
            Ygd = self.scr.get("Yg" + tag) or self.dscr("Yg" + tag, [384, NT], BF16)
        pk_ = [pu + "pr", pu + "pi", pu + "npi"]
        bc = 0
        for q in range(12):
            for m in range(2):
                g = 2 * q + m
                for i in range(8):
                    S.dma("sp", Xg[m][i * 16:(i + 1) * 16, :], Xs.ap()[i, g * 16:(g + 1) * 16].rearrange("c s b -> c (s b)"),
                          reads=["Xs" + tag], writes=[u + "Xg%d" % m])
            for d in range(2):
                col = 0 if d == 0 else NBk
                for ri in range(2):
                    k = d * 2 + ri
                    if use_s0:
                        S.op("act", lambda e, k=k, col=col, d=d, ri=ri, q=q: e.copy(out=eA[:, k, :, col], in_=self.s0[:, (d * 12 + q) * 2 + ri, :]),
                             reads=["s0"], writes=[u + "eA%d" % d], partial=True)
                    else:
                        S.op("pool", lambda e, k=k, col=col: e.memset(eA[:, k, :, col], 0.0), writes=[u + "eA%d" % d], partial=True)
            for d in range(2):
                off = 1 if d == 0 else 0
                for ri in range(2):
                    for sq in range(NS):
                        bc += 1
                        pst, pk = self.ps[bc % 2], self.psk[bc % 2]
                        kk = (d * 12 + q) * 2 + ri

                        def mmf(e, pst=pst, kk=kk, sq=sq):
                            e.matmul(pst[0:64, 0:NBk], lhsT=P["WBT"][:, kk, 0:64], rhs=Xg[0][:, sq * NBk:(sq + 1) * NBk], start=True, stop=True)
                            return e.matmul(pst[64:128, 0:NBk], lhsT=P["WBT"][:, kk, 64:128], rhs=Xg[1][:, sq * NBk:(sq + 1) * NBk], start=True, stop=True)
                        S.op("pe", mmf, reads=[pu + "WBT", u + "Xg0", u + "Xg1"], writes=[pk])
                        S.op("act", lambda e, pst=pst, d=d, ri=ri, sq=sq, off=off: e.copy(out=eA[:, d * 2 + ri, sq, off:off + NBk], in_=pst[:, 0:NBk]),
                             reads=[pk], writes=[u + "eA%d" % d], partial=True)
            for d, eng in ((0, "dve"), (1, "dve")):
                cur, nxt = eA, eB
                ck_, nk_ = u + "eA%d" % d, u + "eB%d" % d
                for lev in range(nlev):
                    dist = 1 << lev
                    ix = PWI(8 * dist)
                    ar = P["pr"][:, d * 12 + q, ix:ix + 1]
                    ai = P["pi"][:, d * 12 + q, ix:ix + 1]
                    nai = P["npi"][:, d * 12 + q, ix:ix + 1]
                    cr_, ci_ = cur[:, d * 2 + 0], cur[:, d * 2 + 1]
                    nr_, ni_ = nxt[:, d * 2 + 0], nxt[:, d * 2 + 1]
                    if dist < E:
                        n = E - dist
                        if d == 0:
                            srcs, dsts = slice(0, n), slice(dist, E)
                            keep = slice(0, dist)
                        else:
                            srcs, dsts = slice(dist, E), slice(0, n)
                            keep = slice(n, E)
                        S.op(eng, lambda e, cr_=cr_, nr_=nr_, ar=ar, srcs=srcs, dsts=dsts: e.scalar_tensor_tensor(
                            out=nr_[:, :, dsts], in0=cr_[:, :, srcs], scalar=ar, in1=cr_[:, :, dsts], op0=ALU.mult, op1=ALU.add),
                            reads=[ck_] + pk_, writes=[nk_], partial=True)
                        S.op(eng, lambda e, ci_=ci_, nr_=nr_, nai=nai, srcs=srcs, dsts=dsts: e.scalar_tensor_tensor(
                            out=nr_[:, :, dsts], in0=ci_[:, :, srcs], scalar=nai, in1=nr_[:, :, dsts], op0=ALU.mult, op1=ALU.add),
                            reads=[ck_, nk_] + pk_, writes=[nk_], partial=True)
                        S.op(eng, lambda e, ci_=ci_, ni_=ni_, ar=ar, srcs=srcs, dsts=dsts: e.scalar_tensor_tensor(
                            out=ni_[:, :, dsts], in0=ci_[:, :, srcs], scalar=ar, in1=ci_[:, :, dsts], op0=ALU.mult, op1=ALU.add),
                            reads=[ck_] + pk_, writes=[nk_], partial=True)
                        S.op(eng, lambda e, cr_=cr_, ni_=ni_, ai=ai, srcs=srcs, dsts=dsts: e.scalar_tensor_tensor(
                            out=ni_[:, :, dsts], in0=cr_[:, :, srcs], scalar=ai, in1=ni_[:, :, dsts], op0=ALU.mult, op1=ALU.add),
                            reads=[ck_, nk_] + pk_, writes=[nk_], partial=True)
                    else:
                        keep = slice(0, E)
                    S.op("act", lambda e, cur=cur, nxt=nxt, d=d, keep=keep: e.copy(out=nxt[:, d * 2:d * 2 + 2, :, keep], in_=cur[:, d * 2:d * 2 + 2, :, keep]),
                         reads=[ck_], writes=[nk_], partial=True)
                    cur, nxt = nxt, cur
                    ck_, nk_ = nk_, ck_
            if not readout or True:
                for d in range(2):
                    col = NBk if d == 0 else 0
                    for ri in range(2):
                        S.op("act", lambda e, d=d, ri=ri, col=col, q=q: e.copy(out=self.s0n[:, (d * 12 + q) * 2 + ri, :], in_=eA[:, d * 2 + ri, :, col]),
                             reads=[u + "eA%d" % d], writes=["s0n"], partial=True)
            if not readout:
                continue
            S.op("act", lambda e: e.copy(out=ebf, in_=eA), reads=[u + "eA0", u + "eA1"], writes=[u + "ebf"])
            qq = q % 3
            gc = q // 3
            YSv = YS.rearrange("p (s b j) -> p s b j", s=NS, j=8)
            for j in range(8):
                for sq in range(NS):
                    bc += 1
                    pst, pk = self.ps[2 + bc % 4], self.psk[2 + bc % 4]

                    def mmf(e, pst=pst, j=j, sq=sq, q=q, qq=qq):
                        o = pst[qq * 32:(qq + 1) * 32, 0:NBk]
                        e.matmul(o, lhsT=P["Mpad"][:, 2 * q, j, :], rhs=Xg[0][:, sq * NBk:(sq + 1) * NBk], start=True, stop=False)
                        e.matmul(o, lhsT=P["Mpad"][:, 2 * q + 1, j, :], rhs=Xg[1][:, sq * NBk:(sq + 1) * NBk], start=False, stop=False)
                        ins = None
                        for d in range(2):
                            c0 = 0 if d == 0 else 1
                            for ri in range(2):
                                ins = e.matmul(o, lhsT=P["Gpad"][:, (d * 12 + q) * 2 + ri, j, :], rhs=ebf[:, d * 2 + ri, sq, c0:c0 + NBk],
                                               start=False, stop=(d == 1 and ri == 1))
                        return ins
                    S.op("pe", mmf, reads=[pu + "Mpad", pu + "Gpad", u + "Xg0", u + "Xg1", u + "ebf"], writes=[pk])
                    S.op("dve", lambda e, pst=pst, j=j, sq=sq, qq=qq: e.tensor_copy(out=YSv[qq * 32:(qq + 1) * 32, sq, :, j], in_=pst[qq * 32:(qq + 1) * 32, 0:NBk]),
                         reads=[pk], writes=[u + "YS"], partial=True)
            if qq == 2:
                for pc in range(NS * L // PC):
                    cs = slice(pc * PC, (pc + 1) * PC)
                    yp, ypk = ygp[pc % 2], u + "ygp%d" % (pc % 2)
                    S.op("act", lambda e, cs=cs: e.activation(out=tq[0:96], in_=YS[0:96, cs], func=AF.Square), reads=[u + "YS"], writes=[u + "tq"])
                    S.op("dve", lambda e: e.tensor_scalar(out=tq[0:96], in0=tq[0:96], scalar1=0.044715, scalar2=1.0, op0=ALU.mult, op1=ALU.add), reads=[u + "tq"], writes=[u + "tq"])
                    S.op("dve", lambda e, cs=cs: e.tensor_tensor(out=tq[0:96], in0=tq[0:96], in1=YS[0:96, cs], op=ALU.mult), reads=[u + "tq", u + "YS"], writes=[u + "tq"])
                    S.op("act", lambda e: e.activation(out=tq[0:96], in_=tq[0:96], func=AF.Sigmoid, scale=1.5957691216057308), reads=[u + "tq"], writes=[u + "tq"])
                    S.op("dve", lambda e, cs=cs, yp=yp: e.tensor_tensor(out=yp[0:96], in0=tq[0:96], in1=YS[0:96, cs], op=ALU.mult), reads=[u + "tq", u + "YS"], writes=[ypk])
                    S.dma("sp", Ygd.ap()[gc * 96:(gc + 1) * 96, cs], yp[0:96], reads=[ypk], writes=["Yg" + tag])
        if readout:
            TT = min(512, NT)
            yo = [A.alloc([TT], BF16) for _ in range(2)]
            sg = [A.alloc([TT], F32) for _ in range(2)]
            ygt = [A.alloc([4, TT], BF16) for _ in range(2)]
            for tt in range(NT // TT):
                yt, ytk = ygt[tt % 2], u + "ygt%d" % (tt % 2)
                S.dma("sp", yt[0:96], Ygd.ap()[:, tt * TT:(tt + 1) * TT].rearrange("(c p) t -> p c t", p=96), reads=["Yg" + tag], writes=[ytk], partial=False)
                for mo in range(4):
                    bc += 1
                    pst, pk = self.ps[bc % 2], self.psk[bc % 2]

                    def mmf(e, pst=pst, yt=yt, mo=mo):
                        ins = None
                        for cc in range(4):
                            ins = e.matmul(pst[0:96, 0:TT], lhsT=P["gluw"][0:96, cc, mo * 96:(mo + 1) * 96], rhs=yt[0:96, cc, :], start=(cc == 0), stop=(cc == 3))
                        return ins
                    S.op("pe", mmf, reads=[pu + "gluw", ytk], writes=[pk])
                    sgt, sk = sg[bc % 2], u + "sg%d" % (bc % 2)
                    S.op("act", lambda e, pst=pst, sgt=sgt, mo=mo: e.activation(out=sgt[0:96], in_=pst[0:96, 0:TT], func=AF.Sigmoid, bias=P["glub"][0:96, mo:mo + 1], scale=1.0),
                         reads=[pk, pu + "glub"], writes=[sk])
                    yot, yk = yo[bc % 2], u + "yo%d" % (bc % 2)
                    S.op("dve", lambda e, yot=yot, sgt=sgt, mo=mo, yt=yt: e.tensor_tensor(out=yot[0:96], in0=sgt[0:96], in1=yt[0:96, mo, :], op=ALU.mult),
                         reads=[sk, ytk], writes=[yk])
                    S.dma("sp", Ym.ap()[640 + mo * 96:640 + (mo + 1) * 96, tt * TT:(tt + 1) * TT], yot[0:96], reads=[yk], writes=["Ym" + tag])
        self.S.barrier()
        A.release(m0)

    def cast_weights(self, name, src_ap_2d, rows, cols):
        t = self.dscr(name, [rows, cols], BF16)
        step = 512
        for r0 in range(0, rows, step):
            r1 = min(rows, r0 + step)
            self.S.dma("pool", t.ap()[r0:r1, :], src_ap_2d[r0:r1, :], writes=[name])
        return t

    def cast_moe(self, l):
        I = self.I
        experts = []
        for e in range(self.ne_cast):
            experts.append((self.cast_weights("wg%d_%d" % (l, e), I["moe_w_gate"].ap()[0, e], D, DFFE),
                            self.cast_weights("wu%d_%d" % (l, e), I["moe_w_up"].ap()[0, e], D, DFFE),
                            self.cast_weights("wd%d_%d" % (l, e), I["moe_w_down"].ap()[0, e], DFFE, D), e))
        self._moe_experts = experts

    def phaseC(self, l, Xsrc, Xdst, L, tag, bl, moe, final):
        S, A, I = self.S, self.arena, self.I
        u = self.key("pC")
        NT = NS * L
        GT = min(512, L)
        TG = GT // 128
        Ym = self.scr["Ym" + tag]
        md = self.scr["modD%d" % l]
        if moe:
            if self._moe_experts is None:
                self.cast_moe(l)
            experts = self._moe_experts[:self.ne_comp]
            dff, FBW = DFFE, 512
        else:
            if ("wgd%d" % l) not in self.scr:
                self.cast_weights("wgd%d" % l, I["ffn_w_gate"].ap()[l // 2], D, DFF)
                self.cast_weights("wud%d" % l, I["ffn_w_up"].ap()[l // 2], D, DFF)
                self.cast_weights("wdd%d" % l, I["ffn_w_down"].ap()[l // 2], DFF, D)
            experts = [(self.scr["wgd%d" % l], self.scr["wud%d" % l], self.scr["wdd%d" % l], None)]
            dff, FBW = DFF, 256
        NCH = dff // 128
        FB = FBW // 128
        NBLK = dff // FBW
        DB = 4 if moe else 11
        NWD = 4 if moe else 2
        m0 = A.mark()
        self._nt_ss = A.alloc([4], F32)
        self._nt_junk = A.alloc([D], F32)
        wout = A.alloc([KC, D], BF16)
        grow = A.alloc([len(bl), 2, D], F32)
        S.dma("pool", wout, I["w_out"].ap()[l].rearrange("(kc p) n -> p kc n", p=128), writes=[u + "wout"])
        for bi, b in enumerate(bl):
            S.dma("sp", grow[:, bi, 0, :], md.ap()[b, 2 * D:3 * D].partition_broadcast(128), reads=["modD%d" % l], writes=[u + "grow"])
            S.dma("sp", grow[:, bi, 1, :], md.ap()[b, 5 * D:6 * D].partition_broadcast(128), reads=["modD%d" % l], writes=[u + "grow"])
        if final:
            fg = A.alloc([D], F32)
            S.dma("sp", fg, I["final_g"].ap().partition_broadcast(128), writes=[u + "fg"])
        if moe:
            rw = A.alloc([KC, NE], F32)
            rb = A.alloc([NE], F32)
            if SKIPR < 2 or SKIPR == 3:
                S.dma("sp", rw, I["moe_router_w"].ap()[0].rearrange("(kc p) n -> p kc n", p=128), writes=[u + "rw"])
                S.dma("sp", rb, I["moe_router_b"].ap()[0].partition_broadcast(128), writes=[u + "rb"])
            h32 = A.alloc([KC, 128], F32)
            comb = A.alloc([TG, NE], F32)
            zb = A.alloc([1], F32)
            tmpc = [A.alloc([512], F32) for _ in range(2)]
            S.op("pool", lambda e: e.memset(zb, 0.0), writes=[u + "zb"])
            rt = A.alloc([6, NE], F32)
        x1t = A.alloc([TG, D], F32)
        acc = A.alloc([TG, D], F32)
        ymg = A.alloc([KC, GT], BF16)
        hfT = A.alloc([KC, GT], BF16)
        actT = A.alloc([NCH, GT], BF16)
        NWB = 2 if moe else 4
        wgb = [A.alloc([KC, FBW], BF16) for _ in range(NWB)]
        wub = [A.alloc([KC, FBW], BF16) for _ in range(NWB)]
        wdb = [A.alloc([DB, 512], BF16) for _ in range(NWD)]
        sgt = [A.alloc([GT], F32) for _ in range(2)]
        xin = [A.alloc([D], F32) for _ in range(2)]
        wc = 0
        dc = 0
        pc = 0
        ec = 0
        for g in range(NT // GT):
            s_ = (g * GT) // L
            bi = s_ if len(bl) > 1 else 0
            b = bl[bi]
            S.dma("sp", ymg, Ym.ap()[:, g * GT:(g + 1) * GT].rearrange("(kc p) t -> p kc t", p=128), reads=["Ym" + tag], writes=[u + "ymg"], partial=False)
            for ti in range(TG):
                t = g * TG + ti
                xt, xk = xin[t % 2], u + "xin%d" % (t % 2)
                S.dma("sp", xt, Xsrc.ap()[t * 128:(t + 1) * 128, :], reads=[self.nm(Xsrc)], writes=[xk], partial=False)
                for half in range(2):
                    pst, pk = self.ps[half], self.psk[half]

                    def mmf(e, pst=pst, ti=ti, half=half):
                        ins = None
                        for kc in range(KC):
                            ins = e.matmul(pst, lhsT=ymg[:, kc, ti * 128:(ti + 1) * 128], rhs=wout[:, kc, half * 512:(half + 1) * 512], start=(kc == 0), stop=(kc == KC - 1))
                        return ins
                    S.op("pe", mmf, reads=[u + "ymg", u + "wout"], writes=[pk])
                    hs = slice(half * 512, (half + 1) * 512)
                    S.op("dve", lambda e, pst=pst, hs=hs, ti=ti, bi=bi: e.tensor_tensor(out=x1t[:, ti, hs], in0=pst, in1=grow[:, bi, 0, hs], op=ALU.mult),
                         reads=[pk, u + "grow"], writes=[u + "x1t%d" % ti], partial=True)
                S.op("dve", lambda e, ti=ti, xt=xt: e.tensor_tensor(out=x1t[:, ti, :], in0=x1t[:, ti, :], in1=xt, op=ALU.add),
                     reads=[u + "x1t%d" % ti, xk], writes=[u + "x1t%d" % ti])
            for ti in range(TG):
                self.norm_transpose(x1t[:, ti, :], u + "x1t%d" % ti, hfT, u + "hfT", ti * 128, 1, b, hT32=((h32, u + "h32") if (moe and SKIPR < 2) else None), single32=True)
                if moe and SKIPR:
                    pass
                if moe and SKIPR:
                    S.op("pool", lambda e, ti=ti: e.memset(comb[:, ti, :], 0.5), reads=[u + "h32"], writes=[u + "comb"])
                elif moe:
                    pst, pk = self.ps[2], self.psk[2]

                    def mmr(e, pst=pst):
                        ins = None
                        for kc in range(KC):
                            ins = e.matmul(pst[:, 0:NE], lhsT=h32[:, kc, :], rhs=rw[:, kc, :], start=(kc == 0), stop=(kc == KC - 1))
                        return ins
                    S.op("pe", mmr, reads=[u + "h32", u + "rw"], writes=[pk])
                    lg, m1, k1, l2, m2, k2 = (rt[:, i, :] for i in range(6))
                    rk_ = u + "rt"
                    dv_ = lambda fn, r=(), w=(rk_,): S.op("dve", fn, reads=[rk_] + list(r), writes=list(w))
                    dv_(lambda e, pst=pst: e.tensor_tensor(out=lg, in0=pst[:, 0:NE], in1=rb, op=ALU.add), [pk, u + "rb"])
                    dv_(lambda e: e.tensor_reduce(out=m1[:, 0:1], in_=lg, axis=AX.X, op=ALU.max))
                    dv_(lambda e: e.tensor_scalar(out=k1, in0=lg, scalar1=m1[:, 0:1], scalar2=None, op0=ALU.is_equal))
                    dv_(lambda e: e.scalar_tensor_tensor(out=l2, in0=k1, scalar=-1e30, in1=lg, op0=ALU.mult, op1=ALU.add))
                    dv_(lambda e: e.tensor_reduce(out=m2[:, 0:1], in_=l2, axis=AX.X, op=ALU.max))
                    dv_(lambda e: e.tensor_scalar(out=k2, in0=l2, scalar1=m2[:, 0:1], scalar2=None, op0=ALU.is_equal))
                    dv_(lambda e: e.tensor_tensor(out=m2[:, 1:2], in0=m2[:, 0:1], in1=m1[:, 0:1], op=ALU.subtract))
                    S.op("act", lambda e: e.activation(out=m2[:, 2:3], in_=m2[:, 1:2], func=AF.Exp), reads=[rk_], writes=[rk_])
                    dv_(lambda e: e.tensor_scalar(out=m2[:, 3:4], in0=m2[:, 2:3], scalar1=1.0, scalar2=None, op0=ALU.add))
                    dv_(lambda e: e.reciprocal(out=m2[:, 3:4], in_=m2[:, 3:4]))
                    dv_(lambda e: e.tensor_tensor(out=m2[:, 4:5], in0=m2[:, 2:3], in1=m2[:, 3:4], op=ALU.mult))
                    dv_(lambda e: e.tensor_scalar(out=k1, in0=k1, scalar1=m2[:, 3:4], scalar2=None, op0=ALU.mult))
                    dv_(lambda e, ti=ti: e.scalar_tensor_tensor(out=comb[:, ti, :], in0=k2, scalar=m2[:, 4:5], in1=k1, op0=ALU.mult, op1=ALU.add), w=(rk_, u + "comb"))
            for xi, (wg, wu, wd, eidx) in enumerate(experts):
                for fb in range(NBLK):
                    wc += 1
                    wgt, wut = wgb[wc % NWB], wub[wc % NWB]
                    wgk, wuk = u + "wg%d" % (wc % NWB), u + "wu%d" % (wc % NWB)
                    S.dma("sp", wgt, wg.ap()[:, fb * FBW:(fb + 1) * FBW].rearrange("(kc p) n -> p kc n", p=128), reads=[self.nm(wg)], writes=[wgk], partial=False)
                    S.dma("sp", wut, wu.ap()[:, fb * FBW:(fb + 1) * FBW].rearrange("(kc p) n -> p kc n", p=128), reads=[self.nm(wu)], writes=[wuk], partial=False)
                    for fc in range(FB):
                        pc += 1
                        pg, pgk = self.ps[(pc % 2) * 2], self.psk[(pc % 2) * 2]
                        pu_, puk = self.ps[(pc % 2) * 2 + 1], self.psk[(pc % 2) * 2 + 1]
                        for (pst, pk, wt, wk) in ((pg, pgk, wgt, wgk), (pu_, puk, wut, wuk)):
                            def mmf(e, pst=pst, wt=wt, fc=fc):
                                ins = None
                                for kc in range(KC):
                                    ins = e.matmul(pst[:, 0:GT], lhsT=wt[:, kc, fc * 128:(fc + 1) * 128], rhs=hfT[:, kc, :], start=(kc == 0), stop=(kc == KC - 1))
                                return ins
                            S.op("pe", mmf, reads=[wk, u + "hfT"], writes=[pk])
                        sg_, sgk = sgt[pc % 2], u + "sgt%d" % (pc % 2)
                        S.op("act", lambda e, sg_=sg_, pg=pg: e.activation(out=sg_, in_=pg[:, 0:GT], func=AF.Silu), reads=[pgk], writes=[sgk])
                        S.op("dve", lambda e, sg_=sg_, pu_=pu_, ch=fb * FB + fc: e.tensor_tensor(out=actT[:, ch, :], in0=sg_, in1=pu_[:, 0:GT], op=ALU.mult),
                             reads=[sgk, puk], writes=[u + "actT"], partial=True)
                for half in range(2):
                    hs = slice(half * 512, (half + 1) * 512)
                    for db in range(NCH // DB):
                        dc += 1
                        wdt, wdk = wdb[dc % NWD], u + "wd%d" % (dc % NWD)
                        S.dma("sp", wdt, wd.ap()[db * DB * 128:(db + 1) * DB * 128, hs].rearrange("(c p) n -> p c n", p=128), reads=[self.nm(wd)], writes=[wdk], partial=False)

                        def mmd(e, wdt=wdt, db=db):
                            ins = None
                            for cc in range(DB):
                                ch = db * DB + cc
                                for ti in range(TG):
                                    ins = e.matmul(self.ps[4 + ti], lhsT=actT[:, ch, ti * 128:(ti + 1) * 128], rhs=wdt[:, cc, :], start=(ch == 0), stop=(ch == NCH - 1))
                            return ins
                        S.op("pe", mmd, reads=[wdk, u + "actT"], writes=[self.psk[4 + ti] for ti in range(TG)], partial=(db > 0))
                    for ti in range(TG):
                        if eidx is None or SKIPC:
                            S.op("dve", lambda e, ti=ti, hs=hs: e.tensor_copy(out=acc[:, ti, hs], in_=self.ps[4 + ti]), reads=[self.psk[4 + ti]], writes=[u + "acc%d" % ti], partial=True)
                        elif xi == 0:
                            S.op("act", lambda e, ti=ti, hs=hs, eidx=eidx: e.activation(out=acc[:, ti, hs], in_=self.ps[4 + ti], func=AF.Identity,
                                                                                      scale=comb[:, ti, eidx:eidx + 1], bias=zb[:, 0:1]),
                                 reads=[self.psk[4 + ti], u + "comb", u + "zb"], writes=[u + "acc%d" % ti], partial=True)
                        else:
                            ec += 1
                            tmp, tk = tmpc[ec % 2], u + "tmpc%d" % (ec % 2)
                            S.op("act", lambda e, ti=ti, tmp=tmp, eidx=eidx: e.activation(out=tmp, in_=self.ps[4 + ti], func=AF.Identity,
                                                                                        scale=comb[:, ti, eidx:eidx + 1], bias=zb[:, 0:1]),
                                 reads=[self.psk[4 + ti], u + "comb", u + "zb"], writes=[tk])
                            S.op("dve", lambda e, ti=ti, hs=hs, tmp=tmp: e.tensor_tensor(out=acc[:, ti, hs], in0=acc[:, ti, hs], in1=tmp, op=ALU.add),
                                 reads=[tk, u + "acc%d" % ti], writes=[u + "acc%d" % ti])
            for ti in range(TG):
                t = g * TG + ti
                ak = u + "acc%d" % ti
                S.op("dve", lambda e, ti=ti, bi=bi: e.tensor_tensor(out=acc[:, ti, :], in0=acc[:, ti, :], in1=grow[:, bi, 1, :], op=ALU.mult), reads=[ak, u + "grow"], writes=[ak])
                S.op("dve", lambda e, ti=ti: e.tensor_tensor(out=acc[:, ti, :], in0=acc[:, ti, :], in1=x1t[:, ti, :], op=ALU.add), reads=[ak, u + "x1t%d" % ti], writes=[ak])
                if final:
                    ss = self._nt_ss
                    S.op("act", lambda e, ti=ti: e.activation(out=self._nt_junk, in_=acc[:, ti, :], func=AF.Square, accum_out=ss[:, 0:1]), reads=[ak], writes=["nt_ss", "nt_junk"])
                    S.op("act", lambda e: e.activation(out=ss[:, 1:2], in_=ss[:, 0:1], func=AF.Sqrt, scale=1.0 / D, bias=self.epsb[:, 0:1]), reads=["nt_ss", "epsb"], writes=["nt_ss"])
                    S.op("dve", lambda e: e.reciprocal(out=ss[:, 2:3], in_=ss[:, 1:2]), reads=["nt_ss"], writes=["nt_rs"])
                    S.op("dve", lambda e, ti=ti: e.scalar_tensor_tensor(out=acc[:, ti, :], in0=acc[:, ti, :], scalar=ss[:, 2:3], in1=fg, op0=ALU.mult, op1=ALU.mult),
                         reads=[ak, "nt_rs", u + "fg"], writes=[ak])
                S.dma("sp", Xdst.ap()[t * 128:(t + 1) * 128, :], acc[:, ti, :], reads=[ak], writes=[self.nm(Xdst)])
        self.S.barrier()
        A.release(m0)

    def program(self):
        S = self.S
        stop = self.stop_after
        xres = self.dscr("xres", [NS * L_LAT, D], F32)
        cres = self.dscr("cres", [NS * L_CTX, D], F32)
        for l in range(DEPTH):
            last = (l == DEPTH - 1)
            S.phase = "prep%d" % l
            self.prep_mod(l)
            self.prep_fnet(l)
            S.barrier()
            csrc = self.I["ctx"] if l == 0 else cres
            S.phase = "ctx%d" % l
            self.phaseA(l, csrc, L_CTX, False, lambda s: 2, "c")
            if not last:
                self.fnet(l, L_CTX, "c")
                self.hyena_filters(l, L_CTX, "c")
                self.hyena(l, L_CTX, "c")
            ms = self.arena.mark()
            P = self.s5_prep(l)
            self.s5_main(l, L_CTX, "c", P, False, not last)
            S.op("act", lambda e: e.copy(out=self.s0, in_=self.s0n), reads=["s0n"], writes=["s0"])
            S.barrier()
            self.arena.release(ms)
            if not last:
                self.phaseC(l, csrc, cres, L_CTX, "c", [2], False, False)
            if stop == "C%d" % l:
                break
            xsrc = self.I["x"] if l == 0 else xres
            xdst = self.out if last else xres
            S.phase = "xA%d" % l
            self.phaseA(l, xsrc, L_LAT, True, lambda s: s, "x")
            if stop == "xA%d" % l:
                break
            S.phase = "xF%d" % l
            self.fnet(l, L_LAT, "x")
            if stop == "xF%d" % l:
                break
            S.phase = "xG%d" % l
            self.hyena_filters(l, L_LAT, "x")
            if stop == "xG%d" % l:
                break
            S.phase = "xH%d" % l
            self.hyena(l, L_LAT, "x")
            if stop == "xH%d" % l:
                break
            ms = self.arena.mark()
            S.phase = "xS%d" % l
            P = self.s5_prep(l)
            if l == 0 and DEPTH > 1 and stop is None:
                self.cast_moe(1)
            self.s5_main(l, L_LAT, "x", P, True, True)
            S.barrier()
            self.arena.release(ms)
            if stop == "M%d" % l:
                break
            S.phase = "xC%d" % l
            self.phaseC(l, xsrc, xdst, L_LAT, "x", [0, 1], last, last)
            if stop == "L%d" % l:
                break
        self.finish()

    def finish(self):
        S = self.S
        keys = [k for k in S.res.keys() if k in self.debug or k == "out"]
        toks = []
        for k in list(S.res.keys()):
            r = S.res[k]
            toks += r["w"] + r["r"]
        waits = S._waits("sp", toks)
        S.q["sp"].append((waits, None, None, 0, S.phase))
        S.emit()
        self.st.close()


_CACHE = {}


def make_in_maps(inputs, cores=range(NCORES)):
    cst = _CACHE.get("consts")
    if cst is None:
        cst = _consts()
        _CACHE["consts"] = cst
    maps = []
    f32 = lambda a: np.ascontiguousarray(np.asarray(a, dtype=np.float32))
    shared = {}
    for name, shape in IN_SPECS:
        if name in ("x", "ctx", "cvec"):
            continue
        shared[name] = f32(inputs[name]).reshape(shape)
    x = np.asarray(inputs["x"])
    ctx = np.asarray(inputs["ctx"])
    c = np.asarray(inputs["c"])
    c_ctx = np.asarray(inputs["c_ctx"])
    for ci in cores:
        m = dict(shared)
        m.update(cst)
        m["x"] = f32(x[ci * NS:(ci + 1) * NS]).reshape(NS * L_LAT, D)
        m["ctx"] = f32(ctx[ci * NS:(ci + 1) * NS]).reshape(NS * L_CTX, D)
        cc = np.stack([c[ci * NS], c[ci * NS + 1], c_ctx], axis=0)
        m["cvec"] = f32(cc.T.reshape(KC, 128, 3).transpose(1, 0, 2))
        maps.append(m)
    return maps


def kernel(**inputs):
    B = _CACHE.get("B")
    if B is None:
        B = Builder()
        B.program()
        _CACHE["B"] = B
    maps = make_in_maps(inputs)
    res = run_bass_kernel_spmd(B.nc, maps, core_ids=list(range(NCORES)))
    out = np.concatenate([r["out"].reshape(NS, L_LAT, D) for r in res.results], axis=0)
    return out.astype(np.float32)
```
